# Optimizing a Trainium2 kernel written in Bass

```python
import jax, jax.numpy as jnp
from jax import lax
import numpy as np

D_MODEL = 1024
BATCH = 8
SEQ = 4096
DEPTH = 1

CHUNK = 64
Q_BLOCK = 128
RET_HEADS = 4
RET_DK = 128
RET_DV = 128
RET_W = RET_HEADS * RET_DK
RET_VW = RET_HEADS * RET_DV
SB_HEADS = 8
SB_DH = 64
SB_W = SB_HEADS * SB_DH
ROPE_BASE = 10000.0
PLE_DIM = 256
N_GROUPS = 4
EXPERTS_PER_GROUP = 4
N_EXPERTS = N_GROUPS * EXPERTS_PER_GROUP
TOP_K = 2
D_EXPERT = 512
EXPERT_BLOCK = 128
EPS = 1e-6
IN_WIDTHS = (RET_W, RET_W, RET_VW, RET_VW, SB_W, SB_W, SB_W, D_MODEL, D_MODEL)
IN_SPLITS = tuple(int(c) for c in np.cumsum(IN_WIDTHS)[:-1])
W_IN_COLS = sum(IN_WIDTHS)

kernel_name = "hybrid_retention_stickbreak_hmoe_block"


def rms_norm(x, g):
    xf = x.astype(jnp.float32)
    y = xf * lax.rsqrt(jnp.mean(xf * xf, axis=-1, keepdims=True) + EPS) * g.astype(jnp.float32)
    return y.astype(x.dtype)


def apply_rope(t, cos, sin):
    half = t.shape[-1] // 2
    t1, t2 = t[..., :half], t[..., half:]
    c = cos[None, :, None, :]
    s = sin[None, :, None, :]
    return jnp.concatenate([t1 * c - t2 * s, t1 * s + t2 * c], axis=-1)


def retention(q, k, v, gate, gn_gain):
    b, s, h, dk = q.shape
    dv = v.shape[-1]
    nc = s // CHUNK
    gamma = 1.0 - jnp.exp2(-5.0 - jnp.arange(h, dtype=jnp.float32))
    log_g = jnp.log(gamma)
    idx = jnp.arange(CHUNK, dtype=jnp.float32)
    d_intra = jnp.exp(log_g[:, None, None] * jnp.abs(idx[:, None] - idx[None, :]))
    q_dec = jnp.exp(log_g[:, None] * (idx + 1.0))
    k_dec = jnp.exp(log_g[:, None] * (CHUNK - 1.0 - idx))
    c_dec = jnp.exp(log_g * CHUNK)

    def to_chunks(t):
        return t.reshape(b, nc, CHUNK, h, t.shape[-1]).transpose(1, 0, 3, 2, 4)

    qc, kc, vc = to_chunks(q), to_chunks(k), to_chunks(v)

    def step(state, inp):
        qi, ki, vi = inp
        scores = jnp.einsum('bhid,bhjd->bhij', qi, ki) * d_intra
        o = (jnp.einsum('bhij,bhje->bhie', scores, vi)
             + jnp.einsum('bhid,bhde->bhie', qi * q_dec[..., None], state))
        state = c_dec[:, None, None] * state + jnp.einsum('bhjd,bhje->bhde', ki * k_dec[..., None], vi)
        return state, o

    state0 = jnp.zeros((b, h, dk, dv), jnp.float32)
    _, oc = lax.scan(step, state0, (qc, kc, vc))
    o = oc.transpose(1, 0, 3, 2, 4).reshape(b, s, h, dv)
    mu = jnp.mean(o, axis=-1, keepdims=True)
    var = jnp.mean((o - mu) ** 2, axis=-1, keepdims=True)
    o = ((o - mu) * lax.rsqrt(var + EPS)).reshape(b, s, h * dv) * gn_gain.astype(jnp.float32)
    return jax.nn.silu(gate) * o


def stick_breaking(q, k, v):
    b, s, h, d = q.shape
    q = q.transpose(0, 2, 1, 3) * (d ** -0.5)
    k = k.transpose(0, 2, 1, 3)
    v = v.transpose(0, 2, 1, 3)
    outs = []
    for i in range(s // Q_BLOCK):
        kl = (i + 1) * Q_BLOCK
        qb = q[:, :, i * Q_BLOCK:kl]
        z = jnp.einsum('bhtd,bhsd->bhts', qb, k[:, :, :kl])
        t_pos = i * Q_BLOCK + jnp.arange(Q_BLOCK)
        mask = jnp.arange(kl)[None, :] < t_pos[:, None]
        log_keep = jnp.where(mask, jax.nn.log_sigmoid(-z), 0.0)
        later = lax.cumsum(log_keep, axis=3, reverse=True) - log_keep
        a = jnp.where(mask, jnp.exp(jax.nn.log_sigmoid(z) + later), 0.0)
        outs.append(jnp.einsum('bhts,bhsd->bhtd', a, v[:, :, :kl]))
    o = jnp.concatenate(outs, axis=2)
    return o.transpose(0, 2, 1, 3).reshape(b, s, h * d)


def hier_moe(h, w_grp, b_grp, w_exp, b_exp, w_gate, w_up, w_down):
    b, s, dm = h.shape
    n = b * s
    hf = h.reshape(n, dm)
    grp_prob = jax.nn.softmax((hf @ w_grp).astype(jnp.float32) + b_grp.astype(jnp.float32), axis=-1)
    g_idx = jnp.argmax(grp_prob, axis=-1)
    p_grp = jnp.take_along_axis(grp_prob, g_idx[:, None], axis=1)[:, 0]
    exp_logits = ((hf @ w_exp).astype(jnp.float32) + b_exp.astype(jnp.float32)).reshape(n, N_GROUPS, EXPERTS_PER_GROUP)
    in_grp = jnp.take_along_axis(exp_logits, g_idx[:, None, None], axis=1)[:, 0]
    top_p, top_i = lax.top_k(jax.nn.softmax(in_grp, axis=-1), TOP_K)
    weights = top_p / jnp.sum(top_p, axis=-1, keepdims=True) * p_grp[:, None]
    expert_id = g_idx[:, None] * EXPERTS_PER_GROUP + top_i

    a = n * TOP_K
    flat_e = expert_id.reshape(a)
    flat_tok = jnp.repeat(jnp.arange(n, dtype=jnp.int32), TOP_K)
    flat_w = weights.reshape(a)
    order = jnp.argsort(flat_e)
    sorted_e = flat_e[order]
    counts = jax.ops.segment_sum(jnp.ones((a,), jnp.int32), flat_e, num_segments=N_EXPERTS)
    padded = (counts + EXPERT_BLOCK - 1) // EXPERT_BLOCK * EXPERT_BLOCK
    pad_end = jnp.cumsum(padded)
    pad_start = pad_end - padded
    start = jnp.cumsum(counts) - counts
    dest = pad_start[sorted_e] + (jnp.arange(a, dtype=jnp.int32) - start[sorted_e])
    m = a + N_EXPERTS * EXPERT_BLOCK
    nb = m // EXPERT_BLOCK
    buf_tok = jnp.zeros((m,), jnp.int32).at[dest].set(flat_tok[order])
    buf_w = jnp.zeros((m,), jnp.float32).at[dest].set(flat_w[order])
    block_e = jnp.minimum(
        jnp.searchsorted(pad_end, jnp.arange(nb, dtype=jnp.int32) * EXPERT_BLOCK, side='right'),
        N_EXPERTS - 1)
    xb = hf[buf_tok].reshape(nb, EXPERT_BLOCK, dm)

    def expert_block(args):
        xi, e = args
        hid = jax.nn.silu(xi @ w_gate[e]) * (xi @ w_up[e])
        return hid @ w_down[e]

    yb = lax.map(expert_block, (xb, block_e)).reshape(m, dm)
    y = jax.ops.segment_sum(yb.astype(jnp.float32) * buf_w[:, None], buf_tok, num_segments=n)
    return y.reshape(b, s, dm).astype(h.dtype)


def setup_inputs(seed: int = 0) -> dict:
    key = jax.random.key(seed)
    ks = jax.random.split(key, 24)
    f32 = jnp.float32

    def nrm(k, shape, fan_in):
        return jax.random.normal(k, shape, f32) * (fan_in ** -0.5)

    def gain(k, shape):
        return 1.0 + 0.02 * jax.random.normal(k, shape, f32)

    return {
        "x": jax.random.normal(ks[0], (BATCH, SEQ, D_MODEL), f32),
        "p": jax.random.normal(ks[1], (DEPTH, BATCH, SEQ, PLE_DIM), f32),
        "norm_mix": gain(ks[2], (DEPTH, D_MODEL)),
        "w_in": nrm(ks[3], (DEPTH, D_MODEL, W_IN_COLS), D_MODEL),
        "ret_gn": gain(ks[4], (DEPTH, RET_VW)),
        "w_br_ret": nrm(ks[5], (DEPTH, RET_VW, D_MODEL), RET_VW),
        "w_br_sb": nrm(ks[6], (DEPTH, SB_W, D_MODEL), SB_W),
        "w_out": nrm(ks[7], (DEPTH, D_MODEL, D_MODEL), D_MODEL),
        "norm_ffn": gain(ks[8], (DEPTH, D_MODEL)),
        "w_grp": nrm(ks[9], (DEPTH, D_MODEL, N_GROUPS), D_MODEL),
        "b_grp": 0.01 * jax.random.normal(ks[10], (DEPTH, N_GROUPS), f32),
        "w_exp": nrm(ks[11], (DEPTH, D_MODEL, N_EXPERTS), D_MODEL),
        "b_exp": 0.01 * jax.random.normal(ks[12], (DEPTH, N_EXPERTS), f32),
        "w_gate": nrm(ks[13], (DEPTH, N_EXPERTS, D_MODEL, D_EXPERT), D_MODEL),
        "w_up": nrm(ks[14], (DEPTH, N_EXPERTS, D_MODEL, D_EXPERT), D_MODEL),
        "w_down": nrm(ks[15], (DEPTH, N_EXPERTS, D_EXPERT, D_MODEL), D_EXPERT),
        "norm_ple": gain(ks[16], (DEPTH, D_MODEL)),
        "w_ple": nrm(ks[17], (DEPTH, PLE_DIM, D_MODEL), PLE_DIM),
        "w_ple_gate": nrm(ks[18], (DEPTH, D_MODEL, D_MODEL), D_MODEL),
        "norm_final": gain(ks[19], (D_MODEL,)),
    }


def reference(x, p, norm_mix, w_in, ret_gn, w_br_ret, w_br_sb, w_out, norm_ffn,
              w_grp, b_grp, w_exp, b_exp, w_gate, w_up, w_down,
              norm_ple, w_ple, w_ple_gate, norm_final):
    b, s, _ = x.shape
    f32 = jnp.float32
    pos = jnp.arange(s, dtype=f32)
    inv_freq = ROPE_BASE ** (-jnp.arange(0, RET_DK, 2, dtype=f32) / RET_DK)
    ang = pos[:, None] * inv_freq[None, :]
    cos, sin = jnp.cos(ang), jnp.sin(ang)
    for i in range(DEPTH):
        h = rms_norm(x, norm_mix[i])
        proj = h @ w_in[i]
        rq, rk, rv, rg, sq, sk, sv, g_ret, g_sb = jnp.split(proj, IN_SPLITS, axis=-1)
        rq = apply_rope(rq.reshape(b, s, RET_HEADS, RET_DK).astype(f32), cos, sin)
        rk = apply_rope(rk.reshape(b, s, RET_HEADS, RET_DK).astype(f32), cos, sin) * (RET_DK ** -0.5)
        rv = rv.reshape(b, s, RET_HEADS, RET_DV).astype(f32)
        ret = retention(rq, rk, rv, rg.astype(f32), ret_gn[i]).astype(x.dtype)
        sb = stick_breaking(sq.reshape(b, s, SB_HEADS, SB_DH).astype(f32),
                            sk.reshape(b, s, SB_HEADS, SB_DH).astype(f32),
                            sv.reshape(b, s, SB_HEADS, SB_DH).astype(f32)).astype(x.dtype)
        merged = (jax.nn.sigmoid(g_ret) * (ret @ w_br_ret[i])
                  + jax.nn.sigmoid(g_sb) * (sb @ w_br_sb[i]))
        x = x + merged @ w_out[i]
        x = x + hier_moe(rms_norm(x, norm_ffn[i]), w_grp[i], b_grp[i], w_exp[i], b_exp[i],
                         w_gate[i], w_up[i], w_down[i])
        hp = rms_norm(x, norm_ple[i])
        x = x + jax.nn.sigmoid(hp @ w_ple_gate[i]) * (p[i].astype(x.dtype) @ w_ple[i])
    return rms_norm(x, norm_final)
```

```python
import numpy as np
from contextlib import ExitStack
import ml_dtypes
from concourse.bass_utils import run_bass_kernel_spmd
import concourse.bass as bass
import concourse.mybir as mybir

F32 = mybir.dt.float32
BF16 = mybir.dt.bfloat16
I32 = mybir.dt.int32
AF = mybir.ActivationFunctionType
ALU = mybir.AluOpType
AX = mybir.AxisListType


class Sched:
    RING = 12

    def __init__(self, nc, stack):
        self.nc = nc
        self.eng = dict(pe=nc.tensor, act=nc.scalar, dve=nc.vector, pool=nc.gpsimd, sp=nc.sync)
        self.ops = []
        self.sem = {e: stack.enter_context(nc.semaphore("sem_" + e)) for e in self.eng}
        self.rings = {
            q: [stack.enter_context(nc.semaphore("ring_%s_%d" % (q, i))) for i in range(self.RING)]
            for q in ("sp", "pool")
        }

    def op(self, eng, fn, r=(), w=()):
        self.ops.append((eng, fn, tuple(r), tuple(w), False))

    def dma(self, q, fn, r=(), w=()):
        self.ops.append((q, fn, tuple(r), tuple(w), True))

    def barrier(self):
        self.ops.append(("barrier", None, (), (), False))

    def emit(self):
        ops = self.ops
        n = len(ops)
        last_w = {}
        readers = {}
        deps = [None] * n
        need_sig = [False] * n
        last_on = {}
        for i, (e, fn, r, w, isd) in enumerate(ops):
            if e == "barrier":
                for x, j in last_on.items():
                    need_sig[j] = True
                deps[i] = set()
                continue
            d = set()
            for k in r:
                if k in last_w:
                    d.update(last_w[k])
            for k in w:
                if k in last_w:
                    d.update(last_w[k])
                d.update(readers.get(k, ()))
            d.discard(i)
            best = {}
            dd = set()
            for j in d:
                ej, _, _, _, isdj = ops[j]
                if isdj:
                    dd.add(j)
                else:
                    if ej not in best or best[ej] < j:
                        best[ej] = j
            for ej, j in best.items():
                if ej == "pe" and e == "pe" and not isd:
                    continue
                dd.add(j)
            deps[i] = dd
            for j in dd:
                need_sig[j] = True
            for k in w:
                if isd and k in last_w and all(ops[j][4] for j in last_w[k]) and not readers.get(k):
                    last_w[k] = last_w[k] + [i]
                else:
                    last_w[k] = [i]
                readers[k] = []
            for k in r:
                if k not in w:
                    readers.setdefault(k, []).append(i)
            if not isd:
                last_on[e] = i
        cnt = {e: 0 for e in self.eng}
        sig = [None] * n
        waited = {e: {} for e in self.eng}
        dma_n = {q: 0 for q in self.rings}
        ring_last = {}
        nwaits = 0

        snap = {}

        def wait(e, sem, val):
            nonlocal nwaits
            key = id(sem)
            if waited[e].get(key, 0) >= val:
                return
            waited[e][key] = val
            self.eng[e].wait_ge(sem, val)
            nwaits += 1
            for k2, v2 in snap.get((key, val), {}).items():
                if waited[e].get(k2, 0) < v2:
                    waited[e][k2] = v2

        for i, (e, fn, r, w, isd) in enumerate(ops):
            if e == "barrier":
                for x in self.eng:
                    for y in self.eng:
                        if cnt[y] > 0:
                            wait(x, self.sem[y], cnt[y])
                    for q in self.rings:
                        for s_, v_ in ring_last.get(q, {}).values():
                            wait(x, s_, v_)
                continue
            for j in sorted(deps[i]):
                s_, v_ = sig[j]
                wait(e, s_, v_)
            if isd:
                m = dma_n[e]
                dma_n[e] += 1
                s_ = self.rings[e][m % self.RING]
                v_ = 16 * (m // self.RING + 1)
                if v_ > 16:
                    wait(e, s_, v_ - 16)
                inst = fn(self.eng[e])
                inst.then_inc(s_, 16)
                sig[i] = (s_, v_)
                snap[(id(s_), v_)] = dict(waited[e])
                ring_last.setdefault(e, {})[m % self.RING] = (s_, v_)
            else:
                inst = fn(self.eng[e])
                if need_sig[i]:
                    cnt[e] += 1
                    inst.then_inc(self.sem[e], 1)
                    sig[i] = (self.sem[e], cnt[e])
                    snap[(id(self.sem[e]), cnt[e])] = dict(waited[e])
        for q in self.rings:
            for s_, v_ in ring_last.get(q, {}).values():
                wait("sp", s_, v_)
        self.stats = dict(n_ops=n, n_waits=nwaits, cnt=dict(cnt), dma=dict(dma_n))
        return self.stats


D = 1024
EPS = 1e-6
BF = ml_dtypes.bfloat16


def consts_np(S, C):
    NB = S // 128
    c = {}
    c["ident_bf"] = np.eye(128, dtype=np.float32).astype(BF)
    c["ident_f"] = np.eye(128, dtype=np.float32)
    j = np.arange(128)[:, None]
    s = np.arange(128)[None, :]
    c["ntri"] = np.where(j >= s, -1.0, 0.0).astype(np.float32).astype(BF)
    c["nones"] = (-np.ones((128, 128), np.float32)).astype(BF)
    c["tri_lt"] = np.where(j < s, 1.0, 0.0).astype(np.float32).astype(BF)
    c["ones_bf"] = np.ones((128, 128), np.float32).astype(BF)
    m = np.zeros((4, 128, 512), np.float32)
    for jd in range(4):
        key = jd * 128 + np.arange(128)[:, None]
        tq = np.arange(512)[None, :]
        m[jd] = (key < tq).astype(np.float32)
    c["mask01"] = ((m.transpose(1, 0, 2) - 1.0) * 30000.0).astype(np.float32).copy()
    pos = np.arange(S, dtype=np.float32)
    inv_freq = (10000.0 ** (-np.arange(0, 128, 2, dtype=np.float32) / 128)).astype(np.float32)
    ang = pos[:, None] * inv_freq[None, :]
    c["cos_t"] = np.cos(ang).astype(np.float32).reshape(NB, 128, 64).transpose(1, 0, 2).copy()
    c["sin_t"] = np.sin(ang).astype(np.float32).reshape(NB, 128, 64).transpose(1, 0, 2).copy()
    gamma = 1.0 - np.exp2(-5.0 - np.arange(4, dtype=np.float64))
    lg = np.log(gamma)
    jj = np.arange(128)[:, None]
    ii = np.arange(128)[None, :]
    allowed = (jj // 64) <= (ii // 64)
    sc = 128.0 ** -0.5
    Dp = np.zeros((128, 4, 128), np.float64)
    for h in range(4):
        Dp[:, h, :] = np.where(allowed, np.exp(lg[h] * (np.abs(ii - jj) - (ii + 1.0))), 0.0) * sc
    c["Dp"] = Dp.astype(np.float32)
    qdec = np.exp(lg[None, :] * (np.arange(128)[:, None] + 1.0))
    c["qdec"] = np.repeat(qdec[:, :, None], 128, axis=2).astype(np.float32)
    kdec = np.exp(lg[None, :] * (127.0 - np.arange(128)[:, None])) * sc
    c["kdec"] = np.repeat(kdec[:, :, None], 128, axis=2).astype(np.float32)
    c["cdec"] = np.exp(lg * 128.0)
    c["ebase"] = np.tile((np.arange(16, dtype=np.float32) * C)[None], (128, 1))
    return c


def rep(v):
    return np.tile(np.asarray(v, np.float32).reshape(1, -1), (128, 1))


def build(S, C, debug=False):
    NB = S // 128
    NG = S // 512
    cst = consts_np(S, C)
    cdec = [float(v) for v in cst["cdec"]]
    nc = bass.Bass("TRN2", target_bir_lowering=False)

    def din(name, shape, dt=F32):
        return nc.dram_tensor(name, list(shape), dt, kind="ExternalInput").ap()

    def dscr(name, shape, dt):
        return nc.dram_tensor(name, list(shape), dt, kind="ExternalOutput" if debug else "Internal").ap()

    x = din("x", [S, D])
    p_in = din("p", [S, 256])
    w_in = din("w_in", [D, 5632])
    w_br_ret = din("w_br_ret", [512, D])
    w_br_sb = din("w_br_sb", [512, D])
    w_out = din("w_out", [D, D])
    w_rt = din("w_rt", [D, 20])
    b_rt = din("b_rt", [128, 20])
    w_gate = din("w_gate", [16, D, 512])
    w_up = din("w_up", [16, D, 512])
    w_down = din("w_down", [16, 512, D])
    w_ple = din("w_ple", [256, D])
    w_pg = din("w_ple_gate", [D, D])
    gmix = din("gmix", [128, D])
    gffn = din("gffn", [128, D])
    gple = din("gple", [128, D])
    gfin = din("gfin", [128, D])
    ggn = din("ggn", [128, 512])
    ident_d = din("ident_bf", [128, 128], BF16)
    identf_d = din("ident_f", [128, 128])
    ntri_d = din("ntri", [128, 128], BF16)
    nones_d = din("nones", [128, 128], BF16)
    trilt_d = din("tri_lt", [128, 128], BF16)
    onesbf_d = din("ones_bf", [128, 128], BF16)
    mask01_d = din("mask01", [128, 4, 512])
    cos_d = din("cos_t", [128, NB, 64])
    sin_d = din("sin_t", [128, NB, 64])
    Dp_d = din("Dp", [128, 4, 128])
    qdec_d = din("qdec", [128, 4, 128])
    kdec_d = din("kdec", [128, 4, 128])
    ebase_d = din("ebase", [128, 16])

    out_d = nc.dram_tensor("out", [S, D], F32, kind="ExternalOutput").ap()
    sbT_d = dscr("sbT", [8, 64, S], BF16)
    retT_d = dscr("retT", [128, 4, S], BF16)
    x1_d = dscr("x1", [S, D], F32)
    xe_d = dscr("xe", [16 * C, D], BF16)
    ye_d = dscr("ye", [16 * C, D], F32)
    if debug:
        rt_d = nc.dram_tensor("rt_dbg", [128, NB, 4], F32, kind="ExternalOutput").ap()
        x2_d = nc.dram_tensor("x2_dbg", [S, D], F32, kind="ExternalOutput").ap()

    with ExitStack() as st0:
        sc = Sched(nc, st0)
        psr = [st0.enter_context(nc.psum_tensor("psr%d" % i, [128, 1024], F32)) for i in range(4)]
        ps = [psr[i // 2][:, (i % 2) * 512:(i % 2 + 1) * 512] for i in range(8)]
        cpc = [0]

        def evac(out, in_, r, w, eng=None):
            if eng is None:
                eng = "act" if cpc[0] % 2 == 0 else "dve"
                cpc[0] += 1
            if eng == "act":
                sc.op("act", lambda e: e.copy(out=out, in_=in_), r=r, w=w)
            else:
                sc.op("dve", lambda e: e.tensor_copy(out=out, in_=in_), r=r, w=w)

        def ld(q, out, in_, w, r=()):
            sc.dma(q, lambda e: e.dma_start(out=out, in_=in_), r=r, w=w)

        def sbt(stk, name, shape, dt):
            return stk.enter_context(nc.sbuf_tensor(name, list(shape), dt))

        def rstd_ops(ssq_ap, rs_ap, kssq, krs, n=D):
            sc.op("dve", lambda e: e.tensor_scalar(out=rs_ap, in0=ssq_ap, scalar1=1.0 / n, scalar2=EPS,
                                                   op0=ALU.mult, op1=ALU.add), r=[kssq], w=[krs])
            pow_ops(rs_ap, krs)

        def pow_ops(rs_ap, krs):
            sc.op("pool", lambda e: e.tensor_tensor(out=rs_ap, in0=rs_ap, in1=nhalf[:, 0:rs_ap.shape[-1]], op=ALU.pow),
                  r=[krs, "nhalf"], w=[krs])

        ident = sbt(st0, "ident", [128, 128], BF16)
        ld("sp", ident[:], ident_d, ["ident"])
        junk = sbt(st0, "junk", [128, D], BF16)
        nhalf = sbt(st0, "nhalf", [128, 8], F32)
        sc.op("pool", lambda e: e.memset(nhalf[:], -0.5), w=["nhalf"])

        hT_d = dscr("hT_scr", [128, 8, S], BF16)
        qT_d = dscr("qT_scr", [128, 4, S], BF16)
        with ExitStack() as stH:
            with ExitStack() as st:
                ntri = sbt(st, "ntri_s", [128, 128], BF16)
                nones = sbt(st, "nones_s", [128, 128], BF16)
                mask01 = sbt(st, "mask01_s", [128, 4, 512], BF16)
                ld("sp", ntri[:], ntri_d, ["ntri"])
                ld("sp", nones[:], nones_d, ["nones"])
                ld("pool", mask01[:], mask01_d, ["mask01"])
                kT = sbt(st, "kT", [128, 4, S], BF16)
                vv = sbt(st, "vv", [128, NB, 512], BF16)
                qg = [sbt(st, "qg%d" % i, [128, 4, 512], BF16) for i in range(2)]
                with ExitStack() as stA:
                    hT = sbt(stA, "hT", [128, 8, S], BF16)
                    wsb = sbt(stA, "wsb", [128, 8, 1536], BF16)
                    qst = [sbt(stA, "qst%d" % i, [128, 512], BF16) for i in range(2)]
                    w_v = w_in.rearrange("(k p) c -> p k c", p=128)
                    for k in range(8):
                        ld("pool", wsb[:, k, :], w_v[:, k, 2048:3584], [("wsb", k)])
                    WSB = [("wsb", k) for k in range(8)]
                    gmix_s = sbt(stA, "gmix_s", [128, D], F32)
                    ld("sp", gmix_s[:], gmix, ["gmix"])
                    xt = [sbt(stA, "xt%d" % i, [128, D], F32) for i in range(4)]
                    xn = [sbt(stA, "xn%d" % i, [128, D], BF16) for i in range(3)]
                    ssq = sbt(stA, "ssq", [128, NB], F32)
                    rs = sbt(stA, "rs", [128, NB], F32)
                    pj = [0]
                    qcn = [0]

                    def pbank():
                        pj[0] += 1
                        return 4 + pj[0] % 4

                    def proj_qk(T, j, which):
                        bank = pbank()
                        for k in range(8):
                            sc.op("pe", lambda e, k=k: e.matmul(
                                ps[bank][:], lhsT=wsb[:, k, which * 512 + j * 128:which * 512 + (j + 1) * 128],
                                rhs=hT[:, k, T * 512:(T + 1) * 512], start=(k == 0), stop=(k == 7)),
                                r=WSB + [("hT", T * 4 + i) for i in range(4)], w=[("ps", bank)])
                        if which == 0:
                            q2 = qcn[0] % 2
                            qcn[0] += 1
                            sc.op("act", lambda e: e.mul(out=qst[q2][:], in_=ps[bank][:], mul=0.125),
                                  r=[("ps", bank)], w=[("qst", q2)])
                            ld("sp", qT_d[:, j, T * 512:(T + 1) * 512], qst[q2][:], [("qT_d", T)], r=[("qst", q2)])
                        else:
                            evac(kT[:, j, T * 512:(T + 1) * 512], ps[bank][:], r=[("ps", bank)], w=[("kT", T)], eng="dve")

                    def proj_v(b):
                        bank = pbank()
                        for k in range(8):
                            sc.op("pe", lambda e, k=k: e.matmul(
                                ps[bank][:], lhsT=hT[:, k, b * 128:(b + 1) * 128], rhs=wsb[:, k, 1024:1536],
                                start=(k == 0), stop=(k == 7)),
                                r=WSB + [("hT", b)], w=[("ps", bank)])
                        evac(vv[:, b, :], ps[bank][:], r=[("ps", bank)], w=[("vv", b)], eng="dve")

                    pq_ = []

                    def a_s0(b):
                        ld("sp", xt[b % 4][:], x[b * 128:(b + 1) * 128, :], [("xt", b % 4)])

                    def a_s1(b):
                        sc.op("act", lambda e: e.activation(out=junk[:], in_=xt[b % 4][:], func=AF.Square, accum_out=ssq[:, b:b + 1]),
                              r=[("xt", b % 4)], w=["junk", ("ssq", b)])
                        rstd_ops(ssq[:, b:b + 1], rs[:, b:b + 1], ("ssq", b), ("rs", b))

                    def a_s2(b):
                        sc.op("dve", lambda e: e.scalar_tensor_tensor(
                            out=xn[b % 3][:], in0=xt[b % 4][:], scalar=rs[:, b:b + 1], in1=gmix_s[:], op0=ALU.mult, op1=ALU.mult),
                            r=[("xt", b % 4), ("rs", b), "gmix"], w=[("xn", b % 3)])

                    def a_s3(b):
                        pb = ps[b % 4].bitcast(BF16)
                        for k in range(8):
                            sc.op("pe", lambda e, k=k, pb=pb: e.transpose(
                                out=pb[:, k * 128:(k + 1) * 128], in_=xn[b % 3][:, k * 128:(k + 1) * 128], identity=ident[:]),
                                r=[("xn", b % 3), "ident"], w=[("ps", b % 4)])
                        evac(hT[:, :, b * 128:(b + 1) * 128], pb.rearrange("p (k t) -> p k t", k=8),
                             r=[("ps", b % 4)], w=[("hT", b)], eng="act")
                        if b % 4 == 3:
                            T_ = b // 4
                            ld("sp", hT_d[:, :, T_ * 512:(T_ + 1) * 512], hT[:, :, T_ * 512:(T_ + 1) * 512], [("hT_d", T_)],
                               r=[("hT", T_ * 4 + i) for i in range(4)])
                            for j_ in range(4):
                                for wh_ in range(2):
                                    pq_.append(lambda T_=T_, j_=j_, wh_=wh_: proj_qk(T_, j_, wh_))
                            for i_ in range(4):
                                pq_.append(lambda bb=T_ * 4 + i_: proj_v(bb))

                    stg1 = [a_s0, a_s1, a_s2, a_s3]
                    for step in range(NB + len(stg1) - 1):
                        for si in reversed(range(len(stg1))):
                            bb_ = step - si
                            if 0 <= bb_ < NB:
                                stg1[si](bb_)
                        for _ in range(3):
                            if pq_:
                                pq_.pop(0)()
                    while pq_:
                        pq_.pop(0)()
                sc.barrier()
                NE = 3
                e_bf = [sbt(st, "e_bf%d" % i, [128, 2, 512], BF16) for i in range(NE)]
                sp_bf = [sbt(st, "sp_bf%d" % i, [128, 2, 512], BF16) for i in range(NE)]
                E_bf = [sbt(st, "E_bf%d" % i, [128, 2, 512], BF16) for i in range(2)]
                NA = 5
                a_bf = [sbt(st, "a_bf%d" % i, [128, 2, 512], BF16) for i in range(NA)]
                S_bf = [sbt(st, "S_bf%d" % i, [128, 2, 512], BF16) for i in range(3)]
                o_sb = [sbt(st, "o_sb%d" % i, [128, 512], BF16) for i in range(2)]
                zt = sbt(st, "zt", [128, 1024], BF16)
                sc.op("pool", lambda e: e.memset(zt[:], 0.0), w=["zt"])


                with ExitStack() as st3:
                    wr = sbt(st3, "wr", [128, 8, 2048], BF16)
                    w_v = w_in.rearrange("(k p) c -> p k c", p=128)
                    for k in range(8):
                        ld("pool", wr[:, k, :], w_v[:, k, 0:2048], [("wr", k)])
                    WR = [("wr", k) for k in range(8)]
                    Dp_s = sbt(st3, "Dp_s", [128, 4, 128], F32)
                    qdec_s = sbt(st3, "qdec_s", [128, 4, 128], F32)
                    kdec_s = sbt(st3, "kdec_s", [128, 4, 128], F32)
                    ggn_s = sbt(st3, "ggn_s", [128, 512], F32)
                    ld("sp", Dp_s[:], Dp_d, ["Dp"])
                    ld("sp", qdec_s[:], qdec_d, ["qdec"])
                    ld("sp", kdec_s[:], kdec_d, ["kdec"])
                    ld("sp", ggn_s[:], ggn, ["ggn"])
                    state_f = sbt(st3, "state_f", [128, 4, 128], F32)
                    state_b = [sbt(st3, "state_b%d" % i, [128, 4, 128], BF16) for i in range(2)]
                    hTb = [sbt(st3, "hTb%d" % i, [128, 8, 128], BF16) for i in range(2)]
                    cs = [sbt(st3, "cs%d" % i, [128, 2, 64], F32) for i in range(2)]
                    q_sb = sbt(st3, "q_sb", [128, 512], F32)
                    k_sb = sbt(st3, "k_sb", [128, 512], F32)
                    eg = sbt(st3, "eg", [128, 512], F32)
                    q_r = sbt(st3, "q_r", [128, 4, 2, 64], BF16)
                    k_r = sbt(st3, "k_r", [128, 4, 2, 64], BF16)
                    kd = sbt(st3, "kd", [128, 4, 128], BF16)
                    v_bf = sbt(st3, "v_bf", [128, 512], BF16)
                    g_sil2 = [sbt(st3, "g_sil%d" % i, [128, 512], BF16) for i in range(2)]
                    qkT = sbt(st3, "qkT", [128, 8, 128], BF16)
                    scT = sbt(st3, "scT", [128, 4, 128], BF16)
                    o_s = sbt(st3, "o_s", [128, 4, 128], F32)
                    ret_b = sbt(st3, "ret_b", [128, 512], BF16)
                    retT_s = [sbt(st3, "retT_s%d" % i, [128, 4, 128], BF16) for i in range(2)]
                    tA = [sbt(st3, "tA%d" % i, [128, 4, 64], F32) for i in range(2)]
                    tB = [sbt(st3, "tB%d" % i, [128, 4, 64], F32) for i in range(2)]
                    bnst = sbt(st3, "bnst", [128, 4, 6], F32)
                    mv = sbt(st3, "mv", [128, 4, 2], F32)
                    rsd = sbt(st3, "rsd", [128, 4], F32)
                    sc.op("pool", lambda e: e.memset(state_f[:], 0.0), w=["state_f"])
                    BA, BB, BC = 5, 6, 7

                    def rope(src, dst, b, ksrc, kdst):
                        pv = src[:].rearrange("p (h two d) -> p h two d", h=4, two=2)
                        t1 = pv[:, :, 0, :]
                        t2 = pv[:, :, 1, :]
                        cb = cs[b % 2][:, 0, :].unsqueeze(1).broadcast_to([128, 4, 64])
                        sb_ = cs[b % 2][:, 1, :].unsqueeze(1).broadcast_to([128, 4, 64])
                        for half in range(2):
                            a0, a1 = (cb, sb_) if half == 0 else (sb_, cb)
                            op = ALU.subtract if half == 0 else ALU.add
                            sc.op("dve", lambda e, a0=a0, half=half: e.tensor_tensor(out=tA[half][:], in0=t1, in1=a0, op=ALU.mult),
                                  r=[ksrc, ("cs", b % 2)], w=[("tA", half)])
                            sc.op("dve", lambda e, a1=a1, half=half: e.tensor_tensor(out=tB[half][:], in0=t2, in1=a1, op=ALU.mult),
                                  r=[ksrc, ("cs", b % 2)], w=[("tB", half)])
                            sc.op("pool", lambda e, op=op, half=half: e.tensor_tensor(out=dst[:, :, half, :], in0=tA[half][:],
                                                                                     in1=tB[half][:], op=op),
                                  r=[("tA", half), ("tB", half)], w=[kdst])

                    def proj(b, wi, bank, half):
                        for k in range(half * 4, half * 4 + 4):
                            sc.op("pe", lambda e, k=k: e.matmul(
                                ps[bank][:], lhsT=hTb[b % 2][:, k, :], rhs=wr[:, k, wi * 512:(wi + 1) * 512],
                                start=(k == 0), stop=(k == 7)), r=WR + [("hTb", b % 2)], w=[("ps", bank)])

                    def m0(b):
                        ld("sp", hTb[b % 2][:], hT_d[:, :, b * 128:(b + 1) * 128], [("hTb", b % 2)], r=[("hT_d", b // 4)])
                        ld("sp", cs[b % 2][:, 0, :], cos_d[:, b, :], [("cs", b % 2)])
                        ld("sp", cs[b % 2][:, 1, :], sin_d[:, b, :], [("cs", b % 2)])

                    def pq_a(b):
                        proj(b, 0, BA, 0)

                    def pq_b(b):
                        proj(b, 0, BA, 1)

                    def pk_a(b):
                        sc.op("dve", lambda e: e.tensor_copy(out=q_sb[:], in_=ps[BA][:]), r=[("ps", BA)], w=["q_sb"])
                        proj(b, 1, BB, 0)

                    def pk_b(b):
                        proj(b, 1, BB, 1)
                        m6a(b)

                    def pv_a(b):
                        sc.op("dve", lambda e: e.tensor_copy(out=k_sb[:], in_=ps[BB][:]), r=[("ps", BB)], w=["k_sb"])
                        proj(b, 2, BA, 0)

                    def pv_b(b):
                        proj(b, 2, BA, 1)
                        m6b(b)

                    def pg_a(b):
                        sc.op("dve", lambda e: e.tensor_copy(out=v_bf[:], in_=ps[BA][:]), r=[("ps", BA)], w=["v_bf"])
                        proj(b, 3, BB, 0)

                    def pg_b(b):
                        proj(b, 3, BB, 1)
                        m6c(b)

                    def m5(b):
                        sc.op("act", lambda e: e.activation(out=eg[:], in_=ps[BB][:], func=AF.Exp, scale=-1.0), r=[("ps", BB)], w=["eg"])
                        sc.op("dve", lambda e: e.tensor_scalar(out=eg[:], in0=eg[:], scalar1=1.0, scalar2=None, op0=ALU.add), r=["eg"], w=["eg"])
                        sc.op("dve", lambda e: e.reciprocal(out=eg[:], in_=eg[:]), r=["eg"], w=["eg"])
                        sc.op("dve", lambda e: e.tensor_tensor(out=g_sil2[b % 2][:], in0=ps[BB][:], in1=eg[:], op=ALU.mult),
                              r=[("ps", BB), "eg"], w=[("g_sil", b % 2)])

                    def rope_half(src, dst, b, ksrc, kdst, half):
                        pv = src[:].rearrange("p (h two d) -> p h two d", h=4, two=2)
                        t1 = pv[:, :, 0, :]
                        t2 = pv[:, :, 1, :]
                        cb = cs[b % 2][:, 0, :].unsqueeze(1).broadcast_to([128, 4, 64])
                        sb_ = cs[b % 2][:, 1, :].unsqueeze(1).broadcast_to([128, 4, 64])
                        a0, a1 = (cb, sb_) if half == 0 else (sb_, cb)
                        op = ALU.subtract if half == 0 else ALU.add
                        sc.op("dve", lambda e: e.tensor_tensor(out=tA[half][:], in0=t1, in1=a0, op=ALU.mult),
                              r=[ksrc, ("cs", b % 2)], w=[("tA", half)])
                        sc.op("dve", lambda e: e.tensor_tensor(out=tB[half][:], in0=t2, in1=a1, op=ALU.mult),
                              r=[ksrc, ("cs", b % 2)], w=[("tB", half)])
                        sc.op("dve", lambda e: e.tensor_tensor(out=dst[:, :, half, :], in0=tA[half][:], in1=tB[half][:], op=op),
                              r=[("tA", half), ("tB", half)], w=[kdst])

                    def m6a(b):
                        rope_half(q_sb, q_r, b, "q_sb", "q_r", 0)

                    def m6b(b):
                        rope_half(q_sb, q_r, b, "q_sb", "q_r", 1)

                    def m6c(b):
                        rope_half(k_sb, k_r, b, "k_sb", "k_r", 0)

                    def m6d(b):
                        rope_half(k_sb, k_r, b, "k_sb", "k_r", 1)
                        sc.op("dve", lambda e: e.tensor_tensor(out=kd[:], in0=k_r[:].rearrange("p h two d -> p h (two d)"),
                                                                in1=kdec_s[:], op=ALU.mult),
                              r=["k_r", "kdec"], w=["kd"])

                    def m7(b):
                        pb = ps[BC].bitcast(BF16)
                        for hh in range(4):
                            sc.op("pe", lambda e, hh=hh, pb=pb: e.transpose(
                                out=pb[:, hh * 128:(hh + 1) * 128], in_=q_r[:, hh].rearrange("p two d -> p (two d)"),
                                identity=ident[:]), r=["q_r", "ident"], w=[("ps", BC)])
                        for hh in range(4):
                            sc.op("pe", lambda e, hh=hh, pb=pb: e.transpose(
                                out=pb[:, (4 + hh) * 128:(5 + hh) * 128], in_=k_r[:, hh].rearrange("p two d -> p (two d)"),
                                identity=ident[:]), r=["k_r", "ident"], w=[("ps", BC)])

                    def m8(b):
                        pb = ps[BC].bitcast(BF16)
                        sc.op("dve", lambda e: e.tensor_copy(out=qkT[:], in_=pb.rearrange("p (k t) -> p k t", k=8)), r=[("ps", BC)], w=["qkT"])

                    def m9(b):
                        for hh in range(4):
                            sc.op("pe", lambda e, hh=hh: e.matmul(
                                ps[BA][:, hh * 128:(hh + 1) * 128], lhsT=qkT[:, 4 + hh, :], rhs=qkT[:, hh, :],
                                start=True, stop=True), r=["qkT"], w=[("ps", BA)])

                    def m10(b):
                        sc.op("dve", lambda e: e.tensor_tensor(out=scT[:], in0=ps[BA][:].rearrange("p (h i) -> p h i", h=4),
                                                               in1=Dp_s[:], op=ALU.mult),
                              r=[("ps", BA), "Dp"], w=["scT"])

                    def m11(b):
                        sbi = b % 2
                        for hh in range(4):
                            sc.op("pe", lambda e, hh=hh: e.matmul(
                                ps[BB][:, hh * 128:(hh + 1) * 128], lhsT=scT[:, hh, :], rhs=v_bf[:, hh * 128:(hh + 1) * 128],
                                start=True, stop=(b == 0)), r=["scT", "v_bf"], w=[("ps", BB)])
                            if b > 0:
                                sc.op("pe", lambda e, hh=hh: e.matmul(
                                    ps[BB][:, hh * 128:(hh + 1) * 128], lhsT=qkT[:, hh, :], rhs=state_b[sbi][:, hh, :],
                                    start=False, stop=True), r=["qkT", ("state_b", sbi)], w=[("ps", BB)])
                        if b < NB - 1:
                            for hh in range(4):
                                sc.op("pe", lambda e, hh=hh: e.matmul(
                                    ps[BC][:, hh * 128:(hh + 1) * 128], lhsT=kd[:, hh, :], rhs=v_bf[:, hh * 128:(hh + 1) * 128],
                                    start=True, stop=True), r=["kd", "v_bf"], w=[("ps", BC)])

                    def m12(b):
                        if b < NB - 1:
                            for hh in range(4):
                                sc.op("dve", lambda e, hh=hh: e.scalar_tensor_tensor(
                                    out=state_f[:, hh, :], in0=state_f[:, hh, :], scalar=cdec[hh], in1=ps[BC][:, hh * 128:(hh + 1) * 128],
                                    op0=ALU.mult, op1=ALU.add), r=["state_f", ("ps", BC)], w=["state_f"])
                            nsb = (b + 1) % 2
                            sc.op("pool", lambda e: e.tensor_copy(out=state_b[nsb][:], in_=state_f[:]),
                                  r=["state_f"], w=[("state_b", nsb)])
                        sc.op("dve", lambda e: e.tensor_tensor(out=o_s[:], in0=ps[BB][:].rearrange("p (h e) -> p h e", h=4),
                                                               in1=qdec_s[:], op=ALU.mult),
                              r=[("ps", BB), "qdec"], w=["o_s"])

                    def m13(b):
                        for hh in range(4):
                            sc.op("dve", lambda e, hh=hh: e.bn_stats(out=bnst[:, hh, :], in_=o_s[:, hh, :]), r=["o_s"], w=["bnst"])
                        for hh in range(4):
                            sc.op("dve", lambda e, hh=hh: e.bn_aggr(out=mv[:, hh, :], in_=bnst[:, hh, :]), r=["bnst"], w=["mv"])
                        sc.op("dve", lambda e: e.tensor_scalar(out=rsd[:], in0=mv[:, :, 1], scalar1=EPS, scalar2=None, op0=ALU.add),
                              r=["mv"], w=["rsd"])
                        pow_ops(rsd[:], "rsd")

                    def m14(b):
                        for hh in range(4):
                            sc.op("dve", lambda e, hh=hh: e.tensor_scalar(
                                out=o_s[:, hh, :], in0=o_s[:, hh, :], scalar1=mv[:, hh, 0:1], scalar2=rsd[:, hh:hh + 1],
                                op0=ALU.subtract, op1=ALU.mult), r=["o_s", "mv", "rsd"], w=["o_s"])
                        sc.op("pool", lambda e: e.tensor_tensor(out=o_s[:], in0=o_s[:], in1=ggn_s[:].rearrange("p (h e) -> p h e", h=4), op=ALU.mult),
                              r=["o_s", "ggn"], w=["o_s"])
                        sc.op("pool", lambda e: e.tensor_tensor(out=ret_b[:], in0=o_s[:].rearrange("p h e -> p (h e)"), in1=g_sil2[b % 2][:], op=ALU.mult),
                              r=["o_s", ("g_sil", b % 2)], w=["ret_b"])

                    def m15(b):
                        pb6 = ps[BC].bitcast(BF16)
                        for hh in range(4):
                            sc.op("pe", lambda e, hh=hh, pb6=pb6: e.transpose(
                                out=pb6[:, hh * 128:(hh + 1) * 128], in_=ret_b[:, hh * 128:(hh + 1) * 128], identity=ident[:]),
                                r=["ret_b", "ident"], w=[("ps", BC)])

                    def m16(b):
                        pb6 = ps[BC].bitcast(BF16)
                        sc.op("dve", lambda e: e.tensor_copy(out=retT_s[b % 2][:], in_=pb6[:, 0:512].rearrange("p (k t) -> p k t", k=4)),
                              r=[("ps", BC)], w=[("retT_s", b % 2)])
                        ld("sp", retT_d[:, :, b * 128:(b + 1) * 128], retT_s[b % 2][:], [("retT_d", b // 4)], r=[("retT_s", b % 2)])

                    msched = [(m0, 0), (pq_a, 1), (pq_b, 2), (pk_a, 3), (pk_b, 4), (pv_a, 5), (pv_b, 6), (pg_a, 7), (pg_b, 8),
                              (m6d, 9), (m5, 10), (m7, 13), (m8, 14), (m9, 15), (m10, 16), (m11, 17), (m12, 18), (m13, 19),
                              (m14, 21), (m15, 25), (m16, 26)]
                    RPER = 18
                    rsteps = {}
                    for b_ in range(NB):
                        for fn_, off_ in msched:
                            rsteps.setdefault(b_ * RPER + off_, []).append((b_, fn_))

                    sbT_v = sbT_d.rearrange("(pr two) d s -> (two d) pr s", two=2)
                    tiles = []
                    for g in range(NG):
                        for j in range(4):
                            nkb = 4 * (g + 1)
                            for idx, kb in enumerate(range(nkb - 1, -1, -1)):
                                tiles.append(dict(g=g, j=j, kb=kb, idx=idx, last=(idx == nkb - 1), gj=g * 4 + j))
                    NT = len(tiles)
                    for i, t in enumerate(tiles):
                        t["i"] = i
                    OB = 4

                    def c0_of(t):
                        jd = t["kb"] - 4 * t["g"]
                        return jd * 128 if jd >= 1 else 0

                    def st_q(g):
                        ld("sp", qg[g % 2][:], qT_d[:, :, g * 512:(g + 1) * 512], [("qg", g % 2)], r=[("qT_d", g)])

                    def st_z(t):
                        i = t["i"]; g = t["g"]; j = t["j"]; kb = t["kb"]
                        c0 = c0_of(t)
                        jd = kb - 4 * g
                        for hh in range(2):
                            po = hh * 64
                            sc.op("pe", lambda e, hh=hh, po=po: e.matmul(
                                psr[0][:, hh * 512 + c0:(hh + 1) * 512], lhsT=kT[po:po + 64, j, kb * 128:(kb + 1) * 128],
                                rhs=qg[g % 2][po:po + 64, j, c0:512], start=True, stop=(jd < 0)),
                                r=[("kT", kb // 4), ("qg", g % 2)], w=["zr"])
                        if jd >= 0:
                            for hh in range(2):
                                sc.op("pe", lambda e, hh=hh: e.matmul(
                                    psr[0][:, hh * 512 + c0:(hh + 1) * 512], lhsT=ident[:], rhs=mask01[:, jd, c0:512],
                                    start=False, stop=True), r=["ident", "mask01"], w=["zr"])
                        eb = i % NE
                        sc.op("act", lambda e: e.activation(out=e_bf[eb][:, :, c0:], in_=psr[0][:].rearrange("p (h q) -> p h q", h=2)[:, :, c0:], func=AF.Exp),
                              r=["zr"], w=[("e", eb)])

                    def st_ln(t):
                        i = t["i"]
                        eb = i % NE
                        c0 = c0_of(t)
                        sc.op("act", lambda e: e.activation(out=sp_bf[eb][:, :, c0:], in_=e_bf[eb][:, :, c0:], func=AF.Ln, bias=1.0),
                              r=[("e", eb)], w=[("sp", eb)] + ([("splo", eb)] if c0 == 0 else []))
                        if c0 > 0:
                            sc.op("pool", lambda e: e.memset(sp_bf[eb][:, :, 0:c0], 0.0), r=[("sp", eb)], w=[("splo", eb)])
                        st_sadd(t)

                    def st_arg(t):
                        i = t["i"]; idx = t["idx"]
                        eb = i % NE
                        c0 = c0_of(t)
                        srcS_extra = []
                        if idx == 0:
                            srcS = None
                        elif idx == 1:
                            srcS = (sp_bf[(i - 1) % NE], ("sp", (i - 1) % NE))
                            srcS_extra = [("splo", (i - 1) % NE)]
                        else:
                            srcS = (S_bf[idx % 3], ("S", idx % 3))
                        for hh in range(2):
                            sc.op("pe", lambda e, hh=hh: e.matmul(psr[1][:, hh * 512 + c0:(hh + 1) * 512], lhsT=ntri[:], rhs=sp_bf[eb][:, hh, c0:],
                                                                  start=True, stop=(srcS is None)),
                                  r=[("sp", eb), "ntri"] + ([("splo", eb)] if c0 == 0 else []), w=["argr"])
                            if srcS is not None:
                                sc.op("pe", lambda e, hh=hh: e.matmul(psr[1][:, hh * 512 + c0:(hh + 1) * 512], lhsT=nones[:], rhs=srcS[0][:, hh, c0:],
                                                                      start=False, stop=True),
                                      r=[srcS[1], "nones"] + srcS_extra, w=["argr"])
                        sc.op("act", lambda e: e.activation(out=E_bf[i % 2][:, :, c0:], in_=psr[1][:].rearrange("p (h q) -> p h q", h=2)[:, :, c0:], func=AF.Exp),
                              r=["argr"], w=[("E", i % 2)])

                    def st_dve(t):
                        i = t["i"]; idx = t["idx"]
                        eb = i % NE
                        c0 = c0_of(t)
                        sc.op("dve", lambda e: e.tensor_tensor(out=a_bf[i % NA][:, :, c0:], in0=e_bf[eb][:, :, c0:], in1=E_bf[i % 2][:, :, c0:], op=ALU.mult),
                              r=[("e", eb), ("E", i % 2)], w=[("a", i % NA)] + ([("alo", i % NA)] if c0 == 0 else []))
                        if c0 > 0:
                            sc.op("pool", lambda e: e.memset(a_bf[i % NA][:, :, 0:c0], 0.0), r=[("a", i % NA)], w=[("alo", i % NA)])

                    def st_sadd(t):
                        i = t["i"]; idx = t["idx"]
                        eb = i % NE
                        if not t["last"] and idx >= 1:
                            nxt = (idx + 1) % 3
                            if idx == 1:
                                pe_ = (i - 1) % NE
                                sc.op("dve", lambda e: e.tensor_tensor(out=S_bf[nxt][:], in0=sp_bf[pe_][:], in1=sp_bf[eb][:], op=ALU.add),
                                      r=[("sp", pe_), ("sp", eb), ("splo", pe_), ("splo", eb)], w=[("S", nxt)])
                            else:
                                cur = idx % 3
                                sc.op("dve", lambda e: e.tensor_tensor(out=S_bf[nxt][:], in0=S_bf[cur][:], in1=sp_bf[eb][:], op=ALU.add),
                                      r=[("S", cur), ("sp", eb), ("splo", eb)], w=[("S", nxt)])

                    def st_o(t):
                        i = t["i"]; g = t["g"]; j = t["j"]; kb = t["kb"]; idx = t["idx"]
                        for hh in range(2):
                            h = 2 * j + hh
                            sc.op("pe", lambda e, hh=hh, h=h: e.matmul(ps[OB][hh * 64:(hh + 1) * 64, :], lhsT=vv[:, kb, h * 64:(h + 1) * 64],
                                                                       rhs=a_bf[i % NA][:, hh, :], start=(idx == 0), stop=t["last"]),
                                  r=[("vv", kb), ("a", i % NA), ("alo", i % NA)], w=[("ps", OB)])
                        if t["last"]:
                            oi = t["gj"] % 2
                            sc.op("dve", lambda e: e.tensor_copy(out=o_sb[oi][:], in_=ps[OB][:]),
                                  r=[("ps", OB)], w=[("o_sb", oi)])
                            ld("sp", sbT_v[:, j, g * 512:(g + 1) * 512], o_sb[oi][:], [("sbT_d", g)], r=[("o_sb", oi)])

                    xe_z = xe_d.rearrange("(n p) f -> n p f", p=128)
                    nzf = xe_z.shape[0]
                    zf_every = max(1, NT // nzf)
                    zf_done = [0]
                    st_q(0)
                    OLAG = 4
                    for step in range(NT + OLAG):
                        if step % zf_every == 0 and zf_done[0] < nzf:
                            ld("sp", xe_z[zf_done[0]], zt[:], ["xe_d"], r=["zt"])
                            zf_done[0] += 1
                        if step < NT and tiles[step]["idx"] == 0 and tiles[step]["j"] == 0 and tiles[step]["g"] + 1 < NG:
                            st_q(tiles[step]["g"] + 1)
                        diag = step < NT and (tiles[step]["kb"] - 4 * tiles[step]["g"] >= 0)
                        if step < NT:
                            st_z(tiles[step])
                            if not diag:
                                st_ln(tiles[step])
                        if 0 <= step - 1 < NT:
                            st_arg(tiles[step - 1])
                        if step < NT and diag:
                            st_ln(tiles[step])
                        if 0 <= step - 1 < NT:
                            st_dve(tiles[step - 1])
                        if 0 <= step - OLAG < NT:
                            st_o(tiles[step - OLAG])
                        for b_, fn_ in rsteps.pop(step, []):
                            fn_(b_)
                    while zf_done[0] < nzf:
                        ld("sp", xe_z[zf_done[0]], zt[:], ["xe_d"], r=["zt"])
                        zf_done[0] += 1
                    for step in sorted(rsteps):
                        for b_, fn_ in rsteps[step]:
                            fn_(b_)
            sc.barrier()

            with ExitStack() as stR:
                dest_i = sbt(stR, "dest_i", [128, NB, 2], I32)
                wts = sbt(stR, "wts", [128, NB, 2], F32)
                with ExitStack() as st:
                    wg = sbt(st, "wg", [128, 8, 2048], BF16)
                    w_v = w_in.rearrange("(k p) c -> p k c", p=128)
                    wbr = sbt(st, "wbr", [128, 4, D], BF16)
                    wbs = sbt(st, "wbs", [128, 4, D], BF16)
                    wo = sbt(st, "wo", [128, 8, D], BF16)
                    ld("pool", wbr[:], w_br_ret.rearrange("(k p) c -> p k c", p=128), ["wbr"])
                    for k in range(8):
                        ld("pool", wg[:, k, 0:1024], w_v[:, k, 3584:4608], [("wg0", k)])
                    ld("pool", wbs[:], w_br_sb.rearrange("(k p) c -> p k c", p=128), ["wbs"])
                    for k in range(8):
                        ld("pool", wg[:, k, 1024:2048], w_v[:, k, 4608:5632], [("wg1", k)])
                    ld("pool", wo[:], w_out.rearrange("(k p) c -> p k c", p=128), ["wo"])
                    WG = [[("wg0", k) for k in range(8)], [("wg1", k) for k in range(8)]]
                    wrt = sbt(st, "wrt", [128, 8, 20], F32)
                    ld("sp", wrt[:], w_rt.rearrange("(k p) c -> p k c", p=128), ["wrt"])
                    brt = sbt(st, "brt", [128, 20], F32)
                    ld("sp", brt[:], b_rt, ["brt"])
                    identf = sbt(st, "identf", [128, 128], F32)
                    ld("sp", identf[:], identf_d, ["identf"])
                    trilt = sbt(st, "trilt", [128, 128], BF16)
                    onesb = sbt(st, "onesb", [128, 128], BF16)
                    ld("sp", trilt[:], trilt_d, ["trilt"])
                    ld("sp", onesb[:], onesbf_d, ["onesb"])
                    ebase = sbt(st, "ebase_s", [128, 16], F32)
                    ld("sp", ebase[:], ebase_d, ["ebase"])
                    gffn_s = sbt(st, "gffn_s", [128, D], F32)
                    ld("sp", gffn_s[:], gffn, ["gffn"])
                    hTt = [sbt(st, "hTt%d" % i, [128, 8, 512], BF16) for i in range(2)]
                    retT_t = [sbt(st, "retT_t%d" % i, [128, 4, 512], BF16) for i in range(2)]
                    sbT_t = [sbt(st, "sbT_t%d" % i, [128, 4, 512], BF16) for i in range(2)]
                    mT = [sbt(st, "mT%d" % i, [128, 8, 512], BF16) for i in range(2)]
                    sg = [sbt(st, "sg%d" % i, [128, 512], F32) for i in range(2)]
                    m1 = [sbt(st, "m1_%d" % i, [128, 512], F32) for i in range(2)]
                    x1s = [sbt(st, "x1s%d" % i, [128, D], F32) for i in range(3)]
                    hnf2 = [sbt(st, "hnf%d" % i, [128, D], F32) for i in range(2)]
                    hnb = [sbt(st, "hnb%d" % i, [128, D], BF16) for i in range(2)]
                    hnT = sbt(st, "hnT", [128, 8, 128], F32)
                    ssq4 = sbt(st, "ssq4", [128, NB], F32)
                    rs4 = sbt(st, "rs4", [128, NB], F32)
                    lg = sbt(st, "lg", [128, 20], F32)
                    sm = sbt(st, "sm", [128, 16], F32)
                    ohg = sbt(st, "ohg", [128, 4], F32)
                    tmp16 = sbt(st, "tmp16", [128, 4, 4], F32)
                    ig = sbt(st, "ig", [128, 4], F32)
                    ig2 = sbt(st, "ig2", [128, 4], F32)
                    oh1 = sbt(st, "oh1", [128, 4], F32)
                    oh2 = sbt(st, "oh2", [128, 4], F32)
                    ohe1 = sbt(st, "ohe1", [128, 4, 4], F32)
                    ohe2 = sbt(st, "ohe2", [128, 4, 4], F32)
                    A_bf = sbt(st, "A_bf", [128, 16], BF16)
                    Acum = [sbt(st, "Acum%d" % i, [128, 16], BF16) for i in range(2)]
                    rk = sbt(st, "rk", [128, 16], F32)
                    destf = sbt(st, "destf", [128, 2], F32)
                    sc.op("pool", lambda e: e.memset(Acum[0][:], 0.0), w=[("Acum", 0)])
                    sbT_v = sbT_d.rearrange("(pr two) d s -> (two d) pr s", two=2)
                    pcnt = [0]

                    def p4_load(T):
                        t2 = T % 2
                        ld("sp", hTt[t2][:], hT_d[:, :, T * 512:(T + 1) * 512], [("hTt", t2)], r=[("hT_d", T)])
                        ld("sp", retT_t[t2][:], retT_d[:, :, T * 512:(T + 1) * 512], [("retT_t", t2)], r=[("retT_d", T)])
                        for pr in range(4):
                            ld("sp", sbT_t[t2][:, pr, :], sbT_v[:, pr, T * 512:(T + 1) * 512], [("sbT_t", t2)], r=[("sbT_d", T)])

                    def p4_c(T, c):
                        t2 = T % 2
                        HT = [("hTt", t2)]
                        for br in range(2):
                            wb_, src, ksrc, kw = (wbr, retT_t[t2], ("retT_t", t2), "wbr") if br == 0 else (wbs, sbT_t[t2], ("sbT_t", t2), "wbs")
                            pp = pcnt[0] % 3
                            pcnt[0] += 1
                            bb = 2 * pp
                            gb = 2 * pp + 1
                            for k in range(4):
                                sc.op("pe", lambda e, k=k, wb_=wb_, src=src, bb=bb: e.matmul(
                                    ps[bb][:], lhsT=wb_[:, k, c * 128:(c + 1) * 128], rhs=src[:, k, :],
                                    start=(k == 0), stop=(k == 3)), r=[kw, ksrc], w=[("ps", bb)])
                            for k in range(8):
                                sc.op("pe", lambda e, k=k, br=br, gb=gb: e.matmul(
                                    ps[gb][:], lhsT=wg[:, k, br * 1024 + c * 128: br * 1024 + (c + 1) * 128],
                                    rhs=hTt[t2][:, k, :], start=(k == 0), stop=(k == 7)),
                                    r=WG[br] + HT, w=[("ps", gb)])
                            sc.op("act", lambda e, br=br, gb=gb: e.activation(out=sg[br][:], in_=ps[gb][:], func=AF.Sigmoid),
                                  r=[("ps", gb)], w=[("sg", br)])
                            sc.op("dve", lambda e, br=br, bb=bb: e.tensor_tensor(out=m1[br][:], in0=ps[bb][:], in1=sg[br][:], op=ALU.mult),
                                  r=[("ps", bb), ("sg", br)], w=[("m1", br)])
                        sc.op("pool", lambda e: e.tensor_tensor(out=mT[t2][:, c, :], in0=m1[0][:], in1=m1[1][:], op=ALU.add),
                              r=[("m1", 0), ("m1", 1)], w=[("mT", t2)])

                    def p4_blk(T, i, part):
                        R = sc.op
                        if True:
                            t2 = T % 2
                            b = T * 4 + i
                            i2 = b % 3
                            if part == "A":
                                ld("sp", x1s[i2][:], x[b * 128:(b + 1) * 128, :], [("x1s", i2)])
                                for hf in range(2):
                                    bank = 6 + hf
                                    for c in range(8):
                                        sc.op("pe", lambda e, c=c, hf=hf, bank=bank: e.matmul(
                                            ps[bank][:], lhsT=mT[t2][:, c, i * 128:(i + 1) * 128], rhs=wo[:, c, hf * 512:(hf + 1) * 512],
                                            start=(c == 0), stop=(c == 7)), r=[("mT", t2), "wo"], w=[("ps", bank)])
                                    sc.op("dve", lambda e, hf=hf, bank=bank: e.tensor_tensor(
                                        out=x1s[i2][:, hf * 512:(hf + 1) * 512], in0=ps[bank][:], in1=x1s[i2][:, hf * 512:(hf + 1) * 512], op=ALU.add),
                                        r=[("ps", bank), ("x1s", i2)], w=[("x1s", i2)])
                                ld("sp", x1_d[b * 128:(b + 1) * 128, :], x1s[i2][:], [("x1_d", b)], r=[("x1s", i2)])
                                sc.op("act", lambda e, b=b, i2=i2: e.activation(out=junk[:], in_=x1s[i2][:], func=AF.Square,
                                                                               accum_out=ssq4[:, b:b + 1]),
                                      r=[("x1s", i2)], w=["junk", ("ssq4", b)])
                                rstd_ops(ssq4[:, b:b + 1], rs4[:, b:b + 1], ("ssq4", b), ("rs4", b))
                                sc.op("dve", lambda e, b=b, i2=i2: e.scalar_tensor_tensor(
                                    out=hnf2[b % 2][:], in0=x1s[i2][:], scalar=rs4[:, b:b + 1], in1=gffn_s[:], op0=ALU.mult, op1=ALU.mult),
                                    r=[("x1s", i2), ("rs4", b), "gffn"], w=[("hnf", b % 2)])
                                sc.op("act", lambda e, b=b: e.copy(out=hnb[b % 2][:], in_=hnf2[b % 2][:]), r=[("hnf", b % 2)], w=[("hnb", b % 2)])
                            if part == "B":
                                for half in range(2):
                                    bank = 6 + half
                                    for kk in range(4):
                                        k = half * 4 + kk
                                        sc.op("pe", lambda e, k=k, kk=kk, bank=bank: e.transpose(
                                            out=ps[bank][:, kk * 128:(kk + 1) * 128], in_=hnf2[b % 2][:, k * 128:(k + 1) * 128], identity=identf[:]),
                                            r=[("hnf", b % 2), "identf"], w=[("ps", bank)])
                                    evac(hnT[:, half * 4:(half + 1) * 4, :], ps[bank][:].rearrange("p (k t) -> p k t", k=4),
                                         r=[("ps", bank)], w=["hnT"])
                            if part == "C":
                                for k in range(8):
                                    sc.op("pe", lambda e, k=k: e.matmul(ps[6][:, 0:20], lhsT=hnT[:, k, :], rhs=wrt[:, k, :],
                                                                        start=(k == 0), stop=(k == 7)),
                                          r=["hnT", "wrt"], w=[("ps", 6)])
                                R("dve", lambda e: e.tensor_tensor(out=lg[:], in0=ps[6][:, 0:20], in1=brt[:], op=ALU.add),
                                  r=[("ps", 6), "brt"], w=["lg"])
                                R("dve", lambda e: e.tensor_reduce(out=sm[:, 0:1], in_=lg[:, 0:4], axis=AX.X, op=ALU.max), r=["lg"], w=["sm"])
                                R("dve", lambda e: e.tensor_scalar(out=ohg[:], in0=lg[:, 0:4], scalar1=sm[:, 0:1], scalar2=None, op0=ALU.is_equal),
                                  r=["lg", "sm"], w=["ohg"])
                                R("dve", lambda e: e.tensor_scalar(out=ig2[:], in0=lg[:, 0:4], scalar1=sm[:, 0:1], scalar2=None, op0=ALU.subtract),
                                  r=["lg", "sm"], w=["ig2"])
                                R("act", lambda e: e.activation(out=ig2[:], in_=ig2[:], func=AF.Sigmoid), r=["ig2"], w=["ig2"])
                                R("dve", lambda e: e.tensor_scalar(out=oh2[:], in0=ig2[:], scalar1=-1.0, scalar2=1.0, op0=ALU.mult, op1=ALU.add),
                                  r=["ig2"], w=["oh2"])
                                R("dve", lambda e: e.reciprocal(out=oh2[:], in_=oh2[:]), r=["oh2"], w=["oh2"])
                                R("dve", lambda e: e.tensor_tensor(out=ig2[:], in0=ig2[:], in1=oh2[:], op=ALU.mult), r=["ig2", "oh2"], w=["ig2"])
                                R("dve", lambda e: e.tensor_reduce(out=sm[:, 2:3], in_=ig2[:], axis=AX.X, op=ALU.add), r=["ig2"], w=["sm"])
                                R("dve", lambda e: e.reciprocal(out=sm[:, 3:4], in_=sm[:, 2:3]), r=["sm"], w=["sm"])
                                R("dve", lambda e: e.tensor_tensor(out=tmp16[:], in0=lg[:, 4:20].rearrange("p (g j) -> p g j", g=4),
                                                                   in1=ohg[:].unsqueeze(2).broadcast_to([128, 4, 4]), op=ALU.mult),
                                  r=["lg", "ohg"], w=["tmp16"])
                                R("dve", lambda e: e.tensor_reduce(out=ig[:], in_=tmp16[:].rearrange("p g j -> p j g"), axis=AX.X, op=ALU.add),
                                  r=["tmp16"], w=["ig"])
                                R("dve", lambda e: e.tensor_reduce(out=sm[:, 4:5], in_=ig[:], axis=AX.X, op=ALU.max), r=["ig"], w=["sm"])
                                R("dve", lambda e: e.tensor_scalar(out=oh1[:], in0=ig[:], scalar1=sm[:, 4:5], scalar2=None, op0=ALU.is_equal),
                                  r=["ig", "sm"], w=["oh1"])
                                R("dve", lambda e: e.scalar_tensor_tensor(out=ig2[:], in0=oh1[:], scalar=-1e30, in1=ig[:], op0=ALU.mult, op1=ALU.add),
                                  r=["oh1", "ig"], w=["ig2"])
                                R("dve", lambda e: e.tensor_reduce(out=sm[:, 5:6], in_=ig2[:], axis=AX.X, op=ALU.max), r=["ig2"], w=["sm"])
                                R("dve", lambda e: e.tensor_scalar(out=oh2[:], in0=ig2[:], scalar1=sm[:, 5:6], scalar2=None, op0=ALU.is_equal),
                                  r=["ig2", "sm"], w=["oh2"])
                                R("dve", lambda e: e.tensor_tensor(out=sm[:, 6:7], in0=sm[:, 4:5], in1=sm[:, 5:6], op=ALU.subtract), r=["sm"], w=["sm"])
                                R("act", lambda e: e.activation(out=sm[:, 7:8], in_=sm[:, 6:7], func=AF.Sigmoid), r=["sm"], w=["sm"])
                                R("dve", lambda e: e.tensor_scalar(out=sm[:, 8:9], in0=sm[:, 7:8], scalar1=-1.0, scalar2=1.0, op0=ALU.mult, op1=ALU.add),
                                  r=["sm"], w=["sm"])
                                R("dve", lambda e, b=b: e.tensor_tensor(out=wts[:, b, 0:1], in0=sm[:, 7:8], in1=sm[:, 3:4], op=ALU.mult),
                                  r=["sm"], w=[("wts", b)])
                                R("dve", lambda e, b=b: e.tensor_tensor(out=wts[:, b, 1:2], in0=sm[:, 8:9], in1=sm[:, 3:4], op=ALU.mult),
                                  r=["sm", ("wts", b)], w=[("wts", b)])
                                R("dve", lambda e: e.tensor_tensor(out=ohe1[:], in0=ohg[:].unsqueeze(2).broadcast_to([128, 4, 4]),
                                                                   in1=oh1[:].unsqueeze(1).broadcast_to([128, 4, 4]), op=ALU.mult),
                                  r=["ohg", "oh1"], w=["ohe1"])
                                R("dve", lambda e: e.tensor_tensor(out=ohe2[:], in0=ohg[:].unsqueeze(2).broadcast_to([128, 4, 4]),
                                                                   in1=oh2[:].unsqueeze(1).broadcast_to([128, 4, 4]), op=ALU.mult),
                                  r=["ohg", "oh2"], w=["ohe2"])
                                R("dve", lambda e: e.tensor_tensor(out=A_bf[:], in0=ohe1[:].rearrange("p g j -> p (g j)"),
                                                                   in1=ohe2[:].rearrange("p g j -> p (g j)"), op=ALU.add),
                                  r=["ohe1", "ohe2"], w=["A_bf"])
                            if part == "D":
                                ac = b % 2
                                R("pe", lambda e: e.matmul(ps[7][:, 0:16], lhsT=trilt[:], rhs=A_bf[:], start=True, stop=False),
                                  r=["trilt", "A_bf"], w=[("ps", 7)])
                                R("pe", lambda e, ac=ac: e.matmul(ps[7][:, 0:16], lhsT=onesb[:], rhs=Acum[ac][:], start=False, stop=True),
                                  r=["onesb", ("Acum", ac)], w=[("ps", 7)])
                                R("pool", lambda e, ac=ac: e.tensor_tensor(out=Acum[1 - ac][:], in0=Acum[ac][:], in1=A_bf[:], op=ALU.add),
                                  r=[("Acum", ac), "A_bf"], w=[("Acum", 1 - ac)])
                                R("dve", lambda e: e.tensor_tensor(out=rk[:], in0=ps[7][:, 0:16], in1=ebase[:], op=ALU.add),
                                  r=[("ps", 7), "ebase"], w=["rk"])
                                for kk, oh in enumerate((ohe1, ohe2)):
                                    R("dve", lambda e, oh=oh: e.tensor_tensor(out=tmp16[:].rearrange("p g j -> p (g j)"), in0=rk[:],
                                                                             in1=oh[:].rearrange("p g j -> p (g j)"), op=ALU.mult),
                                      r=["rk", "ohe1", "ohe2"], w=["tmp16"])
                                    R("dve", lambda e, kk=kk: e.tensor_reduce(out=destf[:, kk:kk + 1], in_=tmp16[:].rearrange("p g j -> p (g j)"),
                                                                             axis=AX.X, op=ALU.add), r=["tmp16"], w=["destf"])
                                R("dve", lambda e, b=b: e.tensor_copy(out=dest_i[:, b, :], in_=destf[:]), r=["destf"], w=[("dest", b)])
                                for kk in range(2):
                                    sc.dma("pool", lambda e, b=b, kk=kk, i2=i2: e.indirect_dma_start(
                                        out=xe_d[:, :], out_offset=bass.IndirectOffsetOnAxis(ap=dest_i[:, b, kk:kk + 1], axis=0),
                                        in_=hnb[b % 2][:], in_offset=None), r=[("dest", b), ("hnb", b % 2)], w=["xe_d"])

                    sched = {0: ["A0"], 1: ["B0", "A1"], 2: ["C0", "B1"], 3: ["D0", "C1", "A2"], 4: ["D1", "B2"],
                             5: ["C2", "A3"], 6: ["D2", "B3"], 7: ["C3"]}
                    for T in range(NG + 1):
                        if T < NG:
                            p4_load(T)
                        for c in range(8):
                            if T < NG:
                                p4_c(T, c)
                            if T >= 2 and c == 0:
                                p4_blk(T - 2, 3, "D")
                            if T >= 1:
                                for it in sched[c]:
                                    p4_blk(T - 1, int(it[1]), it[0])
                    p4_blk(NG - 1, 3, "D")
                    if debug:
                        ld("sp", rt_d[:, :, 0:2], wts[:], ["rt_d"], r=[("wts", b) for b in range(NB)])
                sc.barrier()

                wpg = sbt(stR, "wpg", [128, 8, D], BF16)
                wpl = sbt(stR, "wpl", [128, 2, D], BF16)
                gple_s = sbt(stR, "gple_s", [128, D], F32)
                gfin_s = sbt(stR, "gfin_s", [128, D], F32)
                with ExitStack() as st:
                    SLT = 384 if C % 384 == 0 else 512
                    NSB = SLT // 128
                    NS = C // SLT
                    wge = [sbt(st, "wge%d" % i, [128, 8, 512], BF16) for i in range(2)]
                    wue = [sbt(st, "wue%d" % i, [128, 8, 512], BF16) for i in range(2)]
                    wde = [sbt(st, "wde%d" % i, [128, 4, D], BF16) for i in range(2)]
                    xe_t = [sbt(st, "xe_t%d" % i, [128, NSB, D], BF16) for i in range(3)]
                    xeT = [sbt(st, "xeT%d" % i, [128, 8, SLT], BF16) for i in range(2)]
                    sgl = [sbt(st, "sgl%d" % i, [128, SLT], BF16) for i in range(2)]
                    hid = [sbt(st, "hid%d" % i, [128, 4, SLT], BF16) for i in range(2)]
                    y_sb = [sbt(st, "y_sb%d" % i, [128, D], F32) for i in range(3)]
                    tl = [(ex, s_) for ex in range(16) for s_ in range(NS)]
                    cnt6 = [0]

                    def e_s0(n):
                        ex, s_ = tl[n]
                        e2 = ex % 2
                        if n == 3:
                            ld("pool", wpg[:], w_pg.rearrange("(k p) c -> p k c", p=128), ["wpg"])
                            ld("pool", wpl[:], w_ple.rearrange("(k p) c -> p k c", p=128), ["wpl"])
                            ld("sp", gple_s[:], gple, ["gple"])
                            ld("sp", gfin_s[:], gfin, ["gfin"])
                        if s_ == 0:
                            ld("pool", wge[e2][:], w_gate[ex].rearrange("(k p) c -> p k c", p=128), [("wge", e2)])
                            ld("pool", wue[e2][:], w_up[ex].rearrange("(k p) c -> p k c", p=128), [("wue", e2)])
                            ld("pool", wde[e2][:], w_down[ex].rearrange("(k p) c -> p k c", p=128), [("wde", e2)])
                        r0 = ex * C + s_ * SLT
                        ld("sp", xe_t[n % 3][:], xe_d[r0:r0 + SLT, :].rearrange("(i p) f -> p i f", p=128), [("xe_t", n % 3)], r=["xe_d"])

                    def e_s1(n):
                        for i in range(NSB):
                            bank = i % 2
                            pb = ps[bank].bitcast(BF16)
                            for k in range(8):
                                sc.op("pe", lambda e, k=k, i=i, pb=pb: e.transpose(
                                    out=pb[:, k * 128:(k + 1) * 128], in_=xe_t[n % 3][:, i, k * 128:(k + 1) * 128], identity=ident[:]),
                                    r=[("xe_t", n % 3), "ident"], w=[("ps", bank)])
                            evac(xeT[n % 2][:, :, i * 128:(i + 1) * 128], pb.rearrange("p (k t) -> p k t", k=8), r=[("ps", bank)], w=[("xeT", n % 2)])

                    def e_s2(n):
                        ex, s_ = tl[n]
                        e2 = ex % 2
                        for c in range(4):
                            gb = 2 + (c % 2) * 2
                            ub = gb + 1
                            for k in range(8):
                                sc.op("pe", lambda e, k=k, c=c, gb=gb: e.matmul(
                                    ps[gb][:, 0:SLT], lhsT=wge[e2][:, k, c * 128:(c + 1) * 128], rhs=xeT[n % 2][:, k, :],
                                    start=(k == 0), stop=(k == 7)), r=[("wge", e2), ("xeT", n % 2)], w=[("ps", gb)])
                            for k in range(8):
                                sc.op("pe", lambda e, k=k, c=c, ub=ub: e.matmul(
                                    ps[ub][:, 0:SLT], lhsT=wue[e2][:, k, c * 128:(c + 1) * 128], rhs=xeT[n % 2][:, k, :],
                                    start=(k == 0), stop=(k == 7)), r=[("wue", e2), ("xeT", n % 2)], w=[("ps", ub)])
                            sc.op("act", lambda e, c=c, gb=gb: e.activation(out=sgl[c % 2][:], in_=ps[gb][:, 0:SLT], func=AF.Silu),
                                  r=[("ps", gb)], w=[("sgl", c % 2)])
                            sc.op("dve", lambda e, c=c, ub=ub: e.tensor_tensor(out=hid[n % 2][:, c, :], in0=ps[ub][:, 0:SLT], in1=sgl[c % 2][:], op=ALU.mult),
                                  r=[("ps", ub), ("sgl", c % 2)], w=[("hid", n % 2)])

                    def e_s3(n):
                        ex, s_ = tl[n]
                        e2 = ex % 2
                        r0 = ex * C + s_ * SLT
                        for i in range(NSB):
                            y2 = cnt6[0] % 3
                            cnt6[0] += 1
                            for hf in range(2):
                                bank = 6 + hf
                                for c in range(4):
                                    sc.op("pe", lambda e, c=c, i=i, hf=hf, bank=bank: e.matmul(
                                        ps[bank][:], lhsT=hid[n % 2][:, c, i * 128:(i + 1) * 128], rhs=wde[e2][:, c, hf * 512:(hf + 1) * 512],
                                        start=(c == 0), stop=(c == 3)), r=[("hid", n % 2), ("wde", e2)], w=[("ps", bank)])
                                evac(y_sb[y2][:, hf * 512:(hf + 1) * 512], ps[bank][:], r=[("ps", bank)], w=[("y_sb", y2)],
                                     eng=("act" if hf == 0 else "dve"))
                            ld("sp", ye_d[r0 + i * 128:r0 + (i + 1) * 128, :], y_sb[y2][:], ["ye_d"], r=[("y_sb", y2)])

                    stg = [e_s0, e_s1, e_s2, e_s3]
                    assert NS >= 2
                    for step in range(len(tl) + len(stg) - 1):
                        for si in reversed(range(len(stg))):
                            n = step - si
                            if 0 <= n < len(tl):
                                stg[si](n)
                sc.barrier()

                with ExitStack() as st:
                    NX = 6
                    x1b = [sbt(st, "x1b%d" % i, [128, D], F32) for i in range(NX)]
                    y1 = [sbt(st, "y1_%d" % i, [128, D], F32) for i in range(3)]
                    y2b = [sbt(st, "y2_%d" % i, [128, D], F32) for i in range(3)]
                    hpb = [sbt(st, "hpb%d" % i, [128, D], BF16) for i in range(2)]
                    hpT = [sbt(st, "hpT%d" % i, [128, 8, 128], BF16) for i in range(2)]
                    pbf = [sbt(st, "pbf%d" % i, [128, 256], BF16) for i in range(4)]
                    pT = [sbt(st, "pT%d" % i, [128, 2, 128], BF16) for i in range(2)]
                    sig = [sbt(st, "sig%d" % i, [128, D], F32) for i in range(2)]
                    tmpx = [sbt(st, "tmpx%d" % i, [128, D], F32) for i in range(2)]
                    ob = [sbt(st, "ob%d" % i, [128, D], F32) for i in range(2)]
                    ssq7 = sbt(st, "ssq7", [128, NB, 2], F32)
                    rs7 = sbt(st, "rs7", [128, NB, 2], F32)

                    def f_s0(b):
                        ld("sp", x1b[b % NX][:], x1_d[b * 128:(b + 1) * 128, :], [("x1b", b % NX)], r=[("x1_d", b)])
                        ld("pool", pbf[b % 4][:], p_in[b * 128:(b + 1) * 128, :], [("pbf", b % 4)])
                        for kk, yb in enumerate((y1, y2b)):
                            sc.dma("pool", lambda e, kk=kk, yb=yb: e.indirect_dma_start(
                                out=yb[b % 3][:], out_offset=None, in_=ye_d[:, :],
                                in_offset=bass.IndirectOffsetOnAxis(ap=dest_i[:, b, kk:kk + 1], axis=0)),
                                r=["ye_d", ("dest", b)], w=[("y%d" % kk, b % 3)])

                    def f_s1(b):
                        xx = x1b[b % NX]
                        for kk, yb in enumerate((y1, y2b)):
                            sc.op("dve", lambda e, kk=kk, yb=yb: e.scalar_tensor_tensor(
                                out=xx[:], in0=yb[b % 3][:], scalar=wts[:, b, kk:kk + 1], in1=xx[:], op0=ALU.mult, op1=ALU.add),
                                r=[("y%d" % kk, b % 3), ("wts", b), ("x1b", b % NX)], w=[("x1b", b % NX)])
                        if debug:
                            ld("sp", x2_d[b * 128:(b + 1) * 128, :], xx[:], ["x2_d"], r=[("x1b", b % NX)])
                        sc.op("act", lambda e: e.activation(out=junk[:], in_=xx[:], func=AF.Square, accum_out=ssq7[:, b, 0:1]),
                              r=[("x1b", b % NX)], w=["junk", ("ssq7", b)])
                        rstd_ops(ssq7[:, b, 0:1], rs7[:, b, 0:1], ("ssq7", b), ("rs7", b))
                        sc.op("dve", lambda e: e.scalar_tensor_tensor(
                            out=hpb[b % 2][:], in0=xx[:], scalar=rs7[:, b, 0:1], in1=gple_s[:], op0=ALU.mult, op1=ALU.mult),
                            r=[("x1b", b % NX), ("rs7", b), "gple"], w=[("hpb", b % 2)])

                    def f_s2(b):
                        i2 = b % 2
                        pb = ps[0].bitcast(BF16)
                        for k in range(8):
                            sc.op("pe", lambda e, k=k, pb=pb: e.transpose(
                                out=pb[:, k * 128:(k + 1) * 128], in_=hpb[i2][:, k * 128:(k + 1) * 128], identity=ident[:]),
                                r=[("hpb", i2), "ident"], w=[("ps", 0)])
                        evac(hpT[i2][:], pb.rearrange("p (k t) -> p k t", k=8), r=[("ps", 0)], w=[("hpT", i2)], eng="act")
                        pb1 = ps[1].bitcast(BF16)
                        for k in range(2):
                            sc.op("pe", lambda e, k=k, pb1=pb1: e.transpose(
                                out=pb1[:, k * 128:(k + 1) * 128], in_=pbf[b % 4][:, k * 128:(k + 1) * 128], identity=ident[:]),
                                r=[("pbf", b % 4), "ident"], w=[("ps", 1)])
                        evac(pT[i2][:], pb1[:, 0:256].rearrange("p (k t) -> p k t", k=2), r=[("ps", 1)], w=[("pT", i2)], eng="act")

                    def f_s3(b):
                        i2 = b % 2
                        for hf in range(2):
                            gbk = (2 if b % 2 == 0 else 6) + hf
                            pbk = 4 + hf
                            for k in range(8):
                                sc.op("pe", lambda e, k=k, hf=hf, gbk=gbk: e.matmul(
                                    ps[gbk][:], lhsT=hpT[i2][:, k, :], rhs=wpg[:, k, hf * 512:(hf + 1) * 512],
                                    start=(k == 0), stop=(k == 7)), r=[("hpT", i2), "wpg"], w=[("ps", gbk)])
                            for k in range(2):
                                sc.op("pe", lambda e, k=k, hf=hf, pbk=pbk: e.matmul(
                                    ps[pbk][:], lhsT=pT[i2][:, k, :], rhs=wpl[:, k, hf * 512:(hf + 1) * 512],
                                    start=(k == 0), stop=(k == 1)), r=[("pT", i2), "wpl"], w=[("ps", pbk)])
                            sc.op("act", lambda e, hf=hf, gbk=gbk: e.activation(
                                out=sig[i2][:, hf * 512:(hf + 1) * 512], in_=ps[gbk][:], func=AF.Sigmoid),
                                r=[("ps", gbk)], w=[("sig", i2)])
                            sc.op("dve", lambda e, hf=hf, pbk=pbk: e.tensor_tensor(
                                out=tmpx[i2][:, hf * 512:(hf + 1) * 512], in0=ps[pbk][:], in1=sig[i2][:, hf * 512:(hf + 1) * 512], op=ALU.mult),
                                r=[("ps", pbk), ("sig", i2)], w=[("tmpx", i2)])

                    def f_s4(b):
                        i2 = b % 2
                        sc.op("pool", lambda e: e.tensor_tensor(out=tmpx[i2][:], in0=tmpx[i2][:], in1=x1b[b % NX][:], op=ALU.add),
                              r=[("tmpx", i2), ("x1b", b % NX)], w=[("tmpx", i2)])
                        sc.op("act", lambda e: e.activation(out=junk[:], in_=tmpx[i2][:], func=AF.Square, accum_out=ssq7[:, b, 1:2]),
                              r=[("tmpx", i2)], w=["junk", ("ssq7b", b)])
                        rstd_ops(ssq7[:, b, 1:2], rs7[:, b, 1:2], ("ssq7b", b), ("rs7b", b))
                        sc.op("dve", lambda e: e.scalar_tensor_tensor(
                            out=ob[i2][:], in0=tmpx[i2][:], scalar=rs7[:, b, 1:2], in1=gfin_s[:], op0=ALU.mult, op1=ALU.mult),
                            r=[("tmpx", i2), ("rs7b", b), "gfin"], w=[("ob", i2)])
                        ld("sp", out_d[b * 128:(b + 1) * 128, :], ob[i2][:], ["out_d"], r=[("ob", i2)])

                    stg = [(f_s0, 0), (f_s1, 2), (f_s2, 3), (f_s3, 4), (f_s4, 5)]
                    for step in range(NB + 5):
                        for f, off in stg:
                            b = step - off
                            if 0 <= b < NB:
                                f(b)
        stats = sc.emit()
        print("stats", stats)
    return nc


S_FULL = 4096
C_CAP = 768
_NC_CACHE = {}


def kernel(x, p, norm_mix, w_in, ret_gn, w_br_ret, w_br_sb, w_out, norm_ffn,
           w_grp, b_grp, w_exp, b_exp, w_gate, w_up, w_down,
           norm_ple, w_ple, w_ple_gate, norm_final):
    f = lambda a: np.ascontiguousarray(np.asarray(a, dtype=np.float32))
    x = f(x); p = f(p)
    B = x.shape[0]
    if "nc" not in _NC_CACHE:
        _NC_CACHE["nc"] = build(S_FULL, C_CAP, debug=False)
    nc = _NC_CACHE["nc"]
    shared = {
        "w_in": f(w_in)[0], "w_br_ret": f(w_br_ret)[0], "w_br_sb": f(w_br_sb)[0], "w_out": f(w_out)[0],
        "w_rt": np.ascontiguousarray(np.concatenate([f(w_grp)[0], f(w_exp)[0]], axis=1)),
        "b_rt": rep(np.concatenate([f(b_grp)[0], f(b_exp)[0]])),
        "w_gate": f(w_gate)[0], "w_up": f(w_up)[0], "w_down": f(w_down)[0],
        "w_ple": f(w_ple)[0], "w_ple_gate": f(w_ple_gate)[0],
        "gmix": rep(f(norm_mix)[0]), "gffn": rep(f(norm_ffn)[0]), "gple": rep(f(norm_ple)[0]),
        "gfin": rep(f(norm_final)), "ggn": rep(f(ret_gn)[0]),
    }
    cst = consts_np(S_FULL, C_CAP)
    cst.pop("cdec")
    shared.update({k: np.ascontiguousarray(v) for k, v in cst.items()})
    in_maps = []
    for b in range(B):
        m = dict(shared)
        m["x"] = x[b]
        m["p"] = p[0, b]
        in_maps.append(m)
    res = run_bass_kernel_spmd(nc, in_maps, core_ids=list(range(B)))
    return np.stack([np.asarray(r["out"], dtype=np.float32) for r in res.results], axis=0)
```

```python
import numpy as np
from contextlib import ExitStack
import ml_dtypes
from concourse.bass_utils import run_bass_kernel_spmd
import concourse.bass as bass
import concourse.mybir as mybir

F32 = mybir.dt.float32
BF16 = mybir.dt.bfloat16
I32 = mybir.dt.int32
AF = mybir.ActivationFunctionType
ALU = mybir.AluOpType
AX = mybir.AxisListType


class Sched:
    RING = 12

    def __init__(self, nc, stack):
        self.nc = nc
        self.eng = dict(pe=nc.tensor, act=nc.scalar, dve=nc.vector, pool=nc.gpsimd, sp=nc.sync)
        self.ops = []
        self.sem = {e: stack.enter_context(nc.semaphore("sem_" + e)) for e in self.eng}
        self.rings = {
            q: [stack.enter_context(nc.semaphore("ring_%s_%d" % (q, i))) for i in range(self.RING)]
            for q in ("sp", "pool")
        }

    def op(self, eng, fn, r=(), w=()):
        self.ops.append((eng, fn, tuple(r), tuple(w), False))

    def dma(self, q, fn, r=(), w=()):
        self.ops.append((q, fn, tuple(r), tuple(w), True))

    def barrier(self):
        self.ops.append(("barrier", None, (), (), False))

    def emit(self):
        ops = self.ops
        n = len(ops)
        last_w = {}
        readers = {}
        deps = [None] * n
        need_sig = [False] * n
        last_on = {}
        for i, (e, fn, r, w, isd) in enumerate(ops):
            if e == "barrier":
                for x, j in last_on.items():
                    need_sig[j] = True
                deps[i] = set()
                continue
            d = set()
            for k in r:
                if k in last_w:
                    d.update(last_w[k])
            for k in w:
                if k in last_w:
                    d.update(last_w[k])
                d.update(readers.get(k, ()))
            d.discard(i)
            best = {}
            dd = set()
            for j in d:
                ej, _, _, _, isdj = ops[j]
                if isdj:
                    dd.add(j)
                else:
                    if ej not in best or best[ej] < j:
                        best[ej] = j
            for ej, j in best.items():
                if ej == "pe" and e == "pe" and not isd:
                    continue
                dd.add(j)
            deps[i] = dd
            for j in dd:
                need_sig[j] = True
            for k in w:
                if isd and k in last_w and all(ops[j][4] for j in last_w[k]) and not readers.get(k):
                    last_w[k] = last_w[k] + [i]
                else:
                    last_w[k] = [i]
                readers[k] = []
            for k in r:
                if k not in w:
                    readers.setdefault(k, []).append(i)
            if not isd:
                last_on[e] = i
        cnt = {e: 0 for e in self.eng}
        sig = [None] * n
        waited = {e: {} for e in self.eng}
        dma_n = {q: 0 for q in self.rings}
        ring_last = {}
        nwaits = 0

        snap = {}

        def wait(e, sem, val):
            nonlocal nwaits
            key = id(sem)
            if waited[e].get(key, 0) >= val:
                return
            waited[e][key] = val
            self.eng[e].wait_ge(sem, val)
            nwaits += 1
            for k2, v2 in snap.get((key, val), {}).items():
                if waited[e].get(k2, 0) < v2:
                    waited[e][k2] = v2

        for i, (e, fn, r, w, isd) in enumerate(ops):
            if e == "barrier":
                for x in self.eng:
                    for y in self.eng:
                        if cnt[y] > 0:
                            wait(x, self.sem[y], cnt[y])
                    for q in self.rings:
                        for s_, v_ in ring_last.get(q, {}).values():
                            wait(x, s_, v_)
                continue
            for j in sorted(deps[i]):
                s_, v_ = sig[j]
                wait(e, s_, v_)
            if isd:
                m = dma_n[e]
                dma_n[e] += 1
                s_ = self.rings[e][m % self.RING]
                v_ = 16 * (m // self.RING + 1)
                if v_ > 16:
                    wait(e, s_, v_ - 16)
                inst = fn(self.eng[e])
                inst.then_inc(s_, 16)
                sig[i] = (s_, v_)
                snap[(id(s_), v_)] = dict(waited[e])
                ring_last.setdefault(e, {})[m % self.RING] = (s_, v_)
            else:
                inst = fn(self.eng[e])
                if need_sig[i]:
                    cnt[e] += 1
                    inst.then_inc(self.sem[e], 1)
                    sig[i] = (self.sem[e], cnt[e])
                    snap[(id(self.sem[e]), cnt[e])] = dict(waited[e])
        for q in self.rings:
            for s_, v_ in ring_last.get(q, {}).values():
                wait("sp", s_, v_)
        self.stats = dict(n_ops=n, n_waits=nwaits, cnt=dict(cnt), dma=dict(dma_n))
        return self.stats


D = 1024
EPS = 1e-6
BF = ml_dtypes.bfloat16


def consts_np(S, C):
    NB = S // 128
    c = {}
    c["ident_bf"] = np.eye(128, dtype=np.float32).astype(BF)
    c["ident_f"] = np.eye(128, dtype=np.float32)
    j = np.arange(128)[:, None]
    s = np.arange(128)[None, :]
    c["ntri"] = np.where(j >= s, -1.0, 0.0).astype(np.float32).astype(BF)
    c["nones"] = (-np.ones((128, 128), np.float32)).astype(BF)
    c["tri_lt"] = np.where(j < s, 1.0, 0.0).astype(np.float32).astype(BF)
    c["ones_bf"] = np.ones((128, 128), np.float32).astype(BF)
    m = np.zeros((4, 128, 512), np.float32)
    for jd in range(4):
        key = jd * 128 + np.arange(128)[:, None]
        tq = np.arange(512)[None, :]
        m[jd] = (key < tq).astype(np.float32)
    c["mask01"] = ((m.transpose(1, 0, 2) - 1.0) * 30000.0).astype(np.float32).copy()
    pos = np.arange(S, dtype=np.float32)
    inv_freq = (10000.0 ** (-np.arange(0, 128, 2, dtype=np.float32) / 128)).astype(np.float32)
    ang = pos[:, None] * inv_freq[None, :]
    c["cos_t"] = np.cos(ang).astype(np.float32).reshape(NB, 128, 64).transpose(1, 0, 2).copy()
    c["sin_t"] = np.sin(ang).astype(np.float32).reshape(NB, 128, 64).transpose(1, 0, 2).copy()
    gamma = 1.0 - np.exp2(-5.0 - np.arange(4, dtype=np.float64))
    lg = np.log(gamma)
    jj = np.arange(128)[:, None]
    ii = np.arange(128)[None, :]
    allowed = (jj // 64) <= (ii // 64)
    sc = 128.0 ** -0.5
    Dp = np.zeros((128, 4, 128), np.float64)
    for h in range(4):
        Dp[:, h, :] = np.where(allowed, np.exp(lg[h] * (np.abs(ii - jj) - (ii + 1.0))), 0.0) * sc
    c["Dp"] = Dp.astype(np.float32)
    qdec = np.exp(lg[None, :] * (np.arange(128)[:, None] + 1.0))
    c["qdec"] = np.repeat(qdec[:, :, None], 128, axis=2).astype(np.float32)
    kdec = np.exp(lg[None, :] * (127.0 - np.arange(128)[:, None])) * sc
    c["kdec"] = np.repeat(kdec[:, :, None], 128, axis=2).astype(np.float32)
    c["cdec"] = np.exp(lg * 128.0)
    c["ebase"] = np.tile((np.arange(16, dtype=np.float32) * C)[None], (128, 1))
    return c


def rep(v):
    return np.tile(np.asarray(v, np.float32).reshape(1, -1), (128, 1))


def build(S, C, debug=False):
    NB = S // 128
    NG = S // 512
    cst = consts_np(S, C)
    cdec = [float(v) for v in cst["cdec"]]
    nc = bass.Bass("TRN2", target_bir_lowering=False)

    def din(name, shape, dt=F32):
        return nc.dram_tensor(name, list(shape), dt, kind="ExternalInput").ap()

    def dscr(name, shape, dt):
        return nc.dram_tensor(name, list(shape), dt, kind="ExternalOutput" if debug else "Internal").ap()

    x = din("x", [S, D])
    p_in = din("p", [S, 256])
    w_in = din("w_in", [D, 5632])
    w_br_ret = din("w_br_ret", [512, D])
    w_br_sb = din("w_br_sb", [512, D])
    w_out = din("w_out", [D, D])
    w_rt = din("w_rt", [D, 20])
    b_rt = din("b_rt", [128, 20])
    w_gate = din("w_gate", [16, D, 512])
    w_up = din("w_up", [16, D, 512])
    w_down = din("w_down", [16, 512, D])
    w_ple = din("w_ple", [256, D])
    w_pg = din("w_ple_gate", [D, D])
    gmix = din("gmix", [128, D])
    gffn = din("gffn", [128, D])
    gple = din("gple", [128, D])
    gfin = din("gfin", [128, D])
    ggn = din("ggn", [128, 512])
    ident_d = din("ident_bf", [128, 128], BF16)
    identf_d = din("ident_f", [128, 128])
    ntri_d = din("ntri", [128, 128], BF16)
    nones_d = din("nones", [128, 128], BF16)
    trilt_d = din("tri_lt", [128, 128], BF16)
    onesbf_d = din("ones_bf", [128, 128], BF16)
    mask01_d = din("mask01", [128, 4, 512])
    cos_d = din("cos_t", [128, NB, 64])
    sin_d = din("sin_t", [128, NB, 64])
    Dp_d = din("Dp", [128, 4, 128])
    qdec_d = din("qdec", [128, 4, 128])
    kdec_d = din("kdec", [128, 4, 128])
    ebase_d = din("ebase", [128, 16])

    out_d = nc.dram_tensor("out", [S, D], F32, kind="ExternalOutput").ap()
    sbT_d = dscr("sbT", [8, 64, S], BF16)
    retT_d = dscr("retT", [128, 4, S], BF16)
    x1_d = dscr("x1", [S, D], F32)
    xe_d = dscr("xe", [16 * C, D], BF16)
    ye_d = dscr("ye", [16 * C, D], F32)
    if debug:
        rt_d = nc.dram_tensor("rt_dbg", [128, NB, 4], F32, kind="ExternalOutput").ap()
        x2_d = nc.dram_tensor("x2_dbg", [S, D], F32, kind="ExternalOutput").ap()

    with ExitStack() as st0:
        sc = Sched(nc, st0)
        psr = [st0.enter_context(nc.psum_tensor("psr%d" % i, [128, 1024], F32)) for i in range(4)]
        ps = [psr[i // 2][:, (i % 2) * 512:(i % 2 + 1) * 512] for i in range(8)]
        cpc = [0]

        def evac(out, in_, r, w, eng=None):
            if eng is None:
                eng = "act" if cpc[0] % 2 == 0 else "dve"
                cpc[0] += 1
            if eng == "act":
                sc.op("act", lambda e: e.copy(out=out, in_=in_), r=r, w=w)
            else:
                sc.op("dve", lambda e: e.tensor_copy(out=out, in_=in_), r=r, w=w)

        def ld(q, out, in_, w, r=()):
            sc.dma(q, lambda e: e.dma_start(out=out, in_=in_), r=r, w=w)

        def sbt(stk, name, shape, dt):
            return stk.enter_context(nc.sbuf_tensor(name, list(shape), dt))

        def rstd_ops(ssq_ap, rs_ap, kssq, krs, n=D):
            sc.op("dve", lambda e: e.tensor_scalar(out=rs_ap, in0=ssq_ap, scalar1=1.0 / n, scalar2=EPS,
                                                   op0=ALU.mult, op1=ALU.add), r=[kssq], w=[krs])
            pow_ops(rs_ap, krs)

        def pow_ops(rs_ap, krs):
            sc.op("pool", lambda e: e.tensor_tensor(out=rs_ap, in0=rs_ap, in1=nhalf[:, 0:rs_ap.shape[-1]], op=ALU.pow),
                  r=[krs, "nhalf"], w=[krs])

        ident = sbt(st0, "ident", [128, 128], BF16)
        ld("sp", ident[:], ident_d, ["ident"])
        junk = sbt(st0, "junk", [128, D], BF16)
        nhalf = sbt(st0, "nhalf", [128, 8], F32)
        sc.op("pool", lambda e: e.memset(nhalf[:], -0.5), w=["nhalf"])

        hT_d = dscr("hT_scr", [128, 8, S], BF16)
        qT_d = dscr("qT_scr", [128, 4, S], BF16)
        with ExitStack() as stH:
            with ExitStack() as st:
                ntri = sbt(st, "ntri_s", [128, 128], BF16)
                nones = sbt(st, "nones_s", [128, 128], BF16)
                mask01 = sbt(st, "mask01_s", [128, 4, 512], BF16)
                ld("sp", ntri[:], ntri_d, ["ntri"])
                ld("sp", nones[:], nones_d, ["nones"])
                ld("pool", mask01[:], mask01_d, ["mask01"])
                kT = sbt(st, "kT", [128, 4, S], BF16)
                vv = sbt(st, "vv", [128, NB, 512], BF16)
                qg = [sbt(st, "qg%d" % i, [128, 4, 512], BF16) for i in range(2)]
                with ExitStack() as stA:
                    hT = sbt(stA, "hT", [128, 8, S], BF16)
                    wsb = sbt(stA, "wsb", [128, 8, 1536], BF16)
                    qst = [sbt(stA, "qst%d" % i, [128, 512], BF16) for i in range(2)]
                    w_v = w_in.rearrange("(k p) c -> p k c", p=128)
                    for k in range(8):
                        ld("pool", wsb[:, k, :], w_v[:, k, 2048:3584], [("wsb", k)])
                    WSB = [("wsb", k) for k in range(8)]
                    gmix_s = sbt(stA, "gmix_s", [128, D], F32)
                    ld("sp", gmix_s[:], gmix, ["gmix"])
                    xt = [sbt(stA, "xt%d" % i, [128, D], F32) for i in range(4)]
                    xn = [sbt(stA, "xn%d" % i, [128, D], BF16) for i in range(3)]
                    ssq = sbt(stA, "ssq", [128, NB], F32)
                    rs = sbt(stA, "rs", [128, NB], F32)
                    pj = [0]
                    qcn = [0]

                    def pbank():
                        pj[0] += 1
                        return 4 + pj[0] % 4

                    def proj_qk(T, j, which):
                        bank = pbank()
                        for k in range(8):
                            sc.op("pe", lambda e, k=k: e.matmul(
                                ps[bank][:], lhsT=wsb[:, k, which * 512 + j * 128:which * 512 + (j + 1) * 128],
                                rhs=hT[:, k, T * 512:(T + 1) * 512], start=(k == 0), stop=(k == 7)),
                                r=WSB + [("hT", T * 4 + i) for i in range(4)], w=[("ps", bank)])
                        if which == 0:
                            q2 = qcn[0] % 2
                            qcn[0] += 1
                            sc.op("act", lambda e: e.mul(out=qst[q2][:], in_=ps[bank][:], mul=0.125),
                                  r=[("ps", bank)], w=[("qst", q2)])
                            ld("sp", qT_d[:, j, T * 512:(T + 1) * 512], qst[q2][:], [("qT_d", T)], r=[("qst", q2)])
                        else:
                            evac(kT[:, j, T * 512:(T + 1) * 512], ps[bank][:], r=[("ps", bank)], w=[("kT", T)], eng="dve")

                    def proj_v(b):
                        bank = pbank()
                        for k in range(8):
                            sc.op("pe", lambda e, k=k: e.matmul(
                                ps[bank][:], lhsT=hT[:, k, b * 128:(b + 1) * 128], rhs=wsb[:, k, 1024:1536],
                                start=(k == 0), stop=(k == 7)),
                                r=WSB + [("hT", b)], w=[("ps", bank)])
                        evac(vv[:, b, :], ps[bank][:], r=[("ps", bank)], w=[("vv", b)], eng="dve")

                    pq_ = []

                    def a_s0(b):
                        ld("sp", xt[b % 4][:], x[b * 128:(b + 1) * 128, :], [("xt", b % 4)])

                    def a_s1(b):
                        sc.op("act", lambda e: e.activation(out=junk[:], in_=xt[b % 4][:], func=AF.Square, accum_out=ssq[:, b:b + 1]),
                              r=[("xt", b % 4)], w=["junk", ("ssq", b)])
                        rstd_ops(ssq[:, b:b + 1], rs[:, b:b + 1], ("ssq", b), ("rs", b))

                    def a_s2(b):
                        sc.op("dve", lambda e: e.scalar_tensor_tensor(
                            out=xn[b % 3][:], in0=xt[b % 4][:], scalar=rs[:, b:b + 1], in1=gmix_s[:], op0=ALU.mult, op1=ALU.mult),
                            r=[("xt", b % 4), ("rs", b), "gmix"], w=[("xn", b % 3)])

                    def a_s3(b):
                        pb = ps[b % 4].bitcast(BF16)
                        for k in range(8):
                            sc.op("pe", lambda e, k=k, pb=pb: e.transpose(
                                out=pb[:, k * 128:(k + 1) * 128], in_=xn[b % 3][:, k * 128:(k + 1) * 128], identity=ident[:]),
                                r=[("xn", b % 3), "ident"], w=[("ps", b % 4)])
                        evac(hT[:, :, b * 128:(b + 1) * 128], pb.rearrange("p (k t) -> p k t", k=8),
                             r=[("ps", b % 4)], w=[("hT", b)], eng="act")
                        if b % 4 == 3:
                            T_ = b // 4
                            ld("sp", hT_d[:, :, T_ * 512:(T_ + 1) * 512], hT[:, :, T_ * 512:(T_ + 1) * 512], [("hT_d", T_)],
                               r=[("hT", T_ * 4 + i) for i in range(4)])
                            for j_ in range(4):
                                for wh_ in range(2):
                                    pq_.append(lambda T_=T_, j_=j_, wh_=wh_: proj_qk(T_, j_, wh_))
                            for i_ in range(4):
                                pq_.append(lambda bb=T_ * 4 + i_: proj_v(bb))

                    stg1 = [a_s0, a_s1, a_s2, a_s3]
                    for step in range(NB + len(stg1) - 1):
                        for si in reversed(range(len(stg1))):
                            bb_ = step - si
                            if 0 <= bb_ < NB:
                                stg1[si](bb_)
                        for _ in range(3):
                            if pq_:
                                pq_.pop(0)()
                    while pq_:
                        pq_.pop(0)()
                sc.barrier()
                NE = 3
                e_bf = [sbt(st, "e_bf%d" % i, [128, 2, 512], BF16) for i in range(NE)]
                sp_bf = [sbt(st, "sp_bf%d" % i, [128, 2, 512], BF16) for i in range(NE)]
                E_bf = [sbt(st, "E_bf%d" % i, [128, 2, 512], BF16) for i in range(2)]
                NA = 5
                a_bf = [sbt(st, "a_bf%d" % i, [128, 2, 512], BF16) for i in range(NA)]
                S_bf = [sbt(st, "S_bf%d" % i, [128, 2, 512], BF16) for i in range(3)]
                o_sb = [sbt(st, "o_sb%d" % i, [128, 512], BF16) for i in range(2)]
                zt = sbt(st, "zt", [128, 1024], BF16)
                sc.op("pool", lambda e: e.memset(zt[:], 0.0), w=["zt"])


                with ExitStack() as st3:
                    wr = sbt(st3, "wr", [128, 8, 2048], BF16)
                    w_v = w_in.rearrange("(k p) c -> p k c", p=128)
                    for k in range(8):
                        ld("pool", wr[:, k, :], w_v[:, k, 0:2048], [("wr", k)])
                    WR = [("wr", k) for k in range(8)]
                    Dp_s = sbt(st3, "Dp_s", [128, 4, 128], F32)
                    qdec_s = sbt(st3, "qdec_s", [128, 4, 128], F32)
                    kdec_s = sbt(st3, "kdec_s", [128, 4, 128], F32)
                    ggn_s = sbt(st3, "ggn_s", [128, 512], F32)
                    ld("sp", Dp_s[:], Dp_d, ["Dp"])
                    ld("sp", qdec_s[:], qdec_d, ["qdec"])
                    ld("sp", kdec_s[:], kdec_d, ["kdec"])
                    ld("sp", ggn_s[:], ggn, ["ggn"])
                    state_f = sbt(st3, "state_f", [128, 4, 128], F32)
                    state_b = [sbt(st3, "state_b%d" % i, [128, 4, 128], BF16) for i in range(2)]
                    hTb = [sbt(st3, "hTb%d" % i, [128, 8, 128], BF16) for i in range(2)]
                    cs = [sbt(st3, "cs%d" % i, [128, 2, 64], F32) for i in range(2)]
                    q_sb = sbt(st3, "q_sb", [128, 512], F32)
                    k_sb = sbt(st3, "k_sb", [128, 512], F32)
                    eg = sbt(st3, "eg", [128, 512], F32)
                    q_r = sbt(st3, "q_r", [128, 4, 2, 64], BF16)
                    k_r = sbt(st3, "k_r", [128, 4, 2, 64], BF16)
                    kd = sbt(st3, "kd", [128, 4, 128], BF16)
                    v_bf = sbt(st3, "v_bf", [128, 512], BF16)
                    g_sil2 = [sbt(st3, "g_sil%d" % i, [128, 512], BF16) for i in range(2)]
                    qkT = sbt(st3, "qkT", [128, 8, 128], BF16)
                    scT = sbt(st3, "scT", [128, 4, 128], BF16)
                    o_s = sbt(st3, "o_s", [128, 4, 128], F32)
                    ret_b = sbt(st3, "ret_b", [128, 512], BF16)
                    retT_s = [sbt(st3, "retT_s%d" % i, [128, 4, 128], BF16) for i in range(2)]
                    tA = [sbt(st3, "tA%d" % i, [128, 4, 64], F32) for i in range(2)]
                    tB = [sbt(st3, "tB%d" % i, [128, 4, 64], F32) for i in range(2)]
                    bnst = sbt(st3, "bnst", [128, 4, 6], F32)
                    mv = sbt(st3, "mv", [128, 4, 2], F32)
                    rsd = sbt(st3, "rsd", [128, 4], F32)
                    sc.op("pool", lambda e: e.memset(state_f[:], 0.0), w=["state_f"])
                    BA, BB, BC = 5, 6, 7

                    def rope(src, dst, b, ksrc, kdst):
                        pv = src[:].rearrange("p (h two d) -> p h two d", h=4, two=2)
                        t1 = pv[:, :, 0, :]
                        t2 = pv[:, :, 1, :]
                        cb = cs[b % 2][:, 0, :].unsqueeze(1).broadcast_to([128, 4, 64])
                        sb_ = cs[b % 2][:, 1, :].unsqueeze(1).broadcast_to([128, 4, 64])
                        for half in range(2):
                            a0, a1 = (cb, sb_) if half == 0 else (sb_, cb)
                            op = ALU.subtract if half == 0 else ALU.add
                            sc.op("dve", lambda e, a0=a0, half=half: e.tensor_tensor(out=tA[half][:], in0=t1, in1=a0, op=ALU.mult),
                                  r=[ksrc, ("cs", b % 2)], w=[("tA", half)])
                            sc.op("dve", lambda e, a1=a1, half=half: e.tensor_tensor(out=tB[half][:], in0=t2, in1=a1, op=ALU.mult),
                                  r=[ksrc, ("cs", b % 2)], w=[("tB", half)])
                            sc.op("pool", lambda e, op=op, half=half: e.tensor_tensor(out=dst[:, :, half, :], in0=tA[half][:],
                                                                                     in1=tB[half][:], op=op),
                                  r=[("tA", half), ("tB", half)], w=[kdst])

                    def proj(b, wi, bank, half):
                        for k in range(half * 4, half * 4 + 4):
                            sc.op("pe", lambda e, k=k: e.matmul(
                                ps[bank][:], lhsT=hTb[b % 2][:, k, :], rhs=wr[:, k, wi * 512:(wi + 1) * 512],
                                start=(k == 0), stop=(k == 7)), r=WR + [("hTb", b % 2)], w=[("ps", bank)])

                    def m0(b):
                        ld("sp", hTb[b % 2][:], hT_d[:, :, b * 128:(b + 1) * 128], [("hTb", b % 2)], r=[("hT_d", b // 4)])
                        ld("sp", cs[b % 2][:, 0, :], cos_d[:, b, :], [("cs", b % 2)])
                        ld("sp", cs[b % 2][:, 1, :], sin_d[:, b, :], [("cs", b % 2)])

                    def pq_a(b):
                        proj(b, 0, BA, 0)

                    def pq_b(b):
                        proj(b, 0, BA, 1)

                    def pk_a(b):
                        sc.op("dve", lambda e: e.tensor_copy(out=q_sb[:], in_=ps[BA][:]), r=[("ps", BA)], w=["q_sb"])
                        proj(b, 1, BB, 0)

                    def pk_b(b):
                        proj(b, 1, BB, 1)
                        m6a(b)

                    def pv_a(b):
                        sc.op("dve", lambda e: e.tensor_copy(out=k_sb[:], in_=ps[BB][:]), r=[("ps", BB)], w=["k_sb"])
                        proj(b, 2, BA, 0)

                    def pv_b(b):
                        proj(b, 2, BA, 1)
                        m6b(b)

                    def pg_a(b):
                        sc.op("dve", lambda e: e.tensor_copy(out=v_bf[:], in_=ps[BA][:]), r=[("ps", BA)], w=["v_bf"])
                        proj(b, 3, BB, 0)

                    def pg_b(b):
                        proj(b, 3, BB, 1)
                        m6c(b)

                    def m5(b):
                        sc.op("act", lambda e: e.activation(out=eg[:], in_=ps[BB][:], func=AF.Exp, scale=-1.0), r=[("ps", BB)], w=["eg"])
                        sc.op("dve", lambda e: e.tensor_scalar(out=eg[:], in0=eg[:], scalar1=1.0, scalar2=None, op0=ALU.add), r=["eg"], w=["eg"])
                        sc.op("dve", lambda e: e.reciprocal(out=eg[:], in_=eg[:]), r=["eg"], w=["eg"])
                        sc.op("dve", lambda e: e.tensor_tensor(out=g_sil2[b % 2][:], in0=ps[BB][:], in1=eg[:], op=ALU.mult),
                              r=[("ps", BB), "eg"], w=[("g_sil", b % 2)])

                    def rope_half(src, dst, b, ksrc, kdst, half):
                        pv = src[:].rearrange("p (h two d) -> p h two d", h=4, two=2)
                        t1 = pv[:, :, 0, :]
                        t2 = pv[:, :, 1, :]
                        cb = cs[b % 2][:, 0, :].unsqueeze(1).broadcast_to([128, 4, 64])
                        sb_ = cs[b % 2][:, 1, :].unsqueeze(1).broadcast_to([128, 4, 64])
                        a0, a1 = (cb, sb_) if half == 0 else (sb_, cb)
                        op = ALU.subtract if half == 0 else ALU.add
                        sc.op("dve", lambda e: e.tensor_tensor(out=tA[half][:], in0=t1, in1=a0, op=ALU.mult),
                              r=[ksrc, ("cs", b % 2)], w=[("tA", half)])
                        sc.op("dve", lambda e: e.tensor_tensor(out=tB[half][:], in0=t2, in1=a1, op=ALU.mult),
                              r=[ksrc, ("cs", b % 2)], w=[("tB", half)])
                        sc.op("dve", lambda e: e.tensor_tensor(out=dst[:, :, half, :], in0=tA[half][:], in1=tB[half][:], op=op),
                              r=[("tA", half), ("tB", half)], w=[kdst])

                    def m6a(b):
                        rope_half(q_sb, q_r, b, "q_sb", "q_r", 0)

                    def m6b(b):
                        rope_half(q_sb, q_r, b, "q_sb", "q_r", 1)

                    def m6c(b):
                        rope_half(k_sb, k_r, b, "k_sb", "k_r", 0)

                    def m6d(b):
                        rope_half(k_sb, k_r, b, "k_sb", "k_r", 1)
                        sc.op("dve", lambda e: e.tensor_tensor(out=kd[:], in0=k_r[:].rearrange("p h two d -> p h (two d)"),
                                                                in1=kdec_s[:], op=ALU.mult),
                              r=["k_r", "kdec"], w=["kd"])

                    def m7(b):
                        pb = ps[BC].bitcast(BF16)
                        for hh in range(4):
                            sc.op("pe", lambda e, hh=hh, pb=pb: e.transpose(
                                out=pb[:, hh * 128:(hh + 1) * 128], in_=q_r[:, hh].rearrange("p two d -> p (two d)"),
                                identity=ident[:]), r=["q_r", "ident"], w=[("ps", BC)])
                        for hh in range(4):
                            sc.op("pe", lambda e, hh=hh, pb=pb: e.transpose(
                                out=pb[:, (4 + hh) * 128:(5 + hh) * 128], in_=k_r[:, hh].rearrange("p two d -> p (two d)"),
                                identity=ident[:]), r=["k_r", "ident"], w=[("ps", BC)])

                    def m8(b):
                        pb = ps[BC].bitcast(BF16)
                        sc.op("dve", lambda e: e.tensor_copy(out=qkT[:], in_=pb.rearrange("p (k t) -> p k t", k=8)), r=[("ps", BC)], w=["qkT"])

                    def m9(b):
                        for hh in range(4):
                            sc.op("pe", lambda e, hh=hh: e.matmul(
                                ps[BA][:, hh * 128:(hh + 1) * 128], lhsT=qkT[:, 4 + hh, :], rhs=qkT[:, hh, :],
                                start=True, stop=True), r=["qkT"], w=[("ps", BA)])

                    def m10(b):
                        sc.op("dve", lambda e: e.tensor_tensor(out=scT[:], in0=ps[BA][:].rearrange("p (h i) -> p h i", h=4),
                                                               in1=Dp_s[:], op=ALU.mult),
                              r=[("ps", BA), "Dp"], w=["scT"])

                    def m11(b):
                        sbi = b % 2
                        for hh in range(4):
                            sc.op("pe", lambda e, hh=hh: e.matmul(
                                ps[BB][:, hh * 128:(hh + 1) * 128], lhsT=scT[:, hh, :], rhs=v_bf[:, hh * 128:(hh + 1) * 128],
                                start=True, stop=(b == 0)), r=["scT", "v_bf"], w=[("ps", BB)])
                            if b > 0:
                                sc.op("pe", lambda e, hh=hh: e.matmul(
                                    ps[BB][:, hh * 128:(hh + 1) * 128], lhsT=qkT[:, hh, :], rhs=state_b[sbi][:, hh, :],
                                    start=False, stop=True), r=["qkT", ("state_b", sbi)], w=[("ps", BB)])
                        if b < NB - 1:
                            for hh in range(4):
                                sc.op("pe", lambda e, hh=hh: e.matmul(
                                    ps[BC][:, hh * 128:(hh + 1) * 128], lhsT=kd[:, hh, :], rhs=v_bf[:, hh * 128:(hh + 1) * 128],
                                    start=True, stop=True), r=["kd", "v_bf"], w=[("ps", BC)])

                    def m12(b):
                        if b < NB - 1:
                            for hh in range(4):
                                sc.op("dve", lambda e, hh=hh: e.scalar_tensor_tensor(
                                    out=state_f[:, hh, :], in0=state_f[:, hh, :], scalar=cdec[hh], in1=ps[BC][:, hh * 128:(hh + 1) * 128],
                                    op0=ALU.mult, op1=ALU.add), r=["state_f", ("ps", BC)], w=["state_f"])
                            nsb = (b + 1) % 2
                            sc.op("pool", lambda e: e.tensor_copy(out=state_b[nsb][:], in_=state_f[:]),
                                  r=["state_f"], w=[("state_b", nsb)])
                        sc.op("dve", lambda e: e.tensor_tensor(out=o_s[:], in0=ps[BB][:].rearrange("p (h e) -> p h e", h=4),
                                                               in1=qdec_s[:], op=ALU.mult),
                              r=[("ps", BB), "qdec"], w=["o_s"])

                    def m13(b):
                        for hh in range(4):
                            sc.op("dve", lambda e, hh=hh: e.bn_stats(out=bnst[:, hh, :], in_=o_s[:, hh, :]), r=["o_s"], w=["bnst"])
                        for hh in range(4):
                            sc.op("dve", lambda e, hh=hh: e.bn_aggr(out=mv[:, hh, :], in_=bnst[:, hh, :]), r=["bnst"], w=["mv"])
                        sc.op("dve", lambda e: e.tensor_scalar(out=rsd[:], in0=mv[:, :, 1], scalar1=EPS, scalar2=None, op0=ALU.add),
                              r=["mv"], w=["rsd"])
                        pow_ops(rsd[:], "rsd")

                    def m14(b):
                        for hh in range(4):
                            sc.op("dve", lambda e, hh=hh: e.tensor_scalar(
                                out=o_s[:, hh, :], in0=o_s[:, hh, :], scalar1=mv[:, hh, 0:1], scalar2=rsd[:, hh:hh + 1],
                                op0=ALU.subtract, op1=ALU.mult), r=["o_s", "mv", "rsd"], w=["o_s"])
                        sc.op("pool", lambda e: e.tensor_tensor(out=o_s[:], in0=o_s[:], in1=ggn_s[:].rearrange("p (h e) -> p h e", h=4), op=ALU.mult),
                              r=["o_s", "ggn"], w=["o_s"])
                        sc.op("pool", lambda e: e.tensor_tensor(out=ret_b[:], in0=o_s[:].rearrange("p h e -> p (h e)"), in1=g_sil2[b % 2][:], op=ALU.mult),
                              r=["o_s", ("g_sil", b % 2)], w=["ret_b"])

                    def m15(b):
                        pb6 = ps[BC].bitcast(BF16)
                        for hh in range(4):
                            sc.op("pe", lambda e, hh=hh, pb6=pb6: e.transpose(
                                out=pb6[:, hh * 128:(hh + 1) * 128], in_=ret_b[:, hh * 128:(hh + 1) * 128], identity=ident[:]),
                                r=["ret_b", "ident"], w=[("ps", BC)])

                    def m16(b):
                        pb6 = ps[BC].bitcast(BF16)
                        sc.op("dve", lambda e: e.tensor_copy(out=retT_s[b % 2][:], in_=pb6[:, 0:512].rearrange("p (k t) -> p k t", k=4)),
                              r=[("ps", BC)], w=[("retT_s", b % 2)])
                        ld("sp", retT_d[:, :, b * 128:(b + 1) * 128], retT_s[b % 2][:], [("retT_d", b // 4)], r=[("retT_s", b % 2)])

                    msched = [(m0, 0), (pq_a, 1), (pq_b, 2), (pk_a, 3), (pk_b, 4), (pv_a, 5), (pv_b, 6), (pg_a, 7), (pg_b, 8),
                              (m6d, 9), (m5, 10), (m7, 13), (m8, 14), (m9, 15), (m10, 16), (m11, 17), (m12, 18), (m13, 19),
                              (m14, 21), (m15, 25), (m16, 26)]
                    RPER = 18
                    rsteps = {}
                    for b_ in range(NB):
                        for fn_, off_ in msched:
                            rsteps.setdefault(b_ * RPER + off_, []).append((b_, fn_))

                    sbT_v = sbT_d.rearrange("(pr two) d s -> (two d) pr s", two=2)
                    tiles = []
                    for g in range(NG):
                        for j in range(4):
                            nkb = 4 * (g + 1)
                            for idx, kb in enumerate(range(nkb - 1, -1, -1)):
                                tiles.append(dict(g=g, j=j, kb=kb, idx=idx, last=(idx == nkb - 1), gj=g * 4 + j))
                    NT = len(tiles)
                    for i, t in enumerate(tiles):
                        t["i"] = i
                    OB = 4

                    def c0_of(t):
                        jd = t["kb"] - 4 * t["g"]
                        return jd * 128 if jd >= 1 else 0

                    def st_q(g):
                        ld("sp", qg[g % 2][:], qT_d[:, :, g * 512:(g + 1) * 512], [("qg", g % 2)], r=[("qT_d", g)])

                    def st_z(t):
                        i = t["i"]; g = t["g"]; j = t["j"]; kb = t["kb"]
                        c0 = c0_of(t)
                        jd = kb - 4 * g
                        for hh in range(2):
                            po = hh * 64
                            sc.op("pe", lambda e, hh=hh, po=po: e.matmul(
                                psr[0][:, hh * 512 + c0:(hh + 1) * 512], lhsT=kT[po:po + 64, j, kb * 128:(kb + 1) * 128],
                                rhs=qg[g % 2][po:po + 64, j, c0:512], start=True, stop=(jd < 0)),
                                r=[("kT", kb // 4), ("qg", g % 2)], w=["zr"])
                        if jd >= 0:
                            for hh in range(2):
                                sc.op("pe", lambda e, hh=hh: e.matmul(
                                    psr[0][:, hh * 512 + c0:(hh + 1) * 512], lhsT=ident[:], rhs=mask01[:, jd, c0:512],
                                    start=False, stop=True), r=["ident", "mask01"], w=["zr"])
                        eb = i % NE
                        sc.op("act", lambda e: e.activation(out=e_bf[eb][:, :, c0:], in_=psr[0][:].rearrange("p (h q) -> p h q", h=2)[:, :, c0:], func=AF.Exp),
                              r=["zr"], w=[("e", eb)])

                    def st_ln(t):
                        i = t["i"]
                        eb = i % NE
                        c0 = c0_of(t)
                        sc.op("act", lambda e: e.activation(out=sp_bf[eb][:, :, c0:], in_=e_bf[eb][:, :, c0:], func=AF.Ln, bias=1.0),
                              r=[("e", eb)], w=[("sp", eb)] + ([("splo", eb)] if c0 == 0 else []))
                        if c0 > 0:
                            sc.op("pool", lambda e: e.memset(sp_bf[eb][:, :, 0:c0], 0.0), r=[("sp", eb)], w=[("splo", eb)])
                        st_sadd(t)

                    def st_arg(t):
                        i = t["i"]; idx = t["idx"]
                        eb = i % NE
                        c0 = c0_of(t)
                        srcS_extra = []
                        if idx == 0:
                            srcS = None
                        elif idx == 1:
                            srcS = (sp_bf[(i - 1) % NE], ("sp", (i - 1) % NE))
                            srcS_extra = [("splo", (i - 1) % NE)]
                        else:
                            srcS = (S_bf[idx % 3], ("S", idx % 3))
                        for hh in range(2):
                            sc.op("pe", lambda e, hh=hh: e.matmul(psr[1][:, hh * 512 + c0:(hh + 1) * 512], lhsT=ntri[:], rhs=sp_bf[eb][:, hh, c0:],
                                                                  start=True, stop=(srcS is None)),
                                  r=[("sp", eb), "ntri"] + ([("splo", eb)] if c0 == 0 else []), w=["argr"])
                            if srcS is not None:
                                sc.op("pe", lambda e, hh=hh: e.matmul(psr[1][:, hh * 512 + c0:(hh + 1) * 512], lhsT=nones[:], rhs=srcS[0][:, hh, c0:],
                                                                      start=False, stop=True),
                                      r=[srcS[1], "nones"] + srcS_extra, w=["argr"])
                        sc.op("act", lambda e: e.activation(out=E_bf[i % 2][:, :, c0:], in_=psr[1][:].rearrange("p (h q) -> p h q", h=2)[:, :, c0:], func=AF.Exp),
                              r=["argr"], w=[("E", i % 2)])

                    def st_dve(t):
                        i = t["i"]; idx = t["idx"]
                        eb = i % NE
                        c0 = c0_of(t)
                        sc.op("dve", lambda e: e.tensor_tensor(out=a_bf[i % NA][:, :, c0:], in0=e_bf[eb][:, :, c0:], in1=E_bf[i % 2][:, :, c0:], op=ALU.mult),
                              r=[("e", eb), ("E", i % 2)], w=[("a", i % NA)] + ([("alo", i % NA)] if c0 == 0 else []))
                        if c0 > 0:
                            sc.op("pool", lambda e: e.memset(a_bf[i % NA][:, :, 0:c0], 0.0), r=[("a", i % NA)], w=[("alo", i % NA)])

                    def st_sadd(t):
                        i = t["i"]; idx = t["idx"]
                        eb = i % NE
                        if not t["last"] and idx >= 1:
                            nxt = (idx + 1) % 3
                            if idx == 1:
                                pe_ = (i - 1) % NE
                                sc.op("dve", lambda e: e.tensor_tensor(out=S_bf[nxt][:], in0=sp_bf[pe_][:], in1=sp_bf[eb][:], op=ALU.add),
                                      r=[("sp", pe_), ("sp", eb), ("splo", pe_), ("splo", eb)], w=[("S", nxt)])
                            else:
                                cur = idx % 3
                                sc.op("dve", lambda e: e.tensor_tensor(out=S_bf[nxt][:], in0=S_bf[cur][:], in1=sp_bf[eb][:], op=ALU.add),
                                      r=[("S", cur), ("sp", eb), ("splo", eb)], w=[("S", nxt)])

                    def st_o(t):
                        i = t["i"]; g = t["g"]; j = t["j"]; kb = t["kb"]; idx = t["idx"]
                        for hh in range(2):
                            h = 2 * j + hh
                            sc.op("pe", lambda e, hh=hh, h=h: e.matmul(ps[OB][hh * 64:(hh + 1) * 64, :], lhsT=vv[:, kb, h * 64:(h + 1) * 64],
                                                                       rhs=a_bf[i % NA][:, hh, :], start=(idx == 0), stop=t["last"]),
                                  r=[("vv", kb), ("a", i % NA), ("alo", i % NA)], w=[("ps", OB)])
                        if t["last"]:
                            oi = t["gj"] % 2
                            sc.op("dve", lambda e: e.tensor_copy(out=o_sb[oi][:], in_=ps[OB][:]),
                                  r=[("ps", OB)], w=[("o_sb", oi)])
                            ld("sp", sbT_v[:, j, g * 512:(g + 1) * 512], o_sb[oi][:], [("sbT_d", g)], r=[("o_sb", oi)])

                    xe_z = xe_d.rearrange("(n p) f -> n p f", p=128)
                    nzf = xe_z.shape[0]
                    zf_every = max(1, NT // nzf)
                    zf_done = [0]
                    st_q(0)
                    OLAG = 4
                    for step in range(NT + OLAG):
                        if step % zf_every == 0 and zf_done[0] < nzf:
                            ld("sp", xe_z[zf_done[0]], zt[:], ["xe_d"], r=["zt"])
                            zf_done[0] += 1
                        if step < NT and tiles[step]["idx"] == 0 and tiles[step]["j"] == 0 and tiles[step]["g"] + 1 < NG:
                            st_q(tiles[step]["g"] + 1)
                        diag = step < NT and (tiles[step]["kb"] - 4 * tiles[step]["g"] >= 0)
                        if step < NT:
                            st_z(tiles[step])
                            if not diag:
                                st_ln(tiles[step])
                        if 0 <= step - 1 < NT:
                            st_arg(tiles[step - 1])
                        if step < NT and diag:
                            st_ln(tiles[step])
                        if 0 <= step - 1 < NT:
                            st_dve(tiles[step - 1])
                        if 0 <= step - OLAG < NT:
                            st_o(tiles[step - OLAG])
                        for b_, fn_ in rsteps.pop(step, []):
                            fn_(b_)
                    while zf_done[0] < nzf:
                        ld("sp", xe_z[zf_done[0]], zt[:], ["xe_d"], r=["zt"])
                        zf_done[0] += 1
                    for step in sorted(rsteps):
                        for b_, fn_ in rsteps[step]:
                            fn_(b_)
            sc.barrier()

            with ExitStack() as stR:
                dest_i = sbt(stR, "dest_i", [128, NB, 2], I32)
                wts = sbt(stR, "wts", [128, NB, 2], F32)
                with ExitStack() as st:
                    wg = sbt(st, "wg", [128, 8, 2048], BF16)
                    w_v = w_in.rearrange("(k p) c -> p k c", p=128)
                    wbr = sbt(st, "wbr", [128, 4, D], BF16)
                    wbs = sbt(st, "wbs", [128, 4, D], BF16)
                    wo = sbt(st, "wo", [128, 8, D], BF16)
                    ld("pool", wbr[:], w_br_ret.rearrange("(k p) c -> p k c", p=128), ["wbr"])
                    for k in range(8):
                        ld("pool", wg[:, k, 0:1024], w_v[:, k, 3584:4608], [("wg0", k)])
                    ld("pool", wbs[:], w_br_sb.rearrange("(k p) c -> p k c", p=128), ["wbs"])
                    for k in range(8):
                        ld("pool", wg[:, k, 1024:2048], w_v[:, k, 4608:5632], [("wg1", k)])
                    ld("pool", wo[:], w_out.rearrange("(k p) c -> p k c", p=128), ["wo"])
                    WG = [[("wg0", k) for k in range(8)], [("wg1", k) for k in range(8)]]
                    wrt = sbt(st, "wrt", [128, 8, 20], F32)
                    ld("sp", wrt[:], w_rt.rearrange("(k p) c -> p k c", p=128), ["wrt"])
                    brt = sbt(st, "brt", [128, 20], F32)
                    ld("sp", brt[:], b_rt, ["brt"])
                    identf = sbt(st, "identf", [128, 128], F32)
                    ld("sp", identf[:], identf_d, ["identf"])
                    trilt = sbt(st, "trilt", [128, 128], BF16)
                    onesb = sbt(st, "onesb", [128, 128], BF16)
                    ld("sp", trilt[:], trilt_d, ["trilt"])
                    ld("sp", onesb[:], onesbf_d, ["onesb"])
                    ebase = sbt(st, "ebase_s", [128, 16], F32)
                    ld("sp", ebase[:], ebase_d, ["ebase"])
                    gffn_s = sbt(st, "gffn_s", [128, D], F32)
                    ld("sp", gffn_s[:], gffn, ["gffn"])
                    hTt = [sbt(st, "hTt%d" % i, [128, 8, 512], BF16) for i in range(2)]
                    retT_t = [sbt(st, "retT_t%d" % i, [128, 4, 512], BF16) for i in range(2)]
                    sbT_t = [sbt(st, "sbT_t%d" % i, [128, 4, 512], BF16) for i in range(2)]
                    mT = [sbt(st, "mT%d" % i, [128, 8, 512], BF16) for i in range(2)]
                    sg = [sbt(st, "sg%d" % i, [128, 512], F32) for i in range(2)]
                    m1 = [sbt(st, "m1_%d" % i, [128, 512], F32) for i in range(2)]
                    x1s = [sbt(st, "x1s%d" % i, [128, D], F32) for i in range(3)]
                    hnf2 = [sbt(st, "hnf%d" % i, [128, D], F32) for i in range(2)]
                    hnb = [sbt(st, "hnb%d" % i, [128, D], BF16) for i in range(2)]
                    hnT = sbt(st, "hnT", [128, 8, 128], F32)
                    ssq4 = sbt(st, "ssq4", [128, NB], F32)
                    rs4 = sbt(st, "rs4", [128, NB], F32)
                    lg = sbt(st, "lg", [128, 20], F32)
                    sm = sbt(st, "sm", [128, 16], F32)
                    ohg = sbt(st, "ohg", [128, 4], F32)
                    tmp16 = sbt(st, "tmp16", [128, 4, 4], F32)
                    ig = sbt(st, "ig", [128, 4], F32)
                    ig2 = sbt(st, "ig2", [128, 4], F32)
                    oh1 = sbt(st, "oh1", [128, 4], F32)
                    oh2 = sbt(st, "oh2", [128, 4], F32)
                    ohe1 = sbt(st, "ohe1", [128, 4, 4], F32)
                    ohe2 = sbt(st, "ohe2", [128, 4, 4], F32)
                    A_bf = sbt(st, "A_bf", [128, 16], BF16)
                    Acum = [sbt(st, "Acum%d" % i, [128, 16], BF16) for i in range(2)]
                    rk = sbt(st, "rk", [128, 16], F32)
                    destf = sbt(st, "destf", [128, 2], F32)
                    sc.op("pool", lambda e: e.memset(Acum[0][:], 0.0), w=[("Acum", 0)])
                    sbT_v = sbT_d.rearrange("(pr two) d s -> (two d) pr s", two=2)
                    pcnt = [0]

                    def p4_load(T):
                        t2 = T % 2
                        ld("sp", hTt[t2][:], hT_d[:, :, T * 512:(T + 1) * 512], [("hTt", t2)], r=[("hT_d", T)])
                        ld("sp", retT_t[t2][:], retT_d[:, :, T * 512:(T + 1) * 512], [("retT_t", t2)], r=[("retT_d", T)])
                        for pr in range(4):
                            ld("sp", sbT_t[t2][:, pr, :], sbT_v[:, pr, T * 512:(T + 1) * 512], [("sbT_t", t2)], r=[("sbT_d", T)])

                    def p4_c(T, c):
                        t2 = T % 2
                        HT = [("hTt", t2)]
                        for br in range(2):
                            wb_, src, ksrc, kw = (wbr, retT_t[t2], ("retT_t", t2), "wbr") if br == 0 else (wbs, sbT_t[t2], ("sbT_t", t2), "wbs")
                            pp = pcnt[0] % 3
                            pcnt[0] += 1
                            bb = 2 * pp
                            gb = 2 * pp + 1
                            for k in range(4):
                                sc.op("pe", lambda e, k=k, wb_=wb_, src=src, bb=bb: e.matmul(
                                    ps[bb][:], lhsT=wb_[:, k, c * 128:(c + 1) * 128], rhs=src[:, k, :],
                                    start=(k == 0), stop=(k == 3)), r=[kw, ksrc], w=[("ps", bb)])
                            for k in range(8):
                                sc.op("pe", lambda e, k=k, br=br, gb=gb: e.matmul(
                                    ps[gb][:], lhsT=wg[:, k, br * 1024 + c * 128: br * 1024 + (c + 1) * 128],
                                    rhs=hTt[t2][:, k, :], start=(k == 0), stop=(k == 7)),
                                    r=WG[br] + HT, w=[("ps", gb)])
                            sc.op("act", lambda e, br=br, gb=gb: e.activation(out=sg[br][:], in_=ps[gb][:], func=AF.Sigmoid),
                                  r=[("ps", gb)], w=[("sg", br)])
                            sc.op("dve", lambda e, br=br, bb=bb: e.tensor_tensor(out=m1[br][:], in0=ps[bb][:], in1=sg[br][:], op=ALU.mult),
                                  r=[("ps", bb), ("sg", br)], w=[("m1", br)])
                        sc.op("pool", lambda e: e.tensor_tensor(out=mT[t2][:, c, :], in0=m1[0][:], in1=m1[1][:], op=ALU.add),
                              r=[("m1", 0), ("m1", 1)], w=[("mT", t2)])

                    def p4_blk(T, i, part):
                        R = sc.op
                        if True:
                            t2 = T % 2
                            b = T * 4 + i
                            i2 = b % 3
                            if part == "A":
                                if b == 0:
                                    ld("sp", x1s[0][:], x[0:128, :], [("x1s", 0)])
                                if b + 1 < NB:
                                    ld("sp", x1s[(b + 1) % 3][:], x[(b + 1) * 128:(b + 2) * 128, :], [("x1s", (b + 1) % 3)])
                                for hf in range(2):
                                    bank = 6 + hf
                                    for c in range(8):
                                        sc.op("pe", lambda e, c=c, hf=hf, bank=bank: e.matmul(
                                            ps[bank][:], lhsT=mT[t2][:, c, i * 128:(i + 1) * 128], rhs=wo[:, c, hf * 512:(hf + 1) * 512],
                                            start=(c == 0), stop=(c == 7)), r=[("mT", t2), "wo"], w=[("ps", bank)])
                                    sc.op("dve", lambda e, hf=hf, bank=bank: e.tensor_tensor(
                                        out=x1s[i2][:, hf * 512:(hf + 1) * 512], in0=ps[bank][:], in1=x1s[i2][:, hf * 512:(hf + 1) * 512], op=ALU.add),
                                        r=[("ps", bank), ("x1s", i2)], w=[("x1s", i2)])
                                ld("sp", x1_d[b * 128:(b + 1) * 128, :], x1s[i2][:], [("x1_d", b)], r=[("x1s", i2)])
                                sc.op("act", lambda e, b=b, i2=i2: e.activation(out=junk[:], in_=x1s[i2][:], func=AF.Square,
                                                                               accum_out=ssq4[:, b:b + 1]),
                                      r=[("x1s", i2)], w=["junk", ("ssq4", b)])
                                rstd_ops(ssq4[:, b:b + 1], rs4[:, b:b + 1], ("ssq4", b), ("rs4", b))
                                sc.op("dve", lambda e, b=b, i2=i2: e.scalar_tensor_tensor(
                                    out=hnf2[b % 2][:], in0=x1s[i2][:], scalar=rs4[:, b:b + 1], in1=gffn_s[:], op0=ALU.mult, op1=ALU.mult),
                                    r=[("x1s", i2), ("rs4", b), "gffn"], w=[("hnf", b % 2)])
                                sc.op("act", lambda e, b=b: e.copy(out=hnb[b % 2][:], in_=hnf2[b % 2][:]), r=[("hnf", b % 2)], w=[("hnb", b % 2)])
                            if part == "B":
                                for half in range(2):
                                    bank = 6 + half
                                    for kk in range(4):
                                        k = half * 4 + kk
                                        sc.op("pe", lambda e, k=k, kk=kk, bank=bank: e.transpose(
                                            out=ps[bank][:, kk * 128:(kk + 1) * 128], in_=hnf2[b % 2][:, k * 128:(k + 1) * 128], identity=identf[:]),
                                            r=[("hnf", b % 2), "identf"], w=[("ps", bank)])
                                    evac(hnT[:, half * 4:(half + 1) * 4, :], ps[bank][:].rearrange("p (k t) -> p k t", k=4),
                                         r=[("ps", bank)], w=["hnT"])
                            if part == "C":
                                for k in range(8):
                                    sc.op("pe", lambda e, k=k: e.matmul(ps[6][:, 0:20], lhsT=hnT[:, k, :], rhs=wrt[:, k, :],
                                                                        start=(k == 0), stop=(k == 7)),
                                          r=["hnT", "wrt"], w=[("ps", 6)])
                                R("dve", lambda e: e.tensor_tensor(out=lg[:], in0=ps[6][:, 0:20], in1=brt[:], op=ALU.add),
                                  r=[("ps", 6), "brt"], w=["lg"])
                                R("dve", lambda e: e.tensor_reduce(out=sm[:, 0:1], in_=lg[:, 0:4], axis=AX.X, op=ALU.max), r=["lg"], w=["sm"])
                                R("dve", lambda e: e.tensor_scalar(out=ohg[:], in0=lg[:, 0:4], scalar1=sm[:, 0:1], scalar2=None, op0=ALU.is_equal),
                                  r=["lg", "sm"], w=["ohg"])
                                R("dve", lambda e: e.tensor_scalar(out=ig2[:], in0=lg[:, 0:4], scalar1=sm[:, 0:1], scalar2=None, op0=ALU.subtract),
                                  r=["lg", "sm"], w=["ig2"])
                                R("act", lambda e: e.activation(out=ig2[:], in_=ig2[:], func=AF.Sigmoid), r=["ig2"], w=["ig2"])
                                R("dve", lambda e: e.tensor_scalar(out=oh2[:], in0=ig2[:], scalar1=-1.0, scalar2=1.0, op0=ALU.mult, op1=ALU.add),
                                  r=["ig2"], w=["oh2"])
                                R("dve", lambda e: e.reciprocal(out=oh2[:], in_=oh2[:]), r=["oh2"], w=["oh2"])
                                R("dve", lambda e: e.tensor_tensor(out=ig2[:], in0=ig2[:], in1=oh2[:], op=ALU.mult), r=["ig2", "oh2"], w=["ig2"])
                                R("dve", lambda e: e.tensor_reduce(out=sm[:, 2:3], in_=ig2[:], axis=AX.X, op=ALU.add), r=["ig2"], w=["sm"])
                                R("dve", lambda e: e.reciprocal(out=sm[:, 3:4], in_=sm[:, 2:3]), r=["sm"], w=["sm"])
                                R("dve", lambda e: e.tensor_tensor(out=tmp16[:], in0=lg[:, 4:20].rearrange("p (g j) -> p g j", g=4),
                                                                   in1=ohg[:].unsqueeze(2).broadcast_to([128, 4, 4]), op=ALU.mult),
                                  r=["lg", "ohg"], w=["tmp16"])
                                R("dve", lambda e: e.tensor_reduce(out=ig[:], in_=tmp16[:].rearrange("p g j -> p j g"), axis=AX.X, op=ALU.add),
                                  r=["tmp16"], w=["ig"])
                                R("dve", lambda e: e.tensor_reduce(out=sm[:, 4:5], in_=ig[:], axis=AX.X, op=ALU.max), r=["ig"], w=["sm"])
                                R("dve", lambda e: e.tensor_scalar(out=oh1[:], in0=ig[:], scalar1=sm[:, 4:5], scalar2=None, op0=ALU.is_equal),
                                  r=["ig", "sm"], w=["oh1"])
                                R("dve", lambda e: e.scalar_tensor_tensor(out=ig2[:], in0=oh1[:], scalar=-1e30, in1=ig[:], op0=ALU.mult, op1=ALU.add),
                                  r=["oh1", "ig"], w=["ig2"])
                                R("dve", lambda e: e.tensor_reduce(out=sm[:, 5:6], in_=ig2[:], axis=AX.X, op=ALU.max), r=["ig2"], w=["sm"])
                                R("dve", lambda e: e.tensor_scalar(out=oh2[:], in0=ig2[:], scalar1=sm[:, 5:6], scalar2=None, op0=ALU.is_equal),
                                  r=["ig2", "sm"], w=["oh2"])
                                R("dve", lambda e: e.tensor_tensor(out=sm[:, 6:7], in0=sm[:, 4:5], in1=sm[:, 5:6], op=ALU.subtract), r=["sm"], w=["sm"])
                                R("act", lambda e: e.activation(out=sm[:, 7:8], in_=sm[:, 6:7], func=AF.Sigmoid), r=["sm"], w=["sm"])
                                R("dve", lambda e: e.tensor_scalar(out=sm[:, 8:9], in0=sm[:, 7:8], scalar1=-1.0, scalar2=1.0, op0=ALU.mult, op1=ALU.add),
                                  r=["sm"], w=["sm"])
                                R("dve", lambda e, b=b: e.tensor_tensor(out=wts[:, b, 0:1], in0=sm[:, 7:8], in1=sm[:, 3:4], op=ALU.mult),
                                  r=["sm"], w=[("wts", b)])
                                R("dve", lambda e, b=b: e.tensor_tensor(out=wts[:, b, 1:2], in0=sm[:, 8:9], in1=sm[:, 3:4], op=ALU.mult),
                                  r=["sm", ("wts", b)], w=[("wts", b)])
                                R("dve", lambda e: e.tensor_tensor(out=ohe1[:], in0=ohg[:].unsqueeze(2).broadcast_to([128, 4, 4]),
                                                                   in1=oh1[:].unsqueeze(1).broadcast_to([128, 4, 4]), op=ALU.mult),
                                  r=["ohg", "oh1"], w=["ohe1"])
                                R("dve", lambda e: e.tensor_tensor(out=ohe2[:], in0=ohg[:].unsqueeze(2).broadcast_to([128, 4, 4]),
                                                                   in1=oh2[:].unsqueeze(1).broadcast_to([128, 4, 4]), op=ALU.mult),
                                  r=["ohg", "oh2"], w=["ohe2"])
                                R("dve", lambda e: e.tensor_tensor(out=A_bf[:], in0=ohe1[:].rearrange("p g j -> p (g j)"),
                                                                   in1=ohe2[:].rearrange("p g j -> p (g j)"), op=ALU.add),
                                  r=["ohe1", "ohe2"], w=["A_bf"])
                            if part == "D":
                                ac = b % 2
                                R("pe", lambda e: e.matmul(ps[7][:, 0:16], lhsT=trilt[:], rhs=A_bf[:], start=True, stop=False),
                                  r=["trilt", "A_bf"], w=[("ps", 7)])
                                R("pe", lambda e, ac=ac: e.matmul(ps[7][:, 0:16], lhsT=onesb[:], rhs=Acum[ac][:], start=False, stop=True),
                                  r=["onesb", ("Acum", ac)], w=[("ps", 7)])
                                R("pool", lambda e, ac=ac: e.tensor_tensor(out=Acum[1 - ac][:], in0=Acum[ac][:], in1=A_bf[:], op=ALU.add),
                                  r=[("Acum", ac), "A_bf"], w=[("Acum", 1 - ac)])
                                R("dve", lambda e: e.tensor_tensor(out=rk[:], in0=ps[7][:, 0:16], in1=ebase[:], op=ALU.add),
                                  r=[("ps", 7), "ebase"], w=["rk"])
                                for kk, oh in enumerate((ohe1, ohe2)):
                                    R("dve", lambda e, oh=oh: e.tensor_tensor(out=tmp16[:].rearrange("p g j -> p (g j)"), in0=rk[:],
                                                                             in1=oh[:].rearrange("p g j -> p (g j)"), op=ALU.mult),
                                      r=["rk", "ohe1", "ohe2"], w=["tmp16"])
                                    R("dve", lambda e, kk=kk: e.tensor_reduce(out=destf[:, kk:kk + 1], in_=tmp16[:].rearrange("p g j -> p (g j)"),
                                                                             axis=AX.X, op=ALU.add), r=["tmp16"], w=["destf"])
                                R("dve", lambda e, b=b: e.tensor_copy(out=dest_i[:, b, :], in_=destf[:]), r=["destf"], w=[("dest", b)])
                                for kk in range(2):
                                    sc.dma("pool", lambda e, b=b, kk=kk, i2=i2: e.indirect_dma_start(
                                        out=xe_d[:, :], out_offset=bass.IndirectOffsetOnAxis(ap=dest_i[:, b, kk:kk + 1], axis=0),
                                        in_=hnb[b % 2][:], in_offset=None), r=[("dest", b), ("hnb", b % 2)], w=["xe_d"])

                    sched = {0: ["A0"], 1: ["B0", "A1"], 2: ["C0", "B1"], 3: ["D0", "C1", "A2"], 4: ["D1", "B2"],
                             5: ["C2", "A3"], 6: ["D2", "B3"], 7: ["C3"]}
                    p4_load(0)
                    for T in range(NG + 1):
                        if T + 1 < NG:
                            p4_load(T + 1)
                        for c in range(8):
                            if T < NG:
                                p4_c(T, c)
                            if T >= 2 and c == 0:
                                p4_blk(T - 2, 3, "D")
                            if T >= 1:
                                for it in sched[c]:
                                    p4_blk(T - 1, int(it[1]), it[0])
                    p4_blk(NG - 1, 3, "D")
                    if debug:
                        ld("sp", rt_d[:, :, 0:2], wts[:], ["rt_d"], r=[("wts", b) for b in range(NB)])
                sc.barrier()

                wpg = sbt(stR, "wpg", [128, 8, D], BF16)
                wpl = sbt(stR, "wpl", [128, 2, D], BF16)
                gple_s = sbt(stR, "gple_s", [128, D], F32)
                gfin_s = sbt(stR, "gfin_s", [128, D], F32)
                with ExitStack() as st:
                    SLT = 384 if C % 384 == 0 else 512
                    NSB = SLT // 128
                    NS = C // SLT
                    wge = [sbt(st, "wge%d" % i, [128, 8, 512], BF16) for i in range(2)]
                    wue = [sbt(st, "wue%d" % i, [128, 8, 512], BF16) for i in range(2)]
                    wde = [sbt(st, "wde%d" % i, [128, 4, D], BF16) for i in range(2)]
                    xe_t = [sbt(st, "xe_t%d" % i, [128, NSB, D], BF16) for i in range(3)]
                    xeT = [sbt(st, "xeT%d" % i, [128, 8, SLT], BF16) for i in range(2)]
                    sgl = [sbt(st, "sgl%d" % i, [128, SLT], BF16) for i in range(2)]
                    hid = [sbt(st, "hid%d" % i, [128, 4, SLT], BF16) for i in range(2)]
                    y_sb = [sbt(st, "y_sb%d" % i, [128, D], F32) for i in range(3)]
                    tl = [(ex, s_) for ex in range(16) for s_ in range(NS)]
                    cnt6 = [0]

                    def e_s0(n):
                        ex, s_ = tl[n]
                        e2 = ex % 2
                        if n == 3:
                            ld("pool", wpg[:], w_pg.rearrange("(k p) c -> p k c", p=128), ["wpg"])
                            ld("pool", wpl[:], w_ple.rearrange("(k p) c -> p k c", p=128), ["wpl"])
                            ld("sp", gple_s[:], gple, ["gple"])
                            ld("sp", gfin_s[:], gfin, ["gfin"])
                        if s_ == 0:
                            ld("pool", wge[e2][:], w_gate[ex].rearrange("(k p) c -> p k c", p=128), [("wge", e2)])
                            ld("pool", wue[e2][:], w_up[ex].rearrange("(k p) c -> p k c", p=128), [("wue", e2)])
                            ld("pool", wde[e2][:], w_down[ex].rearrange("(k p) c -> p k c", p=128), [("wde", e2)])
                        r0 = ex * C + s_ * SLT
                        ld("sp", xe_t[n % 3][:], xe_d[r0:r0 + SLT, :].rearrange("(i p) f -> p i f", p=128), [("xe_t", n % 3)], r=["xe_d"])

                    def e_s1(n):
                        for i in range(NSB):
                            bank = i % 2
                            pb = ps[bank].bitcast(BF16)
                            for k in range(8):
                                sc.op("pe", lambda e, k=k, i=i, pb=pb: e.transpose(
                                    out=pb[:, k * 128:(k + 1) * 128], in_=xe_t[n % 3][:, i, k * 128:(k + 1) * 128], identity=ident[:]),
                                    r=[("xe_t", n % 3), "ident"], w=[("ps", bank)])
                            evac(xeT[n % 2][:, :, i * 128:(i + 1) * 128], pb.rearrange("p (k t) -> p k t", k=8), r=[("ps", bank)], w=[("xeT", n % 2)])

                    def e_s2(n):
                        ex, s_ = tl[n]
                        e2 = ex % 2
                        for c in range(4):
                            gb = 2 + (c % 2) * 2
                            ub = gb + 1
                            for k in range(8):
                                sc.op("pe", lambda e, k=k, c=c, gb=gb: e.matmul(
                                    ps[gb][:, 0:SLT], lhsT=wge[e2][:, k, c * 128:(c + 1) * 128], rhs=xeT[n % 2][:, k, :],
                                    start=(k == 0), stop=(k == 7)), r=[("wge", e2), ("xeT", n % 2)], w=[("ps", gb)])
                            for k in range(8):
                                sc.op("pe", lambda e, k=k, c=c, ub=ub: e.matmul(
                                    ps[ub][:, 0:SLT], lhsT=wue[e2][:, k, c * 128:(c + 1) * 128], rhs=xeT[n % 2][:, k, :],
                                    start=(k == 0), stop=(k == 7)), r=[("wue", e2), ("xeT", n % 2)], w=[("ps", ub)])
                            sc.op("act", lambda e, c=c, gb=gb: e.activation(out=sgl[c % 2][:], in_=ps[gb][:, 0:SLT], func=AF.Silu),
                                  r=[("ps", gb)], w=[("sgl", c % 2)])
                            sc.op("dve", lambda e, c=c, ub=ub: e.tensor_tensor(out=hid[n % 2][:, c, :], in0=ps[ub][:, 0:SLT], in1=sgl[c % 2][:], op=ALU.mult),
                                  r=[("ps", ub), ("sgl", c % 2)], w=[("hid", n % 2)])

                    def e_s3(n):
                        ex, s_ = tl[n]
                        e2 = ex % 2
                        r0 = ex * C + s_ * SLT
                        for i in range(NSB):
                            y2 = cnt6[0] % 3
                            cnt6[0] += 1
                            for hf in range(2):
                                bank = 6 + hf
                                for c in range(4):
                                    sc.op("pe", lambda e, c=c, i=i, hf=hf, bank=bank: e.matmul(
                                        ps[bank][:], lhsT=hid[n % 2][:, c, i * 128:(i + 1) * 128], rhs=wde[e2][:, c, hf * 512:(hf + 1) * 512],
                                        start=(c == 0), stop=(c == 3)), r=[("hid", n % 2), ("wde", e2)], w=[("ps", bank)])
                                evac(y_sb[y2][:, hf * 512:(hf + 1) * 512], ps[bank][:], r=[("ps", bank)], w=[("y_sb", y2)],
                                     eng=("act" if hf == 0 else "dve"))
                            ld("sp", ye_d[r0 + i * 128:r0 + (i + 1) * 128, :], y_sb[y2][:], ["ye_d"], r=[("y_sb", y2)])

                    stg = [e_s0, e_s1, e_s2, e_s3]
                    assert NS >= 2
                    for step in range(len(tl) + len(stg) - 1):
                        for si in reversed(range(len(stg))):
                            n = step - si
                            if 0 <= n < len(tl):
                                stg[si](n)
                sc.barrier()

                with ExitStack() as st:
                    NX = 6
                    x1b = [sbt(st, "x1b%d" % i, [128, D], F32) for i in range(NX)]
                    y1 = [sbt(st, "y1_%d" % i, [128, D], F32) for i in range(3)]
                    y2b = [sbt(st, "y2_%d" % i, [128, D], F32) for i in range(3)]
                    hpb = [sbt(st, "hpb%d" % i, [128, D], BF16) for i in range(2)]
                    hpT = [sbt(st, "hpT%d" % i, [128, 8, 128], BF16) for i in range(2)]
                    pbf = [sbt(st, "pbf%d" % i, [128, 256], BF16) for i in range(4)]
                    pT = [sbt(st, "pT%d" % i, [128, 2, 128], BF16) for i in range(2)]
                    sig = [sbt(st, "sig%d" % i, [128, D], F32) for i in range(2)]
                    tmpx = [sbt(st, "tmpx%d" % i, [128, D], F32) for i in range(2)]
                    ob = [sbt(st, "ob%d" % i, [128, D], F32) for i in range(2)]
                    ssq7 = sbt(st, "ssq7", [128, NB, 2], F32)
                    rs7 = sbt(st, "rs7", [128, NB, 2], F32)

                    def f_s0(b):
                        ld("sp", x1b[b % NX][:], x1_d[b * 128:(b + 1) * 128, :], [("x1b", b % NX)], r=[("x1_d", b)])
                        ld("pool", pbf[b % 4][:], p_in[b * 128:(b + 1) * 128, :], [("pbf", b % 4)])
                        for kk, yb in enumerate((y1, y2b)):
                            sc.dma("pool", lambda e, kk=kk, yb=yb: e.indirect_dma_start(
                                out=yb[b % 3][:], out_offset=None, in_=ye_d[:, :],
                                in_offset=bass.IndirectOffsetOnAxis(ap=dest_i[:, b, kk:kk + 1], axis=0)),
                                r=["ye_d", ("dest", b)], w=[("y%d" % kk, b % 3)])

                    def f_s1(b):
                        xx = x1b[b % NX]
                        for kk, yb in enumerate((y1, y2b)):
                            sc.op("dve", lambda e, kk=kk, yb=yb: e.scalar_tensor_tensor(
                                out=xx[:], in0=yb[b % 3][:], scalar=wts[:, b, kk:kk + 1], in1=xx[:], op0=ALU.mult, op1=ALU.add),
                                r=[("y%d" % kk, b % 3), ("wts", b), ("x1b", b % NX)], w=[("x1b", b % NX)])
                        if debug:
                            ld("sp", x2_d[b * 128:(b + 1) * 128, :], xx[:], ["x2_d"], r=[("x1b", b % NX)])
                        sc.op("act", lambda e: e.activation(out=junk[:], in_=xx[:], func=AF.Square, accum_out=ssq7[:, b, 0:1]),
                              r=[("x1b", b % NX)], w=["junk", ("ssq7", b)])
                        rstd_ops(ssq7[:, b, 0:1], rs7[:, b, 0:1], ("ssq7", b), ("rs7", b))
                        sc.op("dve", lambda e: e.scalar_tensor_tensor(
                            out=hpb[b % 2][:], in0=xx[:], scalar=rs7[:, b, 0:1], in1=gple_s[:], op0=ALU.mult, op1=ALU.mult),
                            r=[("x1b", b % NX), ("rs7", b), "gple"], w=[("hpb", b % 2)])

                    def f_s2(b):
                        i2 = b % 2
                        pb = ps[0].bitcast(BF16)
                        for k in range(8):
                            sc.op("pe", lambda e, k=k, pb=pb: e.transpose(
                                out=pb[:, k * 128:(k + 1) * 128], in_=hpb[i2][:, k * 128:(k + 1) * 128], identity=ident[:]),
                                r=[("hpb", i2), "ident"], w=[("ps", 0)])
                        evac(hpT[i2][:], pb.rearrange("p (k t) -> p k t", k=8), r=[("ps", 0)], w=[("hpT", i2)], eng="act")
                        pb1 = ps[1].bitcast(BF16)
                        for k in range(2):
                            sc.op("pe", lambda e, k=k, pb1=pb1: e.transpose(
                                out=pb1[:, k * 128:(k + 1) * 128], in_=pbf[b % 4][:, k * 128:(k + 1) * 128], identity=ident[:]),
                                r=[("pbf", b % 4), "ident"], w=[("ps", 1)])
                        evac(pT[i2][:], pb1[:, 0:256].rearrange("p (k t) -> p k t", k=2), r=[("ps", 1)], w=[("pT", i2)], eng="act")

                    def f_s3(b):
                        i2 = b % 2
                        for hf in range(2):
                            gbk = (2 if b % 2 == 0 else 6) + hf
                            pbk = 4 + hf
                            for k in range(8):
                                sc.op("pe", lambda e, k=k, hf=hf, gbk=gbk: e.matmul(
                                    ps[gbk][:], lhsT=hpT[i2][:, k, :], rhs=wpg[:, k, hf * 512:(hf + 1) * 512],
                                    start=(k == 0), stop=(k == 7)), r=[("hpT", i2), "wpg"], w=[("ps", gbk)])
                            for k in range(2):
                                sc.op("pe", lambda e, k=k, hf=hf, pbk=pbk: e.matmul(
                                    ps[pbk][:], lhsT=pT[i2][:, k, :], rhs=wpl[:, k, hf * 512:(hf + 1) * 512],
                                    start=(k == 0), stop=(k == 1)), r=[("pT", i2), "wpl"], w=[("ps", pbk)])
                            sc.op("act", lambda e, hf=hf, gbk=gbk: e.activation(
                                out=sig[i2][:, hf * 512:(hf + 1) * 512], in_=ps[gbk][:], func=AF.Sigmoid),
                                r=[("ps", gbk)], w=[("sig", i2)])
                            sc.op("dve", lambda e, hf=hf, pbk=pbk: e.tensor_tensor(
                                out=tmpx[i2][:, hf * 512:(hf + 1) * 512], in0=ps[pbk][:], in1=sig[i2][:, hf * 512:(hf + 1) * 512], op=ALU.mult),
                                r=[("ps", pbk), ("sig", i2)], w=[("tmpx", i2)])

                    def f_s4(b):
                        i2 = b % 2
                        sc.op("pool", lambda e: e.tensor_tensor(out=tmpx[i2][:], in0=tmpx[i2][:], in1=x1b[b % NX][:], op=ALU.add),
                              r=[("tmpx", i2), ("x1b", b % NX)], w=[("tmpx", i2)])
                        sc.op("act", lambda e: e.activation(out=junk[:], in_=tmpx[i2][:], func=AF.Square, accum_out=ssq7[:, b, 1:2]),
                              r=[("tmpx", i2)], w=["junk", ("ssq7b", b)])
                        rstd_ops(ssq7[:, b, 1:2], rs7[:, b, 1:2], ("ssq7b", b), ("rs7b", b))
                        sc.op("dve", lambda e: e.scalar_tensor_tensor(
                            out=ob[i2][:], in0=tmpx[i2][:], scalar=rs7[:, b, 1:2], in1=gfin_s[:], op0=ALU.mult, op1=ALU.mult),
                            r=[("tmpx", i2), ("rs7b", b), "gfin"], w=[("ob", i2)])
                        ld("sp", out_d[b * 128:(b + 1) * 128, :], ob[i2][:], ["out_d"], r=[("ob", i2)])

                    stg = [(f_s0, 0), (f_s1, 2), (f_s2, 3), (f_s3, 4), (f_s4, 5)]
                    for step in range(NB + 5):
                        for f, off in stg:
                            b = step - off
                            if 0 <= b < NB:
                                f(b)
        stats = sc.emit()
        print("stats", stats)
    return nc


S_FULL = 4096
C_CAP = 768
_NC_CACHE = {}


def kernel(x, p, norm_mix, w_in, ret_gn, w_br_ret, w_br_sb, w_out, norm_ffn,
           w_grp, b_grp, w_exp, b_exp, w_gate, w_up, w_down,
           norm_ple, w_ple, w_ple_gate, norm_final):
    f = lambda a: np.ascontiguousarray(np.asarray(a, dtype=np.float32))
    x = f(x); p = f(p)
    B = x.shape[0]
    if "nc" not in _NC_CACHE:
        _NC_CACHE["nc"] = build(S_FULL, C_CAP, debug=False)
    nc = _NC_CACHE["nc"]
    shared = {
        "w_in": f(w_in)[0], "w_br_ret": f(w_br_ret)[0], "w_br_sb": f(w_br_sb)[0], "w_out": f(w_out)[0],
        "w_rt": np.ascontiguousarray(np.concatenate([f(w_grp)[0], f(w_exp)[0]], axis=1)),
        "b_rt": rep(np.concatenate([f(b_grp)[0], f(b_exp)[0]])),
        "w_gate": f(w_gate)[0], "w_up": f(w_up)[0], "w_down": f(w_down)[0],
        "w_ple": f(w_ple)[0], "w_ple_gate": f(w_ple_gate)[0],
        "gmix": rep(f(norm_mix)[0]), "gffn": rep(f(norm_ffn)[0]), "gple": rep(f(norm_ple)[0]),
        "gfin": rep(f(norm_final)), "ggn": rep(f(ret_gn)[0]),
    }
    cst = consts_np(S_FULL, C_CAP)
    cst.pop("cdec")
    shared.update({k: np.ascontiguousarray(v) for k, v in cst.items()})
    in_maps = []
    for b in range(B):
        m = dict(shared)
        m["x"] = x[b]
        m["p"] = p[0, b]
        in_maps.append(m)
    res = run_bass_kernel_spmd(nc, in_maps, core_ids=list(range(B)))
    return np.stack([np.asarray(r["out"], dtype=np.float32) for r in res.results], axis=0)
```

```python
import numpy as np
from contextlib import ExitStack
import ml_dtypes
from concourse.bass_utils import run_bass_kernel_spmd
import concourse.bass as bass
import concourse.mybir as mybir

F32 = mybir.dt.float32
BF16 = mybir.dt.bfloat16
I32 = mybir.dt.int32
AF = mybir.ActivationFunctionType
ALU = mybir.AluOpType
AX = mybir.AxisListType


class Sched:
    RING = 12

    def __init__(self, nc, stack):
        self.nc = nc
        self.eng = dict(pe=nc.tensor, act=nc.scalar, dve=nc.vector, pool=nc.gpsimd, sp=nc.sync)
        self.ops = []
        self.sem = {e: stack.enter_context(nc.semaphore("sem_" + e)) for e in self.eng}
        self.rings = {
            q: [stack.enter_context(nc.semaphore("ring_%s_%d" % (q, i))) for i in range(self.RING)]
            for q in ("sp", "pool")
        }

    def op(self, eng, fn, r=(), w=()):
        self.ops.append((eng, fn, tuple(r), tuple(w), False))

    def dma(self, q, fn, r=(), w=()):
        self.ops.append((q, fn, tuple(r), tuple(w), True))

    def barrier(self):
        self.ops.append(("barrier", None, (), (), False))

    def emit(self):
        ops = self.ops
        n = len(ops)
        last_w = {}
        readers = {}
        deps = [None] * n
        need_sig = [False] * n
        last_on = {}
        for i, (e, fn, r, w, isd) in enumerate(ops):
            if e == "barrier":
                for x, j in last_on.items():
                    need_sig[j] = True
                deps[i] = set()
                continue
            d = set()
            for k in r:
                if k in last_w:
                    d.update(last_w[k])
            for k in w:
                if k in last_w:
                    d.update(last_w[k])
                d.update(readers.get(k, ()))
            d.discard(i)
            best = {}
            dd = set()
            for j in d:
                ej, _, _, _, isdj = ops[j]
                if isdj:
                    dd.add(j)
                else:
                    if ej not in best or best[ej] < j:
                        best[ej] = j
            for ej, j in best.items():
                if ej == "pe" and e == "pe" and not isd:
                    continue
                dd.add(j)
            deps[i] = dd
            for j in dd:
                need_sig[j] = True
            for k in w:
                if isd and k in last_w and all(ops[j][4] for j in last_w[k]) and not readers.get(k):
                    last_w[k] = last_w[k] + [i]
                else:
                    last_w[k] = [i]
                readers[k] = []
            for k in r:
                if k not in w:
                    readers.setdefault(k, []).append(i)
            if not isd:
                last_on[e] = i
        cnt = {e: 0 for e in self.eng}
        sig = [None] * n
        waited = {e: {} for e in self.eng}
        dma_n = {q: 0 for q in self.rings}
        ring_last = {}
        nwaits = 0

        def wait(e, sem, val):
            nonlocal nwaits
            key = id(sem)
            if waited[e].get(key, 0) >= val:
                return
            waited[e][key] = val
            self.eng[e].wait_ge(sem, val)
            nwaits += 1

        for i, (e, fn, r, w, isd) in enumerate(ops):
            if e == "barrier":
                for x in self.eng:
                    for y in self.eng:
                        if cnt[y] > 0:
                            wait(x, self.sem[y], cnt[y])
                    for q in self.rings:
                        for s_, v_ in ring_last.get(q, {}).values():
                            wait(x, s_, v_)
                continue
            for j in sorted(deps[i]):
                s_, v_ = sig[j]
                wait(e, s_, v_)
            if isd:
                m = dma_n[e]
                dma_n[e] += 1
                s_ = self.rings[e][m % self.RING]
                v_ = 16 * (m // self.RING + 1)
                if v_ > 16:
                    wait(e, s_, v_ - 16)
                inst = fn(self.eng[e])
                inst.then_inc(s_, 16)
                sig[i] = (s_, v_)
                ring_last.setdefault(e, {})[m % self.RING] = (s_, v_)
            else:
                inst = fn(self.eng[e])
                if need_sig[i]:
                    cnt[e] += 1
                    inst.then_inc(self.sem[e], 1)
                    sig[i] = (self.sem[e], cnt[e])
        for q in self.rings:
            for s_, v_ in ring_last.get(q, {}).values():
                wait("sp", s_, v_)
        self.stats = dict(n_ops=n, n_waits=nwaits, cnt=dict(cnt), dma=dict(dma_n))
        return self.stats


D = 1024
EPS = 1e-6
BF = ml_dtypes.bfloat16


def consts_np(S, C):
    NB = S // 128
    c = {}
    c["ident_bf"] = np.eye(128, dtype=np.float32).astype(BF)
    c["ident_f"] = np.eye(128, dtype=np.float32)
    j = np.arange(128)[:, None]
    s = np.arange(128)[None, :]
    c["ntri"] = np.where(j >= s, -1.0, 0.0).astype(np.float32).astype(BF)
    c["nones"] = (-np.ones((128, 128), np.float32)).astype(BF)
    c["tri_lt"] = np.where(j < s, 1.0, 0.0).astype(np.float32).astype(BF)
    c["ones_bf"] = np.ones((128, 128), np.float32).astype(BF)
    m = np.zeros((4, 128, 512), np.float32)
    for jd in range(4):
        key = jd * 128 + np.arange(128)[:, None]
        tq = np.arange(512)[None, :]
        m[jd] = (key < tq).astype(np.float32)
    c["mask01"] = ((m.transpose(1, 0, 2) - 1.0) * 30000.0).astype(np.float32).copy()
    pos = np.arange(S, dtype=np.float32)
    inv_freq = (10000.0 ** (-np.arange(0, 128, 2, dtype=np.float32) / 128)).astype(np.float32)
    ang = pos[:, None] * inv_freq[None, :]
    c["cos_t"] = np.cos(ang).astype(np.float32).reshape(NB, 128, 64).transpose(1, 0, 2).copy()
    c["sin_t"] = np.sin(ang).astype(np.float32).reshape(NB, 128, 64).transpose(1, 0, 2).copy()
    gamma = 1.0 - np.exp2(-5.0 - np.arange(4, dtype=np.float64))
    lg = np.log(gamma)
    jj = np.arange(128)[:, None]
    ii = np.arange(128)[None, :]
    allowed = (jj // 64) <= (ii // 64)
    sc = 128.0 ** -0.5
    Dp = np.zeros((128, 4, 128), np.float64)
    for h in range(4):
        Dp[:, h, :] = np.where(allowed, np.exp(lg[h] * (np.abs(ii - jj) - (ii + 1.0))), 0.0) * sc
    c["Dp"] = Dp.astype(np.float32)
    qdec = np.exp(lg[None, :] * (np.arange(128)[:, None] + 1.0))
    c["qdec"] = np.repeat(qdec[:, :, None], 128, axis=2).astype(np.float32)
    kdec = np.exp(lg[None, :] * (127.0 - np.arange(128)[:, None])) * sc
    c["kdec"] = np.repeat(kdec[:, :, None], 128, axis=2).astype(np.float32)
    c["cdec"] = np.exp(lg * 128.0)
    c["ebase"] = np.tile((np.arange(16, dtype=np.float32) * C)[None], (128, 1))
    return c


def rep(v):
    return np.tile(np.asarray(v, np.float32).reshape(1, -1), (128, 1))


def build(S, C, debug=False):
    NB = S // 128
    NG = S // 512
    cst = consts_np(S, C)
    cdec = [float(v) for v in cst["cdec"]]
    nc = bass.Bass("TRN2", target_bir_lowering=False)

    def din(name, shape, dt=F32):
        return nc.dram_tensor(name, list(shape), dt, kind="ExternalInput").ap()

    def dscr(name, shape, dt):
        return nc.dram_tensor(name, list(shape), dt, kind="ExternalOutput" if debug else "Internal").ap()

    x = din("x", [S, D])
    p_in = din("p", [S, 256])
    w_in = din("w_in", [D, 5632])
    w_br_ret = din("w_br_ret", [512, D])
    w_br_sb = din("w_br_sb", [512, D])
    w_out = din("w_out", [D, D])
    w_rt = din("w_rt", [D, 20])
    b_rt = din("b_rt", [128, 20])
    w_gate = din("w_gate", [16, D, 512])
    w_up = din("w_up", [16, D, 512])
    w_down = din("w_down", [16, 512, D])
    w_ple = din("w_ple", [256, D])
    w_pg = din("w_ple_gate", [D, D])
    gmix = din("gmix", [128, D])
    gffn = din("gffn", [128, D])
    gple = din("gple", [128, D])
    gfin = din("gfin", [128, D])
    ggn = din("ggn", [128, 512])
    ident_d = din("ident_bf", [128, 128], BF16)
    identf_d = din("ident_f", [128, 128])
    ntri_d = din("ntri", [128, 128], BF16)
    nones_d = din("nones", [128, 128], BF16)
    trilt_d = din("tri_lt", [128, 128], BF16)
    onesbf_d = din("ones_bf", [128, 128], BF16)
    mask01_d = din("mask01", [128, 4, 512])
    cos_d = din("cos_t", [128, NB, 64])
    sin_d = din("sin_t", [128, NB, 64])
    Dp_d = din("Dp", [128, 4, 128])
    qdec_d = din("qdec", [128, 4, 128])
    kdec_d = din("kdec", [128, 4, 128])
    ebase_d = din("ebase", [128, 16])

    out_d = nc.dram_tensor("out", [S, D], F32, kind="ExternalOutput").ap()
    sbT_d = dscr("sbT", [8, 64, S], BF16)
    retT_d = dscr("retT", [128, 4, S], BF16)
    x1_d = dscr("x1", [S, D], F32)
    xe_d = dscr("xe", [16 * C, D], BF16)
    ye_d = dscr("ye", [16 * C, D], F32)
    if debug:
        rt_d = nc.dram_tensor("rt_dbg", [128, NB, 4], F32, kind="ExternalOutput").ap()
        x2_d = nc.dram_tensor("x2_dbg", [S, D], F32, kind="ExternalOutput").ap()

    with ExitStack() as st0:
        sc = Sched(nc, st0)
        psr = [st0.enter_context(nc.psum_tensor("psr%d" % i, [128, 1024], F32)) for i in range(4)]
        ps = [psr[i // 2][:, (i % 2) * 512:(i % 2 + 1) * 512] for i in range(8)]
        cpc = [0]

        def evac(out, in_, r, w, eng=None):
            if eng is None:
                eng = "act" if cpc[0] % 2 == 0 else "dve"
                cpc[0] += 1
            if eng == "act":
                sc.op("act", lambda e: e.copy(out=out, in_=in_), r=r, w=w)
            else:
                sc.op("dve", lambda e: e.tensor_copy(out=out, in_=in_), r=r, w=w)

        def ld(q, out, in_, w, r=()):
            sc.dma(q, lambda e: e.dma_start(out=out, in_=in_), r=r, w=w)

        def sbt(stk, name, shape, dt):
            return stk.enter_context(nc.sbuf_tensor(name, list(shape), dt))

        def rstd_ops(ssq_ap, rs_ap, kssq, krs, n=D):
            sc.op("dve", lambda e: e.tensor_scalar(out=rs_ap, in0=ssq_ap, scalar1=1.0 / n, scalar2=EPS,
                                                   op0=ALU.mult, op1=ALU.add), r=[kssq], w=[krs])
            pow_ops(rs_ap, krs)

        def pow_ops(rs_ap, krs):
            sc.op("pool", lambda e: e.tensor_tensor(out=rs_ap, in0=rs_ap, in1=nhalf[:, 0:rs_ap.shape[-1]], op=ALU.pow),
                  r=[krs, "nhalf"], w=[krs])

        ident = sbt(st0, "ident", [128, 128], BF16)
        ld("sp", ident[:], ident_d, ["ident"])
        junk = sbt(st0, "junk", [128, D], BF16)
        nhalf = sbt(st0, "nhalf", [128, 8], F32)
        sc.op("pool", lambda e: e.memset(nhalf[:], -0.5), w=["nhalf"])

        hT_d = dscr("hT_scr", [128, 8, S], BF16)
        qT_d = dscr("qT_scr", [128, 4, S], BF16)
        with ExitStack() as stH:
            with ExitStack() as st:
                ntri = sbt(st, "ntri_s", [128, 128], BF16)
                nones = sbt(st, "nones_s", [128, 128], BF16)
                mask01 = sbt(st, "mask01_s", [128, 4, 512], BF16)
                ld("sp", ntri[:], ntri_d, ["ntri"])
                ld("sp", nones[:], nones_d, ["nones"])
                ld("pool", mask01[:], mask01_d, ["mask01"])
                kT = sbt(st, "kT", [128, 4, S], BF16)
                vv = sbt(st, "vv", [128, NB, 512], BF16)
                qg = [sbt(st, "qg%d" % i, [128, 4, 512], BF16) for i in range(2)]
                with ExitStack() as stA:
                    hT = sbt(stA, "hT", [128, 8, S], BF16)
                    wsb = sbt(stA, "wsb", [128, 8, 1536], BF16)
                    qst = [sbt(stA, "qst%d" % i, [128, 512], BF16) for i in range(2)]
                    w_v = w_in.rearrange("(k p) c -> p k c", p=128)
                    for k in range(8):
                        ld("pool", wsb[:, k, :], w_v[:, k, 2048:3584], [("wsb", k)])
                    WSB = [("wsb", k) for k in range(8)]
                    gmix_s = sbt(stA, "gmix_s", [128, D], F32)
                    ld("sp", gmix_s[:], gmix, ["gmix"])
                    xt = [sbt(stA, "xt%d" % i, [128, D], F32) for i in range(4)]
                    xn = [sbt(stA, "xn%d" % i, [128, D], BF16) for i in range(3)]
                    ssq = sbt(stA, "ssq", [128, NB], F32)
                    rs = sbt(stA, "rs", [128, NB], F32)
                    pj = [0]
                    qcn = [0]

                    def pbank():
                        pj[0] += 1
                        return 4 + pj[0] % 4

                    def proj_qk(T, j, which):
                        bank = pbank()
                        for k in range(8):
                            sc.op("pe", lambda e, k=k: e.matmul(
                                ps[bank][:], lhsT=wsb[:, k, which * 512 + j * 128:which * 512 + (j + 1) * 128],
                                rhs=hT[:, k, T * 512:(T + 1) * 512], start=(k == 0), stop=(k == 7)),
                                r=WSB + [("hT", T * 4 + i) for i in range(4)], w=[("ps", bank)])
                        if which == 0:
                            q2 = qcn[0] % 2
                            qcn[0] += 1
                            sc.op("act", lambda e: e.mul(out=qst[q2][:], in_=ps[bank][:], mul=0.125),
                                  r=[("ps", bank)], w=[("qst", q2)])
                            ld("sp", qT_d[:, j, T * 512:(T + 1) * 512], qst[q2][:], [("qT_d", T)], r=[("qst", q2)])
                        else:
                            evac(kT[:, j, T * 512:(T + 1) * 512], ps[bank][:], r=[("ps", bank)], w=[("kT", T)], eng="dve")

                    def proj_v(b):
                        bank = pbank()
                        for k in range(8):
                            sc.op("pe", lambda e, k=k: e.matmul(
                                ps[bank][:], lhsT=hT[:, k, b * 128:(b + 1) * 128], rhs=wsb[:, k, 1024:1536],
                                start=(k == 0), stop=(k == 7)),
                                r=WSB + [("hT", b)], w=[("ps", bank)])
                        evac(vv[:, b, :], ps[bank][:], r=[("ps", bank)], w=[("vv", b)], eng="dve")

                    pq_ = []

                    def a_s0(b):
                        ld("sp", xt[b % 4][:], x[b * 128:(b + 1) * 128, :], [("xt", b % 4)])

                    def a_s1(b):
                        sc.op("act", lambda e: e.activation(out=junk[:], in_=xt[b % 4][:], func=AF.Square, accum_out=ssq[:, b:b + 1]),
                              r=[("xt", b % 4)], w=["junk", ("ssq", b)])
                        rstd_ops(ssq[:, b:b + 1], rs[:, b:b + 1], ("ssq", b), ("rs", b))

                    def a_s2(b):
                        sc.op("dve", lambda e: e.scalar_tensor_tensor(
                            out=xn[b % 3][:], in0=xt[b % 4][:], scalar=rs[:, b:b + 1], in1=gmix_s[:], op0=ALU.mult, op1=ALU.mult),
                            r=[("xt", b % 4), ("rs", b), "gmix"], w=[("xn", b % 3)])

                    def a_s3(b):
                        pb = ps[b % 4].bitcast(BF16)
                        for k in range(8):
                            sc.op("pe", lambda e, k=k, pb=pb: e.transpose(
                                out=pb[:, k * 128:(k + 1) * 128], in_=xn[b % 3][:, k * 128:(k + 1) * 128], identity=ident[:]),
                                r=[("xn", b % 3), "ident"], w=[("ps", b % 4)])
                        evac(hT[:, :, b * 128:(b + 1) * 128], pb.rearrange("p (k t) -> p k t", k=8),
                             r=[("ps", b % 4)], w=[("hT", b)], eng="act")
                        if b % 4 == 3:
                            T_ = b // 4
                            ld("sp", hT_d[:, :, T_ * 512:(T_ + 1) * 512], hT[:, :, T_ * 512:(T_ + 1) * 512], [("hT_d", T_)],
                               r=[("hT", T_ * 4 + i) for i in range(4)])
                            for j_ in range(4):
                                for wh_ in range(2):
                                    pq_.append(lambda T_=T_, j_=j_, wh_=wh_: proj_qk(T_, j_, wh_))
                            for i_ in range(4):
                                pq_.append(lambda bb=T_ * 4 + i_: proj_v(bb))

                    stg1 = [a_s0, a_s1, a_s2, a_s3]
                    for step in range(NB + len(stg1) - 1):
                        for si in reversed(range(len(stg1))):
                            bb_ = step - si
                            if 0 <= bb_ < NB:
                                stg1[si](bb_)
                        for _ in range(3):
                            if pq_:
                                pq_.pop(0)()
                    while pq_:
                        pq_.pop(0)()
                sc.barrier()
                NE = 3
                e_bf = [sbt(st, "e_bf%d" % i, [128, 2, 512], BF16) for i in range(NE)]
                sp_bf = [sbt(st, "sp_bf%d" % i, [128, 2, 512], BF16) for i in range(NE)]
                E_bf = [sbt(st, "E_bf%d" % i, [128, 2, 512], BF16) for i in range(2)]
                NA = 5
                a_bf = [sbt(st, "a_bf%d" % i, [128, 2, 512], BF16) for i in range(NA)]
                S_bf = [sbt(st, "S_bf%d" % i, [128, 2, 512], BF16) for i in range(3)]
                o_sb = [sbt(st, "o_sb%d" % i, [128, 512], BF16) for i in range(2)]
                zt = sbt(st, "zt", [128, 1024], BF16)
                sc.op("pool", lambda e: e.memset(zt[:], 0.0), w=["zt"])


                with ExitStack() as st3:
                    wr = sbt(st3, "wr", [128, 8, 2048], BF16)
                    w_v = w_in.rearrange("(k p) c -> p k c", p=128)
                    for k in range(8):
                        ld("pool", wr[:, k, :], w_v[:, k, 0:2048], [("wr", k)])
                    WR = [("wr", k) for k in range(8)]
                    Dp_s = sbt(st3, "Dp_s", [128, 4, 128], F32)
                    qdec_s = sbt(st3, "qdec_s", [128, 4, 128], F32)
                    kdec_s = sbt(st3, "kdec_s", [128, 4, 128], F32)
                    ggn_s = sbt(st3, "ggn_s", [128, 512], F32)
                    ld("sp", Dp_s[:], Dp_d, ["Dp"])
                    ld("sp", qdec_s[:], qdec_d, ["qdec"])
                    ld("sp", kdec_s[:], kdec_d, ["kdec"])
                    ld("sp", ggn_s[:], ggn, ["ggn"])
                    state_f = sbt(st3, "state_f", [128, 4, 128], F32)
                    state_b = [sbt(st3, "state_b%d" % i, [128, 4, 128], BF16) for i in range(2)]
                    hTb = [sbt(st3, "hTb%d" % i, [128, 8, 128], BF16) for i in range(2)]
                    cs = [sbt(st3, "cs%d" % i, [128, 2, 64], F32) for i in range(2)]
                    q_sb = sbt(st3, "q_sb", [128, 512], F32)
                    k_sb = sbt(st3, "k_sb", [128, 512], F32)
                    eg = sbt(st3, "eg", [128, 512], F32)
                    q_r = sbt(st3, "q_r", [128, 4, 2, 64], BF16)
                    k_r = sbt(st3, "k_r", [128, 4, 2, 64], BF16)
                    kd = sbt(st3, "kd", [128, 4, 128], BF16)
                    v_bf = sbt(st3, "v_bf", [128, 512], BF16)
                    g_sil2 = [sbt(st3, "g_sil%d" % i, [128, 512], BF16) for i in range(2)]
                    qkT = sbt(st3, "qkT", [128, 8, 128], BF16)
                    scT = sbt(st3, "scT", [128, 4, 128], BF16)
                    o_s = sbt(st3, "o_s", [128, 4, 128], F32)
                    ret_b = sbt(st3, "ret_b", [128, 512], BF16)
                    retT_s = [sbt(st3, "retT_s%d" % i, [128, 4, 128], BF16) for i in range(2)]
                    tA = [sbt(st3, "tA%d" % i, [128, 4, 64], F32) for i in range(2)]
                    tB = [sbt(st3, "tB%d" % i, [128, 4, 64], F32) for i in range(2)]
                    bnst = sbt(st3, "bnst", [128, 4, 6], F32)
                    mv = sbt(st3, "mv", [128, 4, 2], F32)
                    rsd = sbt(st3, "rsd", [128, 4], F32)
                    sc.op("pool", lambda e: e.memset(state_f[:], 0.0), w=["state_f"])
                    BA, BB, BC = 5, 6, 7

                    def rope(src, dst, b, ksrc, kdst):
                        pv = src[:].rearrange("p (h two d) -> p h two d", h=4, two=2)
                        t1 = pv[:, :, 0, :]
                        t2 = pv[:, :, 1, :]
                        cb = cs[b % 2][:, 0, :].unsqueeze(1).broadcast_to([128, 4, 64])
                        sb_ = cs[b % 2][:, 1, :].unsqueeze(1).broadcast_to([128, 4, 64])
                        for half in range(2):
                            a0, a1 = (cb, sb_) if half == 0 else (sb_, cb)
                            op = ALU.subtract if half == 0 else ALU.add
                            sc.op("dve", lambda e, a0=a0, half=half: e.tensor_tensor(out=tA[half][:], in0=t1, in1=a0, op=ALU.mult),
                                  r=[ksrc, ("cs", b % 2)], w=[("tA", half)])
                            sc.op("dve", lambda e, a1=a1, half=half: e.tensor_tensor(out=tB[half][:], in0=t2, in1=a1, op=ALU.mult),
                                  r=[ksrc, ("cs", b % 2)], w=[("tB", half)])
                            sc.op("pool", lambda e, op=op, half=half: e.tensor_tensor(out=dst[:, :, half, :], in0=tA[half][:],
                                                                                     in1=tB[half][:], op=op),
                                  r=[("tA", half), ("tB", half)], w=[kdst])

                    def proj(b, wi, bank, half):
                        for k in range(half * 4, half * 4 + 4):
                            sc.op("pe", lambda e, k=k: e.matmul(
                                ps[bank][:], lhsT=hTb[b % 2][:, k, :], rhs=wr[:, k, wi * 512:(wi + 1) * 512],
                                start=(k == 0), stop=(k == 7)), r=WR + [("hTb", b % 2)], w=[("ps", bank)])

                    def m0(b):
                        ld("sp", hTb[b % 2][:], hT_d[:, :, b * 128:(b + 1) * 128], [("hTb", b % 2)], r=[("hT_d", b // 4)])
                        ld("sp", cs[b % 2][:, 0, :], cos_d[:, b, :], [("cs", b % 2)])
                        ld("sp", cs[b % 2][:, 1, :], sin_d[:, b, :], [("cs", b % 2)])

                    def pq_a(b):
                        proj(b, 0, BA, 0)

                    def pq_b(b):
                        proj(b, 0, BA, 1)

                    def pk_a(b):
                        sc.op("dve", lambda e: e.tensor_copy(out=q_sb[:], in_=ps[BA][:]), r=[("ps", BA)], w=["q_sb"])
                        proj(b, 1, BB, 0)

                    def pk_b(b):
                        proj(b, 1, BB, 1)
                        m6a(b)

                    def pv_a(b):
                        sc.op("dve", lambda e: e.tensor_copy(out=k_sb[:], in_=ps[BB][:]), r=[("ps", BB)], w=["k_sb"])
                        proj(b, 2, BA, 0)

                    def pv_b(b):
                        proj(b, 2, BA, 1)
                        m6b(b)

                    def pg_a(b):
                        sc.op("dve", lambda e: e.tensor_copy(out=v_bf[:], in_=ps[BA][:]), r=[("ps", BA)], w=["v_bf"])
                        proj(b, 3, BB, 0)

                    def pg_b(b):
                        proj(b, 3, BB, 1)
                        m6c(b)

                    def m5(b):
                        sc.op("act", lambda e: e.activation(out=eg[:], in_=ps[BB][:], func=AF.Exp, scale=-1.0), r=[("ps", BB)], w=["eg"])
                        sc.op("dve", lambda e: e.tensor_scalar(out=eg[:], in0=eg[:], scalar1=1.0, scalar2=None, op0=ALU.add), r=["eg"], w=["eg"])
                        sc.op("dve", lambda e: e.reciprocal(out=eg[:], in_=eg[:]), r=["eg"], w=["eg"])
                        sc.op("dve", lambda e: e.tensor_tensor(out=g_sil2[b % 2][:], in0=ps[BB][:], in1=eg[:], op=ALU.mult),
                              r=[("ps", BB), "eg"], w=[("g_sil", b % 2)])

                    def rope_half(src, dst, b, ksrc, kdst, half):
                        pv = src[:].rearrange("p (h two d) -> p h two d", h=4, two=2)
                        t1 = pv[:, :, 0, :]
                        t2 = pv[:, :, 1, :]
                        cb = cs[b % 2][:, 0, :].unsqueeze(1).broadcast_to([128, 4, 64])
                        sb_ = cs[b % 2][:, 1, :].unsqueeze(1).broadcast_to([128, 4, 64])
                        a0, a1 = (cb, sb_) if half == 0 else (sb_, cb)
                        op = ALU.subtract if half == 0 else ALU.add
                        sc.op("dve", lambda e: e.tensor_tensor(out=tA[half][:], in0=t1, in1=a0, op=ALU.mult),
                              r=[ksrc, ("cs", b % 2)], w=[("tA", half)])
                        sc.op("dve", lambda e: e.tensor_tensor(out=tB[half][:], in0=t2, in1=a1, op=ALU.mult),
                              r=[ksrc, ("cs", b % 2)], w=[("tB", half)])
                        sc.op("dve", lambda e: e.tensor_tensor(out=dst[:, :, half, :], in0=tA[half][:], in1=tB[half][:], op=op),
                              r=[("tA", half), ("tB", half)], w=[kdst])

                    def m6a(b):
                        rope_half(q_sb, q_r, b, "q_sb", "q_r", 0)

                    def m6b(b):
                        rope_half(q_sb, q_r, b, "q_sb", "q_r", 1)

                    def m6c(b):
                        rope_half(k_sb, k_r, b, "k_sb", "k_r", 0)

                    def m6d(b):
                        rope_half(k_sb, k_r, b, "k_sb", "k_r", 1)
                        sc.op("dve", lambda e: e.tensor_tensor(out=kd[:], in0=k_r[:].rearrange("p h two d -> p h (two d)"),
                                                                in1=kdec_s[:], op=ALU.mult),
                              r=["k_r", "kdec"], w=["kd"])

                    def m7(b):
                        pb = ps[BC].bitcast(BF16)
                        for hh in range(4):
                            sc.op("pe", lambda e, hh=hh, pb=pb: e.transpose(
                                out=pb[:, hh * 128:(hh + 1) * 128], in_=q_r[:, hh].rearrange("p two d -> p (two d)"),
                                identity=ident[:]), r=["q_r", "ident"], w=[("ps", BC)])
                        for hh in range(4):
                            sc.op("pe", lambda e, hh=hh, pb=pb: e.transpose(
                                out=pb[:, (4 + hh) * 128:(5 + hh) * 128], in_=k_r[:, hh].rearrange("p two d -> p (two d)"),
                                identity=ident[:]), r=["k_r", "ident"], w=[("ps", BC)])

                    def m8(b):
                        pb = ps[BC].bitcast(BF16)
                        sc.op("dve", lambda e: e.tensor_copy(out=qkT[:], in_=pb.rearrange("p (k t) -> p k t", k=8)), r=[("ps", BC)], w=["qkT"])

                    def m9(b):
                        for hh in range(4):
                            sc.op("pe", lambda e, hh=hh: e.matmul(
                                ps[BA][:, hh * 128:(hh + 1) * 128], lhsT=qkT[:, 4 + hh, :], rhs=qkT[:, hh, :],
                                start=True, stop=True), r=["qkT"], w=[("ps", BA)])

                    def m10(b):
                        sc.op("dve", lambda e: e.tensor_tensor(out=scT[:], in0=ps[BA][:].rearrange("p (h i) -> p h i", h=4),
                                                               in1=Dp_s[:], op=ALU.mult),
                              r=[("ps", BA), "Dp"], w=["scT"])

                    def m11(b):
                        sbi = b % 2
                        for hh in range(4):
                            sc.op("pe", lambda e, hh=hh: e.matmul(
                                ps[BB][:, hh * 128:(hh + 1) * 128], lhsT=scT[:, hh, :], rhs=v_bf[:, hh * 128:(hh + 1) * 128],
                                start=True, stop=(b == 0)), r=["scT", "v_bf"], w=[("ps", BB)])
                            if b > 0:
                                sc.op("pe", lambda e, hh=hh: e.matmul(
                                    ps[BB][:, hh * 128:(hh + 1) * 128], lhsT=qkT[:, hh, :], rhs=state_b[sbi][:, hh, :],
                                    start=False, stop=True), r=["qkT", ("state_b", sbi)], w=[("ps", BB)])
                        if b < NB - 1:
                            for hh in range(4):
                                sc.op("pe", lambda e, hh=hh: e.matmul(
                                    ps[BC][:, hh * 128:(hh + 1) * 128], lhsT=kd[:, hh, :], rhs=v_bf[:, hh * 128:(hh + 1) * 128],
                                    start=True, stop=True), r=["kd", "v_bf"], w=[("ps", BC)])

                    def m12(b):
                        if b < NB - 1:
                            for hh in range(4):
                                sc.op("dve", lambda e, hh=hh: e.scalar_tensor_tensor(
                                    out=state_f[:, hh, :], in0=state_f[:, hh, :], scalar=cdec[hh], in1=ps[BC][:, hh * 128:(hh + 1) * 128],
                                    op0=ALU.mult, op1=ALU.add), r=["state_f", ("ps", BC)], w=["state_f"])
                            nsb = (b + 1) % 2
                            sc.op("pool", lambda e: e.tensor_copy(out=state_b[nsb][:], in_=state_f[:]),
                                  r=["state_f"], w=[("state_b", nsb)])
                        sc.op("dve", lambda e: e.tensor_tensor(out=o_s[:], in0=ps[BB][:].rearrange("p (h e) -> p h e", h=4),
                                                               in1=qdec_s[:], op=ALU.mult),
                              r=[("ps", BB), "qdec"], w=["o_s"])

                    def m13(b):
                        for hh in range(4):
                            sc.op("dve", lambda e, hh=hh: e.bn_stats(out=bnst[:, hh, :], in_=o_s[:, hh, :]), r=["o_s"], w=["bnst"])
                        for hh in range(4):
                            sc.op("dve", lambda e, hh=hh: e.bn_aggr(out=mv[:, hh, :], in_=bnst[:, hh, :]), r=["bnst"], w=["mv"])
                        sc.op("dve", lambda e: e.tensor_scalar(out=rsd[:], in0=mv[:, :, 1], scalar1=EPS, scalar2=None, op0=ALU.add),
                              r=["mv"], w=["rsd"])
                        pow_ops(rsd[:], "rsd")

                    def m14(b):
                        for hh in range(4):
                            sc.op("dve", lambda e, hh=hh: e.tensor_scalar(
                                out=o_s[:, hh, :], in0=o_s[:, hh, :], scalar1=mv[:, hh, 0:1], scalar2=rsd[:, hh:hh + 1],
                                op0=ALU.subtract, op1=ALU.mult), r=["o_s", "mv", "rsd"], w=["o_s"])
                        sc.op("pool", lambda e: e.tensor_tensor(out=o_s[:], in0=o_s[:], in1=ggn_s[:].rearrange("p (h e) -> p h e", h=4), op=ALU.mult),
                              r=["o_s", "ggn"], w=["o_s"])
                        sc.op("pool", lambda e: e.tensor_tensor(out=ret_b[:], in0=o_s[:].rearrange("p h e -> p (h e)"), in1=g_sil2[b % 2][:], op=ALU.mult),
                              r=["o_s", ("g_sil", b % 2)], w=["ret_b"])

                    def m15(b):
                        pb6 = ps[BC].bitcast(BF16)
                        for hh in range(4):
                            sc.op("pe", lambda e, hh=hh, pb6=pb6: e.transpose(
                                out=pb6[:, hh * 128:(hh + 1) * 128], in_=ret_b[:, hh * 128:(hh + 1) * 128], identity=ident[:]),
                                r=["ret_b", "ident"], w=[("ps", BC)])

                    def m16(b):
                        pb6 = ps[BC].bitcast(BF16)
                        sc.op("dve", lambda e: e.tensor_copy(out=retT_s[b % 2][:], in_=pb6[:, 0:512].rearrange("p (k t) -> p k t", k=4)),
                              r=[("ps", BC)], w=[("retT_s", b % 2)])
                        ld("sp", retT_d[:, :, b * 128:(b + 1) * 128], retT_s[b % 2][:], [("retT_d", b // 4)], r=[("retT_s", b % 2)])

                    msched = [(m0, -6), (pq_a, 1), (pq_b, 2), (pk_a, 3), (pk_b, 4), (pv_a, 5), (pv_b, 6), (pg_a, 7), (pg_b, 8),
                              (m6d, 9), (m5, 10), (m7, 13), (m8, 14), (m9, 15), (m10, 16), (m11, 17), (m12, 18), (m13, 19),
                              (m14, 21), (m15, 25), (m16, 26)]
                    RPER = 18
                    rsteps = {}
                    for b_ in range(NB):
                        for fn_, off_ in msched:
                            rsteps.setdefault(max(0, b_ * RPER + off_), []).append((b_, fn_))

                    sbT_v = sbT_d.rearrange("(pr two) d s -> (two d) pr s", two=2)
                    tiles = []
                    for g in range(NG):
                        for j in range(4):
                            nkb = 4 * (g + 1)
                            for idx, kb in enumerate(range(nkb - 1, -1, -1)):
                                tiles.append(dict(g=g, j=j, kb=kb, idx=idx, last=(idx == nkb - 1), gj=g * 4 + j))
                    NT = len(tiles)
                    for i, t in enumerate(tiles):
                        t["i"] = i
                    OB = 4

                    def c0_of(t):
                        jd = t["kb"] - 4 * t["g"]
                        return jd * 128 if jd >= 1 else 0

                    def st_q(g):
                        ld("sp", qg[g % 2][:], qT_d[:, :, g * 512:(g + 1) * 512], [("qg", g % 2)], r=[("qT_d", g)])

                    def st_z(t):
                        i = t["i"]; g = t["g"]; j = t["j"]; kb = t["kb"]
                        c0 = c0_of(t)
                        jd = kb - 4 * g
                        for hh in range(2):
                            po = hh * 64
                            sc.op("pe", lambda e, hh=hh, po=po: e.matmul(
                                psr[0][:, hh * 512 + c0:(hh + 1) * 512], lhsT=kT[po:po + 64, j, kb * 128:(kb + 1) * 128],
                                rhs=qg[g % 2][po:po + 64, j, c0:512], start=True, stop=(jd < 0)),
                                r=[("kT", kb // 4), ("qg", g % 2)], w=["zr"])
                        if jd >= 0:
                            for hh in range(2):
                                sc.op("pe", lambda e, hh=hh: e.matmul(
                                    psr[0][:, hh * 512 + c0:(hh + 1) * 512], lhsT=ident[:], rhs=mask01[:, jd, c0:512],
                                    start=False, stop=True), r=["ident", "mask01"], w=["zr"])
                        eb = i % NE
                        sc.op("act", lambda e: e.activation(out=e_bf[eb][:, :, c0:], in_=psr[0][:].rearrange("p (h q) -> p h q", h=2)[:, :, c0:], func=AF.Exp),
                              r=["zr"], w=[("e", eb)])

                    def st_ln(t):
                        i = t["i"]
                        eb = i % NE
                        c0 = c0_of(t)
                        sc.op("act", lambda e: e.activation(out=sp_bf[eb][:, :, c0:], in_=e_bf[eb][:, :, c0:], func=AF.Ln, bias=1.0),
                              r=[("e", eb)], w=[("sp", eb)] + ([("splo", eb)] if c0 == 0 else []))
                        if c0 > 0:
                            sc.op("pool", lambda e: e.memset(sp_bf[eb][:, :, 0:c0], 0.0), r=[("sp", eb)], w=[("splo", eb)])
                        st_sadd(t)

                    def st_arg(t):
                        i = t["i"]; idx = t["idx"]
                        eb = i % NE
                        c0 = c0_of(t)
                        srcS_extra = []
                        if idx == 0:
                            srcS = None
                        elif idx == 1:
                            srcS = (sp_bf[(i - 1) % NE], ("sp", (i - 1) % NE))
                            srcS_extra = [("splo", (i - 1) % NE)]
                        else:
                            srcS = (S_bf[idx % 3], ("S", idx % 3))
                        for hh in range(2):
                            sc.op("pe", lambda e, hh=hh: e.matmul(psr[1][:, hh * 512 + c0:(hh + 1) * 512], lhsT=ntri[:], rhs=sp_bf[eb][:, hh, c0:],
                                                                  start=True, stop=(srcS is None)),
                                  r=[("sp", eb), "ntri"] + ([("splo", eb)] if c0 == 0 else []), w=["argr"])
                            if srcS is not None:
                                sc.op("pe", lambda e, hh=hh: e.matmul(psr[1][:, hh * 512 + c0:(hh + 1) * 512], lhsT=nones[:], rhs=srcS[0][:, hh, c0:],
                                                                      start=False, stop=True),
                                      r=[srcS[1], "nones"] + srcS_extra, w=["argr"])
                        sc.op("act", lambda e: e.activation(out=E_bf[i % 2][:, :, c0:], in_=psr[1][:].rearrange("p (h q) -> p h q", h=2)[:, :, c0:], func=AF.Exp),
                              r=["argr"], w=[("E", i % 2)])

                    def st_dve(t):
                        i = t["i"]; idx = t["idx"]
                        eb = i % NE
                        c0 = c0_of(t)
                        sc.op("dve", lambda e: e.tensor_tensor(out=a_bf[i % NA][:, :, c0:], in0=e_bf[eb][:, :, c0:], in1=E_bf[i % 2][:, :, c0:], op=ALU.mult),
                              r=[("e", eb), ("E", i % 2)], w=[("a", i % NA)] + ([("alo", i % NA)] if c0 == 0 else []))
                        if c0 > 0:
                            sc.op("pool", lambda e: e.memset(a_bf[i % NA][:, :, 0:c0], 0.0), r=[("a", i % NA)], w=[("alo", i % NA)])

                    def st_sadd(t):
                        i = t["i"]; idx = t["idx"]
                        eb = i % NE
                        if not t["last"] and idx >= 1:
                            nxt = (idx + 1) % 3
                            if idx == 1:
                                pe_ = (i - 1) % NE
                                sc.op("dve", lambda e: e.tensor_tensor(out=S_bf[nxt][:], in0=sp_bf[pe_][:], in1=sp_bf[eb][:], op=ALU.add),
                                      r=[("sp", pe_), ("sp", eb), ("splo", pe_), ("splo", eb)], w=[("S", nxt)])
                            else:
                                cur = idx % 3
                                sc.op("dve", lambda e: e.tensor_tensor(out=S_bf[nxt][:], in0=S_bf[cur][:], in1=sp_bf[eb][:], op=ALU.add),
                                      r=[("S", cur), ("sp", eb), ("splo", eb)], w=[("S", nxt)])

                    def st_o(t):
                        i = t["i"]; g = t["g"]; j = t["j"]; kb = t["kb"]; idx = t["idx"]
                        for hh in range(2):
                            h = 2 * j + hh
                            sc.op("pe", lambda e, hh=hh, h=h: e.matmul(ps[OB][hh * 64:(hh + 1) * 64, :], lhsT=vv[:, kb, h * 64:(h + 1) * 64],
                                                                       rhs=a_bf[i % NA][:, hh, :], start=(idx == 0), stop=t["last"]),
                                  r=[("vv", kb), ("a", i % NA), ("alo", i % NA)], w=[("ps", OB)])
                        if t["last"]:
                            oi = t["gj"] % 2
                            sc.op("dve", lambda e: e.tensor_copy(out=o_sb[oi][:], in_=ps[OB][:]),
                                  r=[("ps", OB)], w=[("o_sb", oi)])
                            ld("sp", sbT_v[:, j, g * 512:(g + 1) * 512], o_sb[oi][:], [("sbT_d", g)], r=[("o_sb", oi)])

                    xe_z = xe_d.rearrange("(n p) f -> n p f", p=128)
                    nzf = xe_z.shape[0]
                    zf_every = max(1, NT // nzf)
                    zf_done = [0]
                    st_q(0)
                    OLAG = 4
                    for step in range(NT + OLAG):
                        if step % zf_every == 0 and zf_done[0] < nzf:
                            ld("sp", xe_z[zf_done[0]], zt[:], ["xe_d"], r=["zt"])
                            zf_done[0] += 1
                        if step < NT and tiles[step]["idx"] == 0 and tiles[step]["j"] == 0 and tiles[step]["g"] + 1 < NG:
                            st_q(tiles[step]["g"] + 1)
                        diag = step < NT and (tiles[step]["kb"] - 4 * tiles[step]["g"] >= 0)
                        if step < NT:
                            st_z(tiles[step])
                            if not diag:
                                st_ln(tiles[step])
                        if 0 <= step - 1 < NT:
                            st_arg(tiles[step - 1])
                        if step < NT and diag:
                            st_ln(tiles[step])
                        if 0 <= step - 1 < NT:
                            st_dve(tiles[step - 1])
                        if 0 <= step - OLAG < NT:
                            st_o(tiles[step - OLAG])
                        for b_, fn_ in rsteps.pop(step, []):
                            fn_(b_)
                    while zf_done[0] < nzf:
                        ld("sp", xe_z[zf_done[0]], zt[:], ["xe_d"], r=["zt"])
                        zf_done[0] += 1
                    for step in sorted(rsteps):
                        for b_, fn_ in rsteps[step]:
                            fn_(b_)
            sc.barrier()

            with ExitStack() as stR:
                dest_i = sbt(stR, "dest_i", [128, NB, 2], I32)
                wts = sbt(stR, "wts", [128, NB, 2], F32)
                with ExitStack() as st:
                    wg = sbt(st, "wg", [128, 8, 2048], BF16)
                    w_v = w_in.rearrange("(k p) c -> p k c", p=128)
                    wbr = sbt(st, "wbr", [128, 4, D], BF16)
                    wbs = sbt(st, "wbs", [128, 4, D], BF16)
                    wo = sbt(st, "wo", [128, 8, D], BF16)
                    ld("pool", wbr[:], w_br_ret.rearrange("(k p) c -> p k c", p=128), ["wbr"])
                    for k in range(8):
                        ld("pool", wg[:, k, 0:1024], w_v[:, k, 3584:4608], [("wg0", k)])
                    ld("pool", wbs[:], w_br_sb.rearrange("(k p) c -> p k c", p=128), ["wbs"])
                    for k in range(8):
                        ld("pool", wg[:, k, 1024:2048], w_v[:, k, 4608:5632], [("wg1", k)])
                    ld("pool", wo[:], w_out.rearrange("(k p) c -> p k c", p=128), ["wo"])
                    WG = [[("wg0", k) for k in range(8)], [("wg1", k) for k in range(8)]]
                    wrt = sbt(st, "wrt", [128, 8, 20], F32)
                    ld("sp", wrt[:], w_rt.rearrange("(k p) c -> p k c", p=128), ["wrt"])
                    brt = sbt(st, "brt", [128, 20], F32)
                    ld("sp", brt[:], b_rt, ["brt"])
                    identf = sbt(st, "identf", [128, 128], F32)
                    ld("sp", identf[:], identf_d, ["identf"])
                    trilt = sbt(st, "trilt", [128, 128], BF16)
                    onesb = sbt(st, "onesb", [128, 128], BF16)
                    ld("sp", trilt[:], trilt_d, ["trilt"])
                    ld("sp", onesb[:], onesbf_d, ["onesb"])
                    ebase = sbt(st, "ebase_s", [128, 16], F32)
                    ld("sp", ebase[:], ebase_d, ["ebase"])
                    gffn_s = sbt(st, "gffn_s", [128, D], F32)
                    ld("sp", gffn_s[:], gffn, ["gffn"])
                    hTt = [sbt(st, "hTt%d" % i, [128, 8, 512], BF16) for i in range(2)]
                    retT_t = [sbt(st, "retT_t%d" % i, [128, 4, 512], BF16) for i in range(2)]
                    sbT_t = [sbt(st, "sbT_t%d" % i, [128, 4, 512], BF16) for i in range(2)]
                    mT = [sbt(st, "mT%d" % i, [128, 8, 512], BF16) for i in range(2)]
                    sg = [sbt(st, "sg%d" % i, [128, 512], F32) for i in range(2)]
                    m1 = [sbt(st, "m1_%d" % i, [128, 512], F32) for i in range(2)]
                    x1s = [sbt(st, "x1s%d" % i, [128, D], F32) for i in range(3)]
                    hnf2 = [sbt(st, "hnf%d" % i, [128, D], F32) for i in range(2)]
                    hnb = [sbt(st, "hnb%d" % i, [128, D], BF16) for i in range(2)]
                    hnT = sbt(st, "hnT", [128, 8, 128], F32)
                    ssq4 = sbt(st, "ssq4", [128, NB], F32)
                    rs4 = sbt(st, "rs4", [128, NB], F32)
                    lg = sbt(st, "lg", [128, 20], F32)
                    sm = sbt(st, "sm", [128, 16], F32)
                    ohg = sbt(st, "ohg", [128, 4], F32)
                    tmp16 = sbt(st, "tmp16", [128, 4, 4], F32)
                    ig = sbt(st, "ig", [128, 4], F32)
                    ig2 = sbt(st, "ig2", [128, 4], F32)
                    oh1 = sbt(st, "oh1", [128, 4], F32)
                    oh2 = sbt(st, "oh2", [128, 4], F32)
                    ohe1 = sbt(st, "ohe1", [128, 4, 4], F32)
                    ohe2 = sbt(st, "ohe2", [128, 4, 4], F32)
                    A_bf = sbt(st, "A_bf", [128, 16], BF16)
                    Acum = [sbt(st, "Acum%d" % i, [128, 16], BF16) for i in range(2)]
                    rk = sbt(st, "rk", [128, 16], F32)
                    destf = sbt(st, "destf", [128, 2], F32)
                    sc.op("pool", lambda e: e.memset(Acum[0][:], 0.0), w=[("Acum", 0)])
                    sbT_v = sbT_d.rearrange("(pr two) d s -> (two d) pr s", two=2)
                    pcnt = [0]

                    def p4_load(T):
                        t2 = T % 2
                        ld("sp", hTt[t2][:], hT_d[:, :, T * 512:(T + 1) * 512], [("hTt", t2)], r=[("hT_d", T)])
                        ld("sp", retT_t[t2][:], retT_d[:, :, T * 512:(T + 1) * 512], [("retT_t", t2)], r=[("retT_d", T)])
                        for pr in range(4):
                            ld("sp", sbT_t[t2][:, pr, :], sbT_v[:, pr, T * 512:(T + 1) * 512], [("sbT_t", t2)], r=[("sbT_d", T)])

                    def p4_c(T, c):
                        t2 = T % 2
                        HT = [("hTt", t2)]
                        for br in range(2):
                            wb_, src, ksrc, kw = (wbr, retT_t[t2], ("retT_t", t2), "wbr") if br == 0 else (wbs, sbT_t[t2], ("sbT_t", t2), "wbs")
                            pp = pcnt[0] % 3
                            pcnt[0] += 1
                            bb = 2 * pp
                            gb = 2 * pp + 1
                            for k in range(4):
                                sc.op("pe", lambda e, k=k, wb_=wb_, src=src, bb=bb: e.matmul(
                                    ps[bb][:], lhsT=wb_[:, k, c * 128:(c + 1) * 128], rhs=src[:, k, :],
                                    start=(k == 0), stop=(k == 3)), r=[kw, ksrc], w=[("ps", bb)])
                            for k in range(8):
                                sc.op("pe", lambda e, k=k, br=br, gb=gb: e.matmul(
                                    ps[gb][:], lhsT=wg[:, k, br * 1024 + c * 128: br * 1024 + (c + 1) * 128],
                                    rhs=hTt[t2][:, k, :], start=(k == 0), stop=(k == 7)),
                                    r=WG[br] + HT, w=[("ps", gb)])
                            sc.op("act", lambda e, br=br, gb=gb: e.activation(out=sg[br][:], in_=ps[gb][:], func=AF.Sigmoid),
                                  r=[("ps", gb)], w=[("sg", br)])
                            sc.op("dve", lambda e, br=br, bb=bb: e.tensor_tensor(out=m1[br][:], in0=ps[bb][:], in1=sg[br][:], op=ALU.mult),
                                  r=[("ps", bb), ("sg", br)], w=[("m1", br)])
                        sc.op("pool", lambda e: e.tensor_tensor(out=mT[t2][:, c, :], in0=m1[0][:], in1=m1[1][:], op=ALU.add),
                              r=[("m1", 0), ("m1", 1)], w=[("mT", t2)])

                    def p4_blk(T, i, part):
                        R = sc.op
                        if True:
                            t2 = T % 2
                            b = T * 4 + i
                            i2 = b % 3
                            if part == "A":
                                ld("sp", x1s[i2][:], x[b * 128:(b + 1) * 128, :], [("x1s", i2)])
                                for hf in range(2):
                                    bank = 6 + hf
                                    for c in range(8):
                                        sc.op("pe", lambda e, c=c, hf=hf, bank=bank: e.matmul(
                                            ps[bank][:], lhsT=mT[t2][:, c, i * 128:(i + 1) * 128], rhs=wo[:, c, hf * 512:(hf + 1) * 512],
                                            start=(c == 0), stop=(c == 7)), r=[("mT", t2), "wo"], w=[("ps", bank)])
                                    sc.op("dve", lambda e, hf=hf, bank=bank: e.tensor_tensor(
                                        out=x1s[i2][:, hf * 512:(hf + 1) * 512], in0=ps[bank][:], in1=x1s[i2][:, hf * 512:(hf + 1) * 512], op=ALU.add),
                                        r=[("ps", bank), ("x1s", i2)], w=[("x1s", i2)])
                                ld("sp", x1_d[b * 128:(b + 1) * 128, :], x1s[i2][:], [("x1_d", b)], r=[("x1s", i2)])
                                sc.op("act", lambda e, b=b, i2=i2: e.activation(out=junk[:], in_=x1s[i2][:], func=AF.Square,
                                                                               accum_out=ssq4[:, b:b + 1]),
                                      r=[("x1s", i2)], w=["junk", ("ssq4", b)])
                                rstd_ops(ssq4[:, b:b + 1], rs4[:, b:b + 1], ("ssq4", b), ("rs4", b))
                                sc.op("dve", lambda e, b=b, i2=i2: e.scalar_tensor_tensor(
                                    out=hnf2[b % 2][:], in0=x1s[i2][:], scalar=rs4[:, b:b + 1], in1=gffn_s[:], op0=ALU.mult, op1=ALU.mult),
                                    r=[("x1s", i2), ("rs4", b), "gffn"], w=[("hnf", b % 2)])
                                sc.op("act", lambda e, b=b: e.copy(out=hnb[b % 2][:], in_=hnf2[b % 2][:]), r=[("hnf", b % 2)], w=[("hnb", b % 2)])
                            if part == "B":
                                for half in range(2):
                                    bank = 6 + half
                                    for kk in range(4):
                                        k = half * 4 + kk
                                        sc.op("pe", lambda e, k=k, kk=kk, bank=bank: e.transpose(
                                            out=ps[bank][:, kk * 128:(kk + 1) * 128], in_=hnf2[b % 2][:, k * 128:(k + 1) * 128], identity=identf[:]),
                                            r=[("hnf", b % 2), "identf"], w=[("ps", bank)])
                                    evac(hnT[:, half * 4:(half + 1) * 4, :], ps[bank][:].rearrange("p (k t) -> p k t", k=4),
                                         r=[("ps", bank)], w=["hnT"])
                            if part == "C":
                                for k in range(8):
                                    sc.op("pe", lambda e, k=k: e.matmul(ps[6][:, 0:20], lhsT=hnT[:, k, :], rhs=wrt[:, k, :],
                                                                        start=(k == 0), stop=(k == 7)),
                                          r=["hnT", "wrt"], w=[("ps", 6)])
                                R("dve", lambda e: e.tensor_tensor(out=lg[:], in0=ps[6][:, 0:20], in1=brt[:], op=ALU.add),
                                  r=[("ps", 6), "brt"], w=["lg"])
                                R("dve", lambda e: e.tensor_reduce(out=sm[:, 0:1], in_=lg[:, 0:4], axis=AX.X, op=ALU.max), r=["lg"], w=["sm"])
                                R("dve", lambda e: e.tensor_scalar(out=ohg[:], in0=lg[:, 0:4], scalar1=sm[:, 0:1], scalar2=None, op0=ALU.is_equal),
                                  r=["lg", "sm"], w=["ohg"])
                                R("dve", lambda e: e.tensor_scalar(out=ig2[:], in0=lg[:, 0:4], scalar1=sm[:, 0:1], scalar2=None, op0=ALU.subtract),
                                  r=["lg", "sm"], w=["ig2"])
                                R("act", lambda e: e.activation(out=ig2[:], in_=ig2[:], func=AF.Sigmoid), r=["ig2"], w=["ig2"])
                                R("dve", lambda e: e.tensor_scalar(out=oh2[:], in0=ig2[:], scalar1=-1.0, scalar2=1.0, op0=ALU.mult, op1=ALU.add),
                                  r=["ig2"], w=["oh2"])
                                R("dve", lambda e: e.reciprocal(out=oh2[:], in_=oh2[:]), r=["oh2"], w=["oh2"])
                                R("dve", lambda e: e.tensor_tensor(out=ig2[:], in0=ig2[:], in1=oh2[:], op=ALU.mult), r=["ig2", "oh2"], w=["ig2"])
                                R("dve", lambda e: e.tensor_reduce(out=sm[:, 2:3], in_=ig2[:], axis=AX.X, op=ALU.add), r=["ig2"], w=["sm"])
                                R("dve", lambda e: e.reciprocal(out=sm[:, 3:4], in_=sm[:, 2:3]), r=["sm"], w=["sm"])
                                R("dve", lambda e: e.tensor_tensor(out=tmp16[:], in0=lg[:, 4:20].rearrange("p (g j) -> p g j", g=4),
                                                                   in1=ohg[:].unsqueeze(2).broadcast_to([128, 4, 4]), op=ALU.mult),
                                  r=["lg", "ohg"], w=["tmp16"])
                                R("dve", lambda e: e.tensor_reduce(out=ig[:], in_=tmp16[:].rearrange("p g j -> p j g"), axis=AX.X, op=ALU.add),
                                  r=["tmp16"], w=["ig"])
                                R("dve", lambda e: e.tensor_reduce(out=sm[:, 4:5], in_=ig[:], axis=AX.X, op=ALU.max), r=["ig"], w=["sm"])
                                R("dve", lambda e: e.tensor_scalar(out=oh1[:], in0=ig[:], scalar1=sm[:, 4:5], scalar2=None, op0=ALU.is_equal),
                                  r=["ig", "sm"], w=["oh1"])
                                R("dve", lambda e: e.scalar_tensor_tensor(out=ig2[:], in0=oh1[:], scalar=-1e30, in1=ig[:], op0=ALU.mult, op1=ALU.add),
                                  r=["oh1", "ig"], w=["ig2"])
                                R("dve", lambda e: e.tensor_reduce(out=sm[:, 5:6], in_=ig2[:], axis=AX.X, op=ALU.max), r=["ig2"], w=["sm"])
                                R("dve", lambda e: e.tensor_scalar(out=oh2[:], in0=ig2[:], scalar1=sm[:, 5:6], scalar2=None, op0=ALU.is_equal),
                                  r=["ig2", "sm"], w=["oh2"])
                                R("dve", lambda e: e.tensor_tensor(out=sm[:, 6:7], in0=sm[:, 4:5], in1=sm[:, 5:6], op=ALU.subtract), r=["sm"], w=["sm"])
                                R("act", lambda e: e.activation(out=sm[:, 7:8], in_=sm[:, 6:7], func=AF.Sigmoid), r=["sm"], w=["sm"])
                                R("dve", lambda e: e.tensor_scalar(out=sm[:, 8:9], in0=sm[:, 7:8], scalar1=-1.0, scalar2=1.0, op0=ALU.mult, op1=ALU.add),
                                  r=["sm"], w=["sm"])
                                R("dve", lambda e, b=b: e.tensor_tensor(out=wts[:, b, 0:1], in0=sm[:, 7:8], in1=sm[:, 3:4], op=ALU.mult),
                                  r=["sm"], w=[("wts", b)])
                                R("dve", lambda e, b=b: e.tensor_tensor(out=wts[:, b, 1:2], in0=sm[:, 8:9], in1=sm[:, 3:4], op=ALU.mult),
                                  r=["sm", ("wts", b)], w=[("wts", b)])
                                R("dve", lambda e: e.tensor_tensor(out=ohe1[:], in0=ohg[:].unsqueeze(2).broadcast_to([128, 4, 4]),
                                                                   in1=oh1[:].unsqueeze(1).broadcast_to([128, 4, 4]), op=ALU.mult),
                                  r=["ohg", "oh1"], w=["ohe1"])
                                R("dve", lambda e: e.tensor_tensor(out=ohe2[:], in0=ohg[:].unsqueeze(2).broadcast_to([128, 4, 4]),
                                                                   in1=oh2[:].unsqueeze(1).broadcast_to([128, 4, 4]), op=ALU.mult),
                                  r=["ohg", "oh2"], w=["ohe2"])
                                R("dve", lambda e: e.tensor_tensor(out=A_bf[:], in0=ohe1[:].rearrange("p g j -> p (g j)"),
                                                                   in1=ohe2[:].rearrange("p g j -> p (g j)"), op=ALU.add),
                                  r=["ohe1", "ohe2"], w=["A_bf"])
                            if part == "D":
                                ac = b % 2
                                R("pe", lambda e: e.matmul(ps[7][:, 0:16], lhsT=trilt[:], rhs=A_bf[:], start=True, stop=False),
                                  r=["trilt", "A_bf"], w=[("ps", 7)])
                                R("pe", lambda e, ac=ac: e.matmul(ps[7][:, 0:16], lhsT=onesb[:], rhs=Acum[ac][:], start=False, stop=True),
                                  r=["onesb", ("Acum", ac)], w=[("ps", 7)])
                                R("pool", lambda e, ac=ac: e.tensor_tensor(out=Acum[1 - ac][:], in0=Acum[ac][:], in1=A_bf[:], op=ALU.add),
                                  r=[("Acum", ac), "A_bf"], w=[("Acum", 1 - ac)])
                                R("dve", lambda e: e.tensor_tensor(out=rk[:], in0=ps[7][:, 0:16], in1=ebase[:], op=ALU.add),
                                  r=[("ps", 7), "ebase"], w=["rk"])
                                for kk, oh in enumerate((ohe1, ohe2)):
                                    R("dve", lambda e, oh=oh: e.tensor_tensor(out=tmp16[:].rearrange("p g j -> p (g j)"), in0=rk[:],
                                                                             in1=oh[:].rearrange("p g j -> p (g j)"), op=ALU.mult),
                                      r=["rk", "ohe1", "ohe2"], w=["tmp16"])
                                    R("dve", lambda e, kk=kk: e.tensor_reduce(out=destf[:, kk:kk + 1], in_=tmp16[:].rearrange("p g j -> p (g j)"),
                                                                             axis=AX.X, op=ALU.add), r=["tmp16"], w=["destf"])
                                R("dve", lambda e, b=b: e.tensor_copy(out=dest_i[:, b, :], in_=destf[:]), r=["destf"], w=[("dest", b)])
                                for kk in range(2):
                                    sc.dma("pool", lambda e, b=b, kk=kk, i2=i2: e.indirect_dma_start(
                                        out=xe_d[:, :], out_offset=bass.IndirectOffsetOnAxis(ap=dest_i[:, b, kk:kk + 1], axis=0),
                                        in_=hnb[b % 2][:], in_offset=None), r=[("dest", b), ("hnb", b % 2)], w=["xe_d"])

                    sched = {0: ["A0"], 1: ["B0", "A1"], 2: ["C0", "B1"], 3: ["D0", "C1", "A2"], 4: ["D1", "B2"],
                             5: ["C2", "A3"], 6: ["D2", "B3"], 7: ["C3"]}
                    for T in range(NG + 1):
                        if T < NG:
                            p4_load(T)
                        for c in range(8):
                            if T < NG:
                                p4_c(T, c)
                            if T >= 2 and c == 0:
                                p4_blk(T - 2, 3, "D")
                            if T >= 1:
                                for it in sched[c]:
                                    p4_blk(T - 1, int(it[1]), it[0])
                    p4_blk(NG - 1, 3, "D")
                    if debug:
                        ld("sp", rt_d[:, :, 0:2], wts[:], ["rt_d"], r=[("wts", b) for b in range(NB)])
                sc.barrier()

                wpg = sbt(stR, "wpg", [128, 8, D], BF16)
                wpl = sbt(stR, "wpl", [128, 2, D], BF16)
                gple_s = sbt(stR, "gple_s", [128, D], F32)
                gfin_s = sbt(stR, "gfin_s", [128, D], F32)
                with ExitStack() as st:
                    SLT = 384 if C % 384 == 0 else 512
                    NSB = SLT // 128
                    NS = C // SLT
                    wge = [sbt(st, "wge%d" % i, [128, 8, 512], BF16) for i in range(2)]
                    wue = [sbt(st, "wue%d" % i, [128, 8, 512], BF16) for i in range(2)]
                    wde = [sbt(st, "wde%d" % i, [128, 4, D], BF16) for i in range(2)]
                    xe_t = [sbt(st, "xe_t%d" % i, [128, NSB, D], BF16) for i in range(3)]
                    xeT = [sbt(st, "xeT%d" % i, [128, 8, SLT], BF16) for i in range(2)]
                    sgl = [sbt(st, "sgl%d" % i, [128, SLT], BF16) for i in range(2)]
                    hid = [sbt(st, "hid%d" % i, [128, 4, SLT], BF16) for i in range(2)]
                    y_sb = [sbt(st, "y_sb%d" % i, [128, D], F32) for i in range(3)]
                    tl = [(ex, s_) for ex in range(16) for s_ in range(NS)]
                    cnt6 = [0]

                    def e_s0(n):
                        ex, s_ = tl[n]
                        e2 = ex % 2
                        if n == 3:
                            ld("pool", wpg[:], w_pg.rearrange("(k p) c -> p k c", p=128), ["wpg"])
                            ld("pool", wpl[:], w_ple.rearrange("(k p) c -> p k c", p=128), ["wpl"])
                            ld("sp", gple_s[:], gple, ["gple"])
                            ld("sp", gfin_s[:], gfin, ["gfin"])
                        if s_ == 0:
                            ld("pool", wge[e2][:], w_gate[ex].rearrange("(k p) c -> p k c", p=128), [("wge", e2)])
                            ld("pool", wue[e2][:], w_up[ex].rearrange("(k p) c -> p k c", p=128), [("wue", e2)])
                            ld("pool", wde[e2][:], w_down[ex].rearrange("(k p) c -> p k c", p=128), [("wde", e2)])
                        r0 = ex * C + s_ * SLT
                        ld("sp", xe_t[n % 3][:], xe_d[r0:r0 + SLT, :].rearrange("(i p) f -> p i f", p=128), [("xe_t", n % 3)], r=["xe_d"])

                    def e_s1(n):
                        for i in range(NSB):
                            bank = i % 2
                            pb = ps[bank].bitcast(BF16)
                            for k in range(8):
                                sc.op("pe", lambda e, k=k, i=i, pb=pb: e.transpose(
                                    out=pb[:, k * 128:(k + 1) * 128], in_=xe_t[n % 3][:, i, k * 128:(k + 1) * 128], identity=ident[:]),
                                    r=[("xe_t", n % 3), "ident"], w=[("ps", bank)])
                            evac(xeT[n % 2][:, :, i * 128:(i + 1) * 128], pb.rearrange("p (k t) -> p k t", k=8), r=[("ps", bank)], w=[("xeT", n % 2)])

                    def e_s2(n):
                        ex, s_ = tl[n]
                        e2 = ex % 2
                        for c in range(4):
                            gb = 2 + (c % 2) * 2
                            ub = gb + 1
                            for k in range(8):
                                sc.op("pe", lambda e, k=k, c=c, gb=gb: e.matmul(
                                    ps[gb][:, 0:SLT], lhsT=wge[e2][:, k, c * 128:(c + 1) * 128], rhs=xeT[n % 2][:, k, :],
                                    start=(k == 0), stop=(k == 7)), r=[("wge", e2), ("xeT", n % 2)], w=[("ps", gb)])
                            for k in range(8):
                                sc.op("pe", lambda e, k=k, c=c, ub=ub: e.matmul(
                                    ps[ub][:, 0:SLT], lhsT=wue[e2][:, k, c * 128:(c + 1) * 128], rhs=xeT[n % 2][:, k, :],
                                    start=(k == 0), stop=(k == 7)), r=[("wue", e2), ("xeT", n % 2)], w=[("ps", ub)])
                            sc.op("act", lambda e, c=c, gb=gb: e.activation(out=sgl[c % 2][:], in_=ps[gb][:, 0:SLT], func=AF.Silu),
                                  r=[("ps", gb)], w=[("sgl", c % 2)])
                            sc.op("dve", lambda e, c=c, ub=ub: e.tensor_tensor(out=hid[n % 2][:, c, :], in0=ps[ub][:, 0:SLT], in1=sgl[c % 2][:], op=ALU.mult),
                                  r=[("ps", ub), ("sgl", c % 2)], w=[("hid", n % 2)])

                    def e_s3(n):
                        ex, s_ = tl[n]
                        e2 = ex % 2
                        r0 = ex * C + s_ * SLT
                        for i in range(NSB):
                            y2 = cnt6[0] % 3
                            cnt6[0] += 1
                            for hf in range(2):
                                bank = 6 + hf
                                for c in range(4):
                                    sc.op("pe", lambda e, c=c, i=i, hf=hf, bank=bank: e.matmul(
                                        ps[bank][:], lhsT=hid[n % 2][:, c, i * 128:(i + 1) * 128], rhs=wde[e2][:, c, hf * 512:(hf + 1) * 512],
                                        start=(c == 0), stop=(c == 3)), r=[("hid", n % 2), ("wde", e2)], w=[("ps", bank)])
                                evac(y_sb[y2][:, hf * 512:(hf + 1) * 512], ps[bank][:], r=[("ps", bank)], w=[("y_sb", y2)],
                                     eng=("act" if hf == 0 else "dve"))
                            ld("sp", ye_d[r0 + i * 128:r0 + (i + 1) * 128, :], y_sb[y2][:], ["ye_d"], r=[("y_sb", y2)])

                    stg = [e_s0, e_s1, e_s2, e_s3]
                    assert NS >= 2
                    for step in range(len(tl) + len(stg) - 1):
                        for si in reversed(range(len(stg))):
                            n = step - si
                            if 0 <= n < len(tl):
                                stg[si](n)
                sc.barrier()

                with ExitStack() as st:
                    NX = 6
                    x1b = [sbt(st, "x1b%d" % i, [128, D], F32) for i in range(NX)]
                    y1 = [sbt(st, "y1_%d" % i, [128, D], F32) for i in range(3)]
                    y2b = [sbt(st, "y2_%d" % i, [128, D], F32) for i in range(3)]
                    hpb = [sbt(st, "hpb%d" % i, [128, D], BF16) for i in range(2)]
                    hpT = [sbt(st, "hpT%d" % i, [128, 8, 128], BF16) for i in range(2)]
                    pbf = [sbt(st, "pbf%d" % i, [128, 256], BF16) for i in range(4)]
                    pT = [sbt(st, "pT%d" % i, [128, 2, 128], BF16) for i in range(2)]
                    sig = [sbt(st, "sig%d" % i, [128, D], F32) for i in range(2)]
                    tmpx = [sbt(st, "tmpx%d" % i, [128, D], F32) for i in range(2)]
                    ob = [sbt(st, "ob%d" % i, [128, D], F32) for i in range(2)]
                    ssq7 = sbt(st, "ssq7", [128, NB, 2], F32)
                    rs7 = sbt(st, "rs7", [128, NB, 2], F32)

                    def f_s0(b):
                        ld("sp", x1b[b % NX][:], x1_d[b * 128:(b + 1) * 128, :], [("x1b", b % NX)], r=[("x1_d", b)])
                        ld("pool", pbf[b % 4][:], p_in[b * 128:(b + 1) * 128, :], [("pbf", b % 4)])
                        for kk, yb in enumerate((y1, y2b)):
                            sc.dma("pool", lambda e, kk=kk, yb=yb: e.indirect_dma_start(
                                out=yb[b % 3][:], out_offset=None, in_=ye_d[:, :],
                                in_offset=bass.IndirectOffsetOnAxis(ap=dest_i[:, b, kk:kk + 1], axis=0)),
                                r=["ye_d", ("dest", b)], w=[("y%d" % kk, b % 3)])

                    def f_s1(b):
                        xx = x1b[b % NX]
                        for kk, yb in enumerate((y1, y2b)):
                            sc.op("dve", lambda e, kk=kk, yb=yb: e.scalar_tensor_tensor(
                                out=xx[:], in0=yb[b % 3][:], scalar=wts[:, b, kk:kk + 1], in1=xx[:], op0=ALU.mult, op1=ALU.add),
                                r=[("y%d" % kk, b % 3), ("wts", b), ("x1b", b % NX)], w=[("x1b", b % NX)])
                        if debug:
                            ld("sp", x2_d[b * 128:(b + 1) * 128, :], xx[:], ["x2_d"], r=[("x1b", b % NX)])
                        sc.op("act", lambda e: e.activation(out=junk[:], in_=xx[:], func=AF.Square, accum_out=ssq7[:, b, 0:1]),
                              r=[("x1b", b % NX)], w=["junk", ("ssq7", b)])
                        rstd_ops(ssq7[:, b, 0:1], rs7[:, b, 0:1], ("ssq7", b), ("rs7", b))
                        sc.op("dve", lambda e: e.scalar_tensor_tensor(
                            out=hpb[b % 2][:], in0=xx[:], scalar=rs7[:, b, 0:1], in1=gple_s[:], op0=ALU.mult, op1=ALU.mult),
                            r=[("x1b", b % NX), ("rs7", b), "gple"], w=[("hpb", b % 2)])

                    def f_s2(b):
                        i2 = b % 2
                        pb = ps[0].bitcast(BF16)
                        for k in range(8):
                            sc.op("pe", lambda e, k=k, pb=pb: e.transpose(
                                out=pb[:, k * 128:(k + 1) * 128], in_=hpb[i2][:, k * 128:(k + 1) * 128], identity=ident[:]),
                                r=[("hpb", i2), "ident"], w=[("ps", 0)])
                        evac(hpT[i2][:], pb.rearrange("p (k t) -> p k t", k=8), r=[("ps", 0)], w=[("hpT", i2)], eng="act")
                        pb1 = ps[1].bitcast(BF16)
                        for k in range(2):
                            sc.op("pe", lambda e, k=k, pb1=pb1: e.transpose(
                                out=pb1[:, k * 128:(k + 1) * 128], in_=pbf[b % 4][:, k * 128:(k + 1) * 128], identity=ident[:]),
                                r=[("pbf", b % 4), "ident"], w=[("ps", 1)])
                        evac(pT[i2][:], pb1[:, 0:256].rearrange("p (k t) -> p k t", k=2), r=[("ps", 1)], w=[("pT", i2)], eng="act")

                    def f_s3(b):
                        i2 = b % 2
                        for hf in range(2):
                            gbk = (2 if b % 2 == 0 else 6) + hf
                            pbk = 4 + hf
                            for k in range(8):
                                sc.op("pe", lambda e, k=k, hf=hf, gbk=gbk: e.matmul(
                                    ps[gbk][:], lhsT=hpT[i2][:, k, :], rhs=wpg[:, k, hf * 512:(hf + 1) * 512],
                                    start=(k == 0), stop=(k == 7)), r=[("hpT", i2), "wpg"], w=[("ps", gbk)])
                            for k in range(2):
                                sc.op("pe", lambda e, k=k, hf=hf, pbk=pbk: e.matmul(
                                    ps[pbk][:], lhsT=pT[i2][:, k, :], rhs=wpl[:, k, hf * 512:(hf + 1) * 512],
                                    start=(k == 0), stop=(k == 1)), r=[("pT", i2), "wpl"], w=[("ps", pbk)])
                            sc.op("act", lambda e, hf=hf, gbk=gbk: e.activation(
                                out=sig[i2][:, hf * 512:(hf + 1) * 512], in_=ps[gbk][:], func=AF.Sigmoid),
                                r=[("ps", gbk)], w=[("sig", i2)])
                            sc.op("dve", lambda e, hf=hf, pbk=pbk: e.tensor_tensor(
                                out=tmpx[i2][:, hf * 512:(hf + 1) * 512], in0=ps[pbk][:], in1=sig[i2][:, hf * 512:(hf + 1) * 512], op=ALU.mult),
                                r=[("ps", pbk), ("sig", i2)], w=[("tmpx", i2)])

                    def f_s4(b):
                        i2 = b % 2
                        sc.op("pool", lambda e: e.tensor_tensor(out=tmpx[i2][:], in0=tmpx[i2][:], in1=x1b[b % NX][:], op=ALU.add),
                              r=[("tmpx", i2), ("x1b", b % NX)], w=[("tmpx", i2)])
                        sc.op("act", lambda e: e.activation(out=junk[:], in_=tmpx[i2][:], func=AF.Square, accum_out=ssq7[:, b, 1:2]),
                              r=[("tmpx", i2)], w=["junk", ("ssq7b", b)])
                        rstd_ops(ssq7[:, b, 1:2], rs7[:, b, 1:2], ("ssq7b", b), ("rs7b", b))
                        sc.op("dve", lambda e: e.scalar_tensor_tensor(
                            out=ob[i2][:], in0=tmpx[i2][:], scalar=rs7[:, b, 1:2], in1=gfin_s[:], op0=ALU.mult, op1=ALU.mult),
                            r=[("tmpx", i2), ("rs7b", b), "gfin"], w=[("ob", i2)])
                        ld("sp", out_d[b * 128:(b + 1) * 128, :], ob[i2][:], ["out_d"], r=[("ob", i2)])

                    stg = [(f_s0, 0), (f_s1, 2), (f_s2, 3), (f_s3, 4), (f_s4, 5)]
                    for step in range(NB + 5):
                        for f, off in stg:
                            b = step - off
                            if 0 <= b < NB:
                                f(b)
        stats = sc.emit()
        print("stats", stats)
    return nc


S_FULL = 4096
C_CAP = 768
_NC_CACHE = {}


def kernel(x, p, norm_mix, w_in, ret_gn, w_br_ret, w_br_sb, w_out, norm_ffn,
           w_grp, b_grp, w_exp, b_exp, w_gate, w_up, w_down,
           norm_ple, w_ple, w_ple_gate, norm_final):
    f = lambda a: np.ascontiguousarray(np.asarray(a, dtype=np.float32))
    x = f(x); p = f(p)
    B = x.shape[0]
    if "nc" not in _NC_CACHE:
        _NC_CACHE["nc"] = build(S_FULL, C_CAP, debug=False)
    nc = _NC_CACHE["nc"]
    shared = {
        "w_in": f(w_in)[0], "w_br_ret": f(w_br_ret)[0], "w_br_sb": f(w_br_sb)[0], "w_out": f(w_out)[0],
        "w_rt": np.ascontiguousarray(np.concatenate([f(w_grp)[0], f(w_exp)[0]], axis=1)),
        "b_rt": rep(np.concatenate([f(b_grp)[0], f(b_exp)[0]])),
        "w_gate": f(w_gate)[0], "w_up": f(w_up)[0], "w_down": f(w_down)[0],
        "w_ple": f(w_ple)[0], "w_ple_gate": f(w_ple_gate)[0],
        "gmix": rep(f(norm_mix)[0]), "gffn": rep(f(norm_ffn)[0]), "gple": rep(f(norm_ple)[0]),
        "gfin": rep(f(norm_final)), "ggn": rep(f(ret_gn)[0]),
    }
    cst = consts_np(S_FULL, C_CAP)
    cst.pop("cdec")
    shared.update({k: np.ascontiguousarray(v) for k, v in cst.items()})
    in_maps = []
    for b in range(B):
        m = dict(shared)
        m["x"] = x[b]
        m["p"] = p[0, b]
        in_maps.append(m)
    res = run_bass_kernel_spmd(nc, in_maps, core_ids=list(range(B)))
    return np.stack([np.asarray(r["out"], dtype=np.float32) for r in res.results], axis=0)
```
